# Optimizing a Trainium2 kernel written in Bass

```python
import jax, jax.numpy as jnp
from jax import lax
import numpy as np

D_MODEL = 1024
BATCH = 4
SEQ = 4096
DEPTH = 2

GRID_W = 64
CTX_LEN = 256
D_MIX = D_MODEL
POOL_WINDOWS = (2, 4, 8, 16)
POOL_GROUPS = len(POOL_WINDOWS)
POOL_WIDTH = D_MIX // 4
POOL_GC = POOL_WIDTH // POOL_GROUPS
HEAD_DIM = 64
N_HEADS = (D_MIX // 2) // HEAD_DIM
N_KV_HEADS = 2
GQA_GROUP = N_HEADS // N_KV_HEADS
ATTN_WIDTH = N_HEADS * HEAD_DIM
KV_WIDTH = N_KV_HEADS * HEAD_DIM
WINDOW = 128
BLOCK = 128
ROPE_BASE = 10000.0
FOUR_WIDTH = D_MIX - POOL_WIDTH - ATTN_WIDTH
FOUR_GROUPS = 4
FOUR_GC = FOUR_WIDTH // FOUR_GROUPS
POOL_OFF = 0
Q_OFF = POOL_OFF + POOL_WIDTH
K_OFF = Q_OFF + ATTN_WIDTH
V_OFF = K_OFF + KV_WIDTH
FOUR_OFF = V_OFF + KV_WIDTH
IN_WIDTH = FOUR_OFF + FOUR_WIDTH
N_EXPERT_GROUPS = 4
EXPERTS_PER_GROUP = 4
N_EXPERTS = N_EXPERT_GROUPS * EXPERTS_PER_GROUP
TOP_K_IN_GROUP = 2
D_EXPERT = D_MODEL // 2
EPS = 1e-6
NEG_INF = -1e30

kernel_name = "hybrid_pool_swa_fourier_hmoe_dit"


def rms_norm(x, g):
    xf = x.astype(jnp.float32)
    y = xf * lax.rsqrt(jnp.mean(xf * xf, axis=-1, keepdims=True) + EPS)
    return (y * g.astype(jnp.float32)).astype(x.dtype)


def axial_rope_tables(n_tokens, dtype):
    rows = n_tokens // GRID_W
    r = jnp.broadcast_to(jnp.arange(rows)[:, None], (rows, GRID_W)).reshape(-1).astype(jnp.float32)
    col = jnp.broadcast_to(jnp.arange(GRID_W)[None, :], (rows, GRID_W)).reshape(-1).astype(jnp.float32)
    half = HEAD_DIM // 2
    inv = 1.0 / (ROPE_BASE ** (jnp.arange(0, half, 2, dtype=jnp.float32) / half))
    ar = r[:, None] * inv
    ac = col[:, None] * inv
    ang = jnp.concatenate([ar, ar, ac, ac], axis=-1)
    return jnp.cos(ang).astype(dtype), jnp.sin(ang).astype(dtype)


def apply_axial_rope(x, cos, sin):
    a, b, c, d = jnp.split(x, 4, axis=-1)
    rot = jnp.concatenate([-b, a, -d, c], axis=-1)
    return x * cos[None, :, None, :] + rot * sin[None, :, None, :]


def pool_mixer(u, w, scale):
    B, N, _ = u.shape
    uf = u.astype(jnp.float32).reshape(B, N, POOL_GROUPS, POOL_GC)
    cs = jnp.concatenate([jnp.zeros_like(uf[:, :1]), jnp.cumsum(uf, axis=1)], axis=1)
    t = jnp.arange(N)
    outs = []
    for g, win in enumerate(POOL_WINDOWS):
        lo = jnp.clip(t - win // 2, 0, N - 1)
        hi = jnp.clip(t + win // 2 - 1, 0, N - 1)
        cnt = (hi - lo + 1).astype(jnp.float32)
        s = cs[:, hi + 1, g] - cs[:, lo, g]
        outs.append(s / cnt[None, :, None] - uf[:, :, g])
    y = jnp.stack(outs, axis=2).astype(u.dtype)
    y = jnp.einsum('bngc,gcd->bngd', y, w).reshape(B, N, POOL_WIDTH)
    return y * scale


def fourier_mixer(u, w):
    B, N, _ = u.shape
    uf = u.astype(jnp.float32).reshape(B, N, FOUR_GROUPS, FOUR_GC)
    y = jnp.fft.fftn(uf, axes=(1, 3), norm="ortho").real.astype(u.dtype)
    return jnp.einsum('bngc,gcd->bngd', y, w).reshape(B, N, FOUR_WIDTH)


def split_qkv(p, q_g, k_g):
    B, N, _ = p.shape
    q = rms_norm(p[..., Q_OFF:K_OFF].reshape(B, N, N_HEADS, HEAD_DIM), q_g)
    k = rms_norm(p[..., K_OFF:V_OFF].reshape(B, N, N_KV_HEADS, HEAD_DIM), k_g)
    v = p[..., V_OFF:FOUR_OFF].reshape(B, N, N_KV_HEADS, HEAD_DIM)
    return q, k, v


def latent_window_attention(q, k, v, kc, vc, sink):
    B, N = q.shape[:2]
    L = kc.shape[1]
    nb = N // BLOCK
    scale = HEAD_DIM ** -0.5
    qb = q.reshape(B, nb, BLOCK, N_KV_HEADS, GQA_GROUP, HEAD_DIM)

    def band(t):
        tp = jnp.pad(t, ((0, 0), (BLOCK, BLOCK), (0, 0), (0, 0))).reshape(B, nb + 2, BLOCK, N_KV_HEADS, HEAD_DIM)
        return jnp.concatenate([tp[:, :-2], tp[:, 1:-1], tp[:, 2:]], axis=2)

    kw, vw = band(k), band(v)
    s_loc = jnp.einsum('bnqhgd,bnkhd->bnhgqk', qb, kw).astype(jnp.float32) * scale
    qpos = jnp.arange(nb)[:, None, None] * BLOCK + jnp.arange(BLOCK)[None, :, None]
    kpos = (jnp.arange(nb)[:, None, None] - 1) * BLOCK + jnp.arange(3 * BLOCK)[None, None, :]
    valid = (jnp.abs(qpos - kpos) <= WINDOW) & (kpos >= 0) & (kpos < N)
    s_loc = jnp.where(valid[None, :, None, None], s_loc, NEG_INF)
    s_ctx = jnp.einsum('bnqhgd,blhd->bnhgql', qb, kc).astype(jnp.float32) * scale
    s_sink = jnp.broadcast_to(sink.astype(jnp.float32).reshape(1, 1, N_KV_HEADS, GQA_GROUP, 1, 1),
                              s_loc.shape[:-1] + (1,))
    p = jax.nn.softmax(jnp.concatenate([s_loc, s_ctx, s_sink], axis=-1), axis=-1).astype(v.dtype)
    o = (jnp.einsum('bnhgqk,bnkhd->bnqhgd', p[..., :3 * BLOCK], vw)
         + jnp.einsum('bnhgql,blhd->bnqhgd', p[..., 3 * BLOCK:3 * BLOCK + L], vc))
    return o.reshape(B, N, ATTN_WIDTH)


def context_attention(qc, kc, vc, sink):
    B, L = qc.shape[:2]
    scale = HEAD_DIM ** -0.5
    qg = qc.reshape(B, L, N_KV_HEADS, GQA_GROUP, HEAD_DIM)
    s = jnp.einsum('blhgd,bmhd->bhglm', qg, kc).astype(jnp.float32) * scale
    s_sink = jnp.broadcast_to(sink.astype(jnp.float32).reshape(1, N_KV_HEADS, GQA_GROUP, 1, 1), s.shape[:-1] + (1,))
    p = jax.nn.softmax(jnp.concatenate([s, s_sink], axis=-1), axis=-1)[..., :L].astype(vc.dtype)
    return jnp.einsum('bhglm,bmhd->blhgd', p, vc).reshape(B, L, ATTN_WIDTH)


def merge_heads(p, attn_out, pool_w, pool_scale, four_w, w_out):
    pool_out = pool_mixer(p[..., POOL_OFF:Q_OFF], pool_w, pool_scale)
    four_out = fourier_mixer(p[..., FOUR_OFF:IN_WIDTH], four_w)
    return jnp.concatenate([pool_out, attn_out, four_out], axis=-1) @ w_out


def hier_moe(h, w_grp, b_grp, w_rtr, b_rtr, w_gate, w_up, w_down):
    T = h.shape[0]
    p_grp = jax.nn.softmax((h @ w_grp + b_grp).astype(jnp.float32), axis=-1)
    pg, g = lax.top_k(p_grp, 1)
    logits_e = (h @ w_rtr + b_rtr).astype(jnp.float32).reshape(T, N_EXPERT_GROUPS, EXPERTS_PER_GROUP)
    logits_sel = jnp.einsum('tge,tg->te', logits_e, jax.nn.one_hot(g[:, 0], N_EXPERT_GROUPS, dtype=jnp.float32))
    pe, ie = lax.top_k(jax.nn.softmax(logits_sel, axis=-1), TOP_K_IN_GROUP)
    wts = pg * pe / jnp.sum(pe, axis=-1, keepdims=True)
    eid = g * EXPERTS_PER_GROUP + ie
    gates = jnp.sum(jax.nn.one_hot(eid, N_EXPERTS, dtype=jnp.float32) * wts[..., None], axis=1).astype(h.dtype)
    y = jnp.zeros_like(h)
    for e in range(N_EXPERTS):
        a = jax.nn.silu(h @ w_gate[e]) * (h @ w_up[e])
        y = y + gates[:, e:e + 1] * (a @ w_down[e])
    return y


def setup_inputs(seed: int = 0) -> dict:
    key = jax.random.key(seed)
    ks = jax.random.split(key, 24)
    f32 = jnp.float32
    D = D_MODEL
    nrm = lambda k, shape, s: jax.random.normal(k, shape, f32) * s
    return {
        "x": nrm(ks[0], (BATCH, SEQ, D), 1.0),
        "c": nrm(ks[1], (BATCH, D), 1.0),
        "ctx": nrm(ks[2], (BATCH, CTX_LEN, D), 1.0),
        "c_ctx": nrm(ks[3], (D,), 1.0),
        "w_mod": nrm(ks[4], (DEPTH, D, 6 * D), 0.5 * D ** -0.5),
        "b_mod": nrm(ks[5], (DEPTH, 6 * D), 0.02),
        "norm1_g": 1.0 + nrm(ks[6], (DEPTH, D), 0.05),
        "w_in": nrm(ks[7], (DEPTH, D, IN_WIDTH), D ** -0.5),
        "q_norm_g": 1.0 + nrm(ks[8], (DEPTH, HEAD_DIM), 0.05),
        "k_norm_g": 1.0 + nrm(ks[9], (DEPTH, HEAD_DIM), 0.05),
        "attn_sink": nrm(ks[10], (DEPTH, N_HEADS), 0.5),
        "pool_w": nrm(ks[11], (DEPTH, POOL_GROUPS, POOL_GC, POOL_GC), POOL_GC ** -0.5),
        "pool_scale": 1.0 + nrm(ks[12], (DEPTH, POOL_WIDTH), 0.05),
        "four_w": nrm(ks[13], (DEPTH, FOUR_GROUPS, FOUR_GC, FOUR_GC), FOUR_GC ** -0.5),
        "w_out": nrm(ks[14], (DEPTH, D_MIX, D), D_MIX ** -0.5),
        "norm2_g": 1.0 + nrm(ks[15], (DEPTH, D), 0.05),
        "w_grp": nrm(ks[16], (DEPTH, D, N_EXPERT_GROUPS), D ** -0.5),
        "b_grp": nrm(ks[17], (DEPTH, N_EXPERT_GROUPS), 0.01),
        "w_rtr": nrm(ks[18], (DEPTH, D, N_EXPERTS), D ** -0.5),
        "b_rtr": nrm(ks[19], (DEPTH, N_EXPERTS), 0.01),
        "w_gate": nrm(ks[20], (DEPTH, N_EXPERTS, D, D_EXPERT), D ** -0.5),
        "w_up": nrm(ks[21], (DEPTH, N_EXPERTS, D, D_EXPERT), D ** -0.5),
        "w_down": nrm(ks[22], (DEPTH, N_EXPERTS, D_EXPERT, D), D_EXPERT ** -0.5),
    }


def reference(x, c, ctx, c_ctx, w_mod, b_mod, norm1_g, w_in, q_norm_g, k_norm_g, attn_sink,
              pool_w, pool_scale, four_w, w_out, norm2_g, w_grp, b_grp, w_rtr, b_rtr,
              w_gate, w_up, w_down):
    B, N, D = x.shape
    L = ctx.shape[1]
    cos, sin = axial_rope_tables(N, x.dtype)
    s_lat = jax.nn.silu(c)
    s_ctx = jax.nn.silu(c_ctx)
    xc = ctx
    for l in range(DEPTH):
        last = l == DEPTH - 1
        mod = s_lat @ w_mod[l] + b_mod[l]
        sh1, sc1, g1, sh2, sc2, g2 = jnp.split(mod[:, None, :], 6, axis=-1)
        modc = s_ctx @ w_mod[l] + b_mod[l]
        ch1, cs1, cg1, ch2, cs2, cg2 = jnp.split(modc, 6)

        hc = rms_norm(xc, norm1_g[l]) * (1.0 + cs1) + ch1
        if last:
            pkv = hc @ w_in[l][:, K_OFF:FOUR_OFF]
            kc = rms_norm(pkv[..., :KV_WIDTH].reshape(B, L, N_KV_HEADS, HEAD_DIM), k_norm_g[l])
            vc = pkv[..., KV_WIDTH:].reshape(B, L, N_KV_HEADS, HEAD_DIM)
        else:
            pc = hc @ w_in[l]
            qc, kc, vc = split_qkv(pc, q_norm_g[l], k_norm_g[l])
            yc = merge_heads(pc, context_attention(qc, kc, vc, attn_sink[l]),
                             pool_w[l], pool_scale[l], four_w[l], w_out[l])
            xc = xc + cg1 * yc

        h = rms_norm(x, norm1_g[l]) * (1.0 + sc1) + sh1
        p = h @ w_in[l]
        q, k, v = split_qkv(p, q_norm_g[l], k_norm_g[l])
        q = apply_axial_rope(q, cos, sin)
        k = apply_axial_rope(k, cos, sin)
        y = merge_heads(p, latent_window_attention(q, k, v, kc, vc, attn_sink[l]),
                        pool_w[l], pool_scale[l], four_w[l], w_out[l])
        x = x + g1 * y

        h2 = (rms_norm(x, norm2_g[l]) * (1.0 + sc2) + sh2).reshape(B * N, D)
        moe_args = (w_grp[l], b_grp[l], w_rtr[l], b_rtr[l], w_gate[l], w_up[l], w_down[l])
        if last:
            x = x + g2 * hier_moe(h2, *moe_args).reshape(B, N, D)
        else:
            h2c = (rms_norm(xc, norm2_g[l]) * (1.0 + cs2) + ch2).reshape(B * L, D)
            out = hier_moe(jnp.concatenate([h2, h2c], axis=0), *moe_args)
            x = x + g2 * out[:B * N].reshape(B, N, D)
            xc = xc + cg2 * out[B * N:].reshape(B, L, D)
    return x
```

```python
import types
import numpy as np
import ml_dtypes
from contextlib import ExitStack
import concourse.bass as bass
import concourse.mybir as mybir
from concourse.bass_utils import run_bass_kernel_spmd

F32 = mybir.dt.float32
BF16 = mybir.dt.bfloat16
AF = mybir.ActivationFunctionType
ALU = mybir.AluOpType
AX = mybir.AxisListType

D = 1024
NTOK = 2048
LC = 256
TOK = NTOK + LC
NB = 20
UPAD = 16
EPS = 1e-6
BIG = 30000.0
SAME_SYNC = True


class T:
    __slots__ = ("name", "w", "r")

    def __init__(self, name=""):
        self.name = name
        self.w = None
        self.r = []


def _freeze(fn):
    if fn.__closure__ is None:
        return fn
    cells = []
    for c in fn.__closure__:
        try:
            cells.append(types.CellType(c.cell_contents))
        except ValueError:
            cells.append(c)
    return types.FunctionType(fn.__code__, fn.__globals__, fn.__name__, fn.__defaults__, tuple(cells))


class Prog:
    ENG = ("pe", "act", "dve", "pool", "sp")

    def __init__(self, nc, stack, same_engine_sync=True):
        self.nc = nc
        self.stack = stack
        self.same = same_engine_sync
        self.sems = {}
        self.cnt = {}
        for e in self.ENG:
            self.sems[e] = stack.enter_context(nc.semaphore("s_" + e))
            self.cnt[e] = 0
        self.ops = {e: [] for e in self.ENG}
        self.waited = {e: {} for e in self.ENG}
        self.pending_silent = {e: False for e in self.ENG}
        self.ndsem = 0
        self.dkeys = []

    def dsem(self):
        k = "d%d" % self.ndsem
        self.ndsem += 1
        self.sems[k] = self.stack.enter_context(self.nc.semaphore("sd_" + k))
        self.cnt[k] = 0
        self.dkeys.append(k)
        return k

    def _need(self, eng, dep):
        if dep is None:
            return
        k, v = dep
        if k == eng:
            if not self.same or v > self.cnt[eng]:
                return
        if self.waited[eng].get(k, 0) >= v:
            return
        self.waited[eng][k] = v
        sem = self.sems[k]
        self.ops[eng].append(lambda h, sem=sem, v=v: h.wait_ge(sem, v))

    def _deps(self, eng, reads, writes):
        for t in reads:
            self._need(eng, t.w)
        for t in writes:
            self._need(eng, t.w)
            for d in t.r:
                self._need(eng, d)

    def _mark(self, tok, reads, writes):
        for t in reads:
            t.r.append(tok)
        for t in writes:
            t.w = tok
            t.r = []

    def op(self, eng, fn, reads=(), writes=(), inc=True):
        fn = _freeze(fn)
        self._deps(eng, reads, writes)
        seq = self.cnt[eng] + 1
        if inc:
            self.cnt[eng] = seq
            sem = self.sems[eng]
            self.ops[eng].append(lambda h, fn=fn, sem=sem: fn(h).then_inc(sem, 1))
            self.pending_silent[eng] = False
        else:
            self.ops[eng].append(lambda h, fn=fn: fn(h))
            self.pending_silent[eng] = True
        self._mark((eng, seq), reads, writes)

    def dma(self, q, out, in_, reads, writes, dk=None):
        if not hasattr(self, "dpool"):
            self.dpool = [self.dsem() for _ in range(40)]
            self.dnext = 0
        dk = self.dpool[self.dnext % len(self.dpool)]
        self.dnext += 1
        if self.cnt[dk] > 0:
            self._need(q, (dk, self.cnt[dk]))
        self._deps(q, reads, writes)
        self.cnt[dk] += 16
        sem = self.sems[dk]
        self.ops[q].append(lambda h, out=out, in_=in_, sem=sem: h.dma_start(out=out, in_=in_).then_inc(sem, 16))
        self._mark((dk, self.cnt[dk]), reads, writes)

    def collective(self, kind, groups, in_ap, out_ap, tin, tout, dk):
        q = "pool"
        if self.cnt[dk] > 0:
            self._need(q, (dk, self.cnt[dk]))
        self._deps(q, [tin], [tout])
        self.cnt[dk] += 1
        sem = self.sems[dk]
        self.ops[q].append(lambda h: h.collective_compute(kind, ALU.bypass, replica_groups=groups,
                                                          ins=[in_ap], outs=[out_ap]).then_inc(sem, 1))
        self._mark((dk, self.cnt[dk]), [tin], [tout])

    def barrier(self, exclude=()):
        for e in self.ENG:
            assert not self.pending_silent[e]
        keys = [k for k in list(self.ENG) + self.dkeys if k not in exclude]
        for e in self.ENG:
            for k in keys:
                if k != e and self.cnt[k] > 0:
                    self._need(e, (k, self.cnt[k]))

    def emit(self):
        for e in self.ENG:
            assert not self.pending_silent[e], "engine %s ends with silent op" % e
        nc = self.nc
        with nc.Block() as block:
            for e, deco in (("pe", block.tensor), ("act", block.scalar), ("dve", block.vector),
                            ("pool", block.gpsimd), ("sp", block.sync)):
                ops = self.ops[e]

                def body(h, ops=ops):
                    for f in ops:
                        f(h)
                deco(body)


class Ring:
    def __init__(self, items):
        self.items = items
        self.i = 0

    def next(self):
        it = self.items[self.i % len(self.items)]
        self.i += 1
        return it


def _bf(a):
    return np.ascontiguousarray(a.astype(ml_dtypes.bfloat16))


def _consts(par):
    c = {}
    pos = np.arange(NTOK) + NTOK * par
    r = (pos // 64).astype(np.float32)
    col = (pos % 64).astype(np.float32)
    half = 32
    inv = (1.0 / (np.float32(10000.0) ** (np.arange(0, half, 2, dtype=np.float32) / np.float32(half)))).astype(np.float32)
    ar = r[:, None] * inv
    ac = col[:, None] * inv
    ang = np.concatenate([ar, ar, ac, ac], axis=-1).astype(np.float32)
    cos = np.cos(ang).astype(np.float32).T
    sin = np.sin(ang).astype(np.float32).T
    cosT = np.ones((128, TOK), np.float32)
    sinT = np.zeros((128, TOK), np.float32)
    cosT[0:64, :NTOK] = cos; cosT[64:128, :NTOK] = cos
    sinT[0:64, :NTOK] = sin; sinT[64:128, :NTOK] = sin
    c["ropeC"] = _bf(cosT); c["ropeS"] = _bf(sinT)
    R = np.zeros((64, 64), np.float32)
    for i in range(16):
        R[i, i + 16] = -1.0
        R[i + 16, i] = 1.0
        R[i + 32, i + 48] = -1.0
        R[i + 48, i + 32] = 1.0
    R2 = np.zeros((128, 128), np.float32)
    R2[0:64, 0:64] = R; R2[64:, 64:] = R
    c["rotm"] = _bf(R2.T)
    hs = np.zeros((128, 128), np.float32)
    hs[0:64, 0:64] = 1.0 / 64; hs[64:, 64:] = 1.0 / 64
    c["hsum"] = _bf(hs)
    c["ones128"] = _bf(np.ones((128, 128), np.float32))
    c["ident32"] = np.eye(128, dtype=np.float32)
    kk = np.arange(128)[:, None]; qq = np.arange(128)[None, :]
    mprev = (kk >= qq).astype(np.float32)
    mnext = (kk <= qq).astype(np.float32)
    masks = np.zeros((128, 4, 128), np.float32)
    masks[:, 0] = mprev; masks[:, 1] = mnext
    masks[:, 2] = mprev if par == 1 else 0.0
    masks[:, 3] = mnext if par == 0 else 0.0
    c["masks"] = _bf(masks)
    wins = {(0, 0): 2, (0, 1): 4, (1, 0): 8, (1, 1): 16}
    fix = np.ones((128, 2, 2, 8), np.float32)
    Nn = 4096
    for (ch, hf), w in wins.items():
        rows = slice(hf * 64, hf * 64 + 64)
        for j in range(8):
            t = j
            lo = max(t - w // 2, 0); hi = min(t + w // 2 - 1, Nn - 1)
            fix[rows, ch, 0, j] = w / (hi - lo + 1)
            t = Nn - 8 + j
            lo = max(t - w // 2, 0); hi = min(t + w // 2 - 1, Nn - 1)
            fix[rows, ch, 1, j] = w / (hi - lo + 1)
    c["poolfixc"] = fix.copy()
    f2 = fix.copy()
    if par == 0:
        f2[:, :, 1, :] = 1.0
    else:
        f2[:, :, 0, :] = 1.0
    c["poolfix"] = f2
    hv = np.zeros((128, 2), np.float32)
    hv[:, 0] = 1.0 if par == 1 else 0.0
    hv[:, 1] = 1.0 if par == 0 else 0.0
    c["halov"] = hv
    n = np.arange(2048, dtype=np.int64)[:, None]
    k = (np.arange(NTOK, dtype=np.int64) + NTOK * par)[None, :]
    ph = ((n * k) % 4096).astype(np.float64) * (2.0 * np.pi / 4096.0)
    tab = np.stack([np.cos(ph), -np.sin(ph)], axis=1)
    tab = tab.reshape(4, 4, 128, 2, 2, 512, 2)
    tab = tab.transpose(6, 4, 0, 2, 1, 3, 5)
    c["dfttab"] = _bf(tab)
    n = np.arange(256, dtype=np.int64)[:, None]; k = np.arange(256, dtype=np.int64)[None, :]
    ph = ((n * k) % 256).astype(np.float64) * (2.0 * np.pi / 256.0)
    t2 = np.stack([np.cos(ph), -np.sin(ph)], axis=1)
    t2 = 4.0 * t2.reshape(2, 128, 2, 256).transpose(1, 0, 2, 3)
    c["dft256"] = _bf(t2)
    cc = np.arange(64, dtype=np.int64)
    ph = ((cc[:, None] * cc[None, :]) % 64).astype(np.float64) * (2.0 * np.pi / 64.0)
    d64 = np.stack([np.cos(ph) / 512.0, np.sin(ph) / 512.0], axis=1)
    c["dft64"] = d64.astype(np.float32)
    sel = np.zeros((16, 16, 128), np.float32)
    for e in range(16):
        sel[e, e, :] = 1.0
    c["sel"] = _bf(sel)
    return c


def host_prep(x, c, ctx, c_ctx, w_mod, b_mod, norm1_g, w_in, q_norm_g, k_norm_g, attn_sink,
              pool_w, pool_scale, four_w, w_out, norm2_g, w_grp, b_grp, w_rtr, b_rtr,
              w_gate, w_up, w_down):
    f = lambda a: np.ascontiguousarray(np.asarray(a, dtype=np.float32))
    x, c, ctx, c_ctx = f(x), f(c), f(ctx), f(c_ctx)
    w_in = f(w_in); w_out = f(w_out)
    sh = {}
    sh["w_mod"] = f(w_mod)
    sh["bmodT"] = f(np.asarray(b_mod).reshape(2, 48, 128).transpose(0, 2, 1))
    sh["n1g"] = f(np.asarray(norm1_g).reshape(2, 8, 128).transpose(0, 2, 1))
    sh["n2g"] = f(np.asarray(norm2_g).reshape(2, 8, 128).transpose(0, 2, 1))
    qcols = []
    for g in range(4):
        qcols += list(range(256 + g * 64, 256 + g * 64 + 64)) + list(range(256 + (4 + g) * 64, 256 + (4 + g) * 64 + 64))
    cols = list(range(0, 256)) + qcols + list(range(768, 896)) + list(range(896, 1024))
    sh["w_inp"] = f(w_in[:, :, cols])
    sh["w_inFT"] = f(w_in[:, :, 1024:1280].reshape(2, 1024, 4, 64).transpose(0, 3, 2, 1))
    sh["four_w2"] = f(np.asarray(four_w).transpose(0, 2, 1, 3))
    sh["qg"] = f(np.tile(np.asarray(q_norm_g), (1, 2))[:, :, None])
    sh["kg"] = f(np.tile(np.asarray(k_norm_g), (1, 2))[:, :, None])
    sh["sinkb"] = f(np.broadcast_to(np.asarray(attn_sink).reshape(2, 1, 2, 4), (2, 128, 2, 4)))
    pw = np.asarray(pool_w, dtype=np.float32)
    bd = np.zeros((2, 2, 128, 128), np.float32)
    for l in range(2):
        for ch in range(2):
            bd[l, ch, 0:64, 0:64] = pw[l, 2 * ch]
            bd[l, ch, 64:, 64:] = pw[l, 2 * ch + 1]
    sh["poolw_bd"] = bd
    sh["pscale"] = f(np.asarray(pool_scale).reshape(2, 2, 128).transpose(0, 2, 1))
    arows = []
    for g in range(4):
        arows += list(range(256 + g * 64, 256 + g * 64 + 64)) + list(range(256 + (4 + g) * 64, 256 + (4 + g) * 64 + 64))
    rows = list(range(0, 256)) + arows + list(range(768, 1024))
    sh["w_outp"] = f(w_out[:, rows, :])
    sh["wr"] = f(np.concatenate([np.asarray(w_grp), np.asarray(w_rtr)], axis=-1))
    br = np.concatenate([np.asarray(b_grp), np.asarray(b_rtr)], axis=-1)
    sh["br"] = f(np.broadcast_to(br[:, None, :], (2, 128, 20)))
    sh["w_gate"] = f(w_gate); sh["w_up"] = f(w_up); sh["w_down"] = f(w_down)
    cons = [_consts(0), _consts(1)]
    in_maps = []
    for core in range(8):
        b, par = core // 2, core % 2
        m = dict(sh)
        m.update(cons[par])
        m["xT"] = f(x[b, par * NTOK:(par + 1) * NTOK, :].T)
        m["ctxT"] = f(ctx[b].T)
        m["sT"] = f(np.stack([c[b], c_ctx], axis=1))
        in_maps.append(m)
    return in_maps


IN_SPECS = [
    ("xT", [D, NTOK], F32), ("ctxT", [D, LC], F32), ("sT", [D, 2], F32),
    ("w_mod", [2, D, 6 * D], F32), ("bmodT", [2, 128, 48], F32), ("n1g", [2, 128, 8], F32), ("n2g", [2, 128, 8], F32),
    ("w_inp", [2, D, 1024], F32), ("w_inFT", [2, 64, 4, D], F32), ("four_w2", [2, 64, 4, 64], F32),
    ("qg", [2, 128, 1], F32), ("kg", [2, 128, 1], F32), ("sinkb", [2, 128, 2, 4], F32),
    ("poolw_bd", [2, 2, 128, 128], F32), ("pscale", [2, 128, 2], F32), ("w_outp", [2, D, D], F32),
    ("wr", [2, D, 20], F32), ("br", [2, 128, 20], F32),
    ("w_gate", [2, 16, D, 512], F32), ("w_up", [2, 16, D, 512], F32), ("w_down", [2, 16, 512, D], F32),
    ("ropeC", [128, TOK], BF16), ("ropeS", [128, TOK], BF16), ("rotm", [128, 128], BF16), ("hsum", [128, 128], BF16),
    ("ones128", [128, 128], BF16), ("ident32", [128, 128], F32), ("masks", [128, 4, 128], BF16),
    ("poolfix", [128, 2, 2, 8], F32), ("poolfixc", [128, 2, 2, 8], F32), ("halov", [128, 2], F32),
    ("dfttab", [2, 2, 4, 128, 4, 2, 512], BF16), ("dft256", [128, 2, 2, 256], BF16), ("dft64", [64, 2, 64], F32),
    ("sel", [16, 16, 128], BF16),
]


def build(layers=(0, 1), stop=None, dumps=()):
    nc = bass.Bass("TRN2", target_bir_lowering=False)
    I = {}
    for name, shape, dt in IN_SPECS:
        I[name] = nc.dram_tensor(name, shape, dt, kind="ExternalInput").ap()
    outT = nc.dram_tensor("outT", [D, NTOK], F32, kind="ExternalOutput").ap()
    DUMP = {}
    dump_specs = {"xT": [D, NTOK], "xcT": [D, LC], "catT": [D, TOK], "mod": [128, 2 * 48 * 2], "kz": [128, 2 * NB * 128],
                  "vaug": [128, NB * 256], "gates": [16, TOK]}
    for dn in dumps:
        DUMP[dn] = nc.dram_tensor("dump_" + dn, dump_specs[dn], F32, kind="ExternalOutput").ap()
    gin_ab = nc.dram_tensor("gin_ab", [NTOK, 512], BF16).ap()
    gout_ab = nc.dram_tensor("gout_ab", [2 * NTOK, 512], BF16).ap()
    gin_h = nc.dram_tensor("gin_h", [256, 512], BF16).ap()
    gout_h = nc.dram_tensor("gout_h", [512, 512], BF16).ap()
    t_gin_ab, t_gout_ab, t_gin_h, t_gout_h = T("gin_ab"), T("gout_ab"), T("gin_h"), T("gout_h")
    PAIRS = [[0, 1], [2, 3], [4, 5], [6, 7]]

    with ExitStack() as top:
        P = Prog(nc, top, same_engine_sync=SAME_SYNC)

        uid = [0]

        def sbt(st, name, shape, dt):
            uid[0] += 1
            return st.enter_context(nc.sbuf_tensor("sb%d_%s" % (uid[0], name), shape, dt))

        xT = sbt(top, "xT", [128, 8, NTOK], F32)
        xcT = sbt(top, "xcT", [128, 8, LC], F32)
        catT = sbt(top, "catT", [128, 8, TOK], BF16)
        modT = sbt(top, "modT", [128, 2, 48, 2], F32)
        A1s = sbt(top, "A1s", [128, 2, 8, 2], F32)
        A2s = sbt(top, "A2s", [128, 2, 8, 2], F32)
        rotm = sbt(top, "rotm", [128, 128], BF16)
        hsum = sbt(top, "hsum", [128, 128], BF16)
        ones128 = sbt(top, "ones128", [128, 128], BF16)
        ident32 = sbt(top, "ident32", [128, 128], F32)
        epsb = sbt(top, "epsb", [128, 1], F32)
        TX = [[T("x%d_%d" % (m, g)) for g in range(4)] for m in range(8)]
        TXC = [T("xc%d" % m) for m in range(8)]
        TC = [[T("c%d_%d" % (k, b)) for b in range(18)] for k in range(8)]
        t_modl = {0: T("mod0"), 1: T("mod1")}
        t_Al = {0: T("A0"), 1: T("A1")}
        t_const = T("const")
        PSA = top.enter_context(nc.psum_tensor("PSA", [128, 2048], F32))
        PSB = top.enter_context(nc.psum_tensor("PSB", [128, 2048], F32))
        TP = [T("bank%d" % i) for i in range(8)]

        def bank(i):
            big = PSA if i < 4 else PSB
            j = i % 4
            return big[:, j * 512:(j + 1) * 512]
        bank_rr = [0]

        reserved = set()

        def next_bank():
            while True:
                i = bank_rr[0] % 8
                bank_rr[0] += 1
                if i not in reserved:
                    return bank(i), TP[i]

        dk_const = P.dsem()
        dk_x = P.dsem()
        dk_w = [P.dsem() for _ in range(4)]
        dk_misc = P.dsem()
        dk_out = P.dsem()
        dk_gw = P.dsem()
        dk_cc = P.dsem()
        dk_cc2 = P.dsem()
        dk_gr = P.dsem()
        dk_tab = [P.dsem() for _ in range(3)]
        dk_ex = [[P.dsem() for _ in range(3)] for _ in range(2)]

        for nm, tl in (("rotm", rotm), ("hsum", hsum), ("ones128", ones128), ("ident32", ident32)):
            P.dma("sp", tl[:], I[nm], [], [t_const], dk_const)
        P.op("pool", lambda h: h.memset(epsb[:], EPS), [], [t_const])
        for m in range(8):
            for g in range(4):
                P.dma("sp", xT[:, m, g * 512:(g + 1) * 512], I["xT"][m * 128:(m + 1) * 128, g * 512:(g + 1) * 512], [], [TX[m][g]], dk_x)
            P.dma("sp", xcT[:, m, :], I["ctxT"][m * 128:(m + 1) * 128, :], [], [TXC[m]], dk_x)

        def cat_T(ks, c0, n):
            return [TC[k][b] for k in ks for b in range(c0 // 128, (c0 + n + 127) // 128)]

        s32 = sbt(top, "s32", [128, 8, 2], F32); t_s32 = T()
        sbf = sbt(top, "sbf", [128, 8, 2], BF16); t_sbf = T()
        bmod = sbt(top, "bmod", [128, 2, 48], F32); t_bmod = T()
        ngam = sbt(top, "ngam", [128, 2, 2, 8], F32); t_ng = T()
        P.dma("sp", s32[:], I["sT"].rearrange("(k p) c -> p k c", p=128), [], [t_s32], dk_misc)
        P.dma("sp", bmod[:], I["bmodT"].rearrange("l p n -> p l n"), [], [t_bmod], dk_misc)
        P.dma("sp", ngam[:, 0], I["n1g"].rearrange("l p k -> p l k"), [], [t_ng], dk_misc)
        P.dma("sp", ngam[:, 1], I["n2g"].rearrange("l p k -> p l k"), [], [t_ng], dk_misc)
        P.op("act", lambda h: h.activation(out=sbf[:], in_=s32[:], func=AF.Silu), [t_s32], [t_sbf])

        def mod_piece_dma(l, v, wmt, t_wmt):
            P.dma("pool", wmt[:], I["w_mod"][l, :, v * 1024:(v + 1) * 1024].rearrange("(k p) c -> p k c", p=128), [], [t_wmt], dk_misc)

        def mod_piece_mm(l, v, wmt, t_wmt, pb, tpb):
            for c8 in range(8):
                n = v * 8 + c8
                for k in range(8):
                    P.op("pe", lambda h, o=pb[:, 2 * n:2 * n + 2], w=wmt[:, k, c8 * 128:(c8 + 1) * 128], r=sbf[:, k, :], k=k:
                         h.matmul(o, lhsT=w, rhs=r, start=(k == 0), stop=(k == 7)),
                         [t_wmt, t_sbf], [tpb], inc=(k == 7))

        def mod_finalize(l, pb, tpb):
            P.op("dve", lambda h, l=l, pb=pb: h.tensor_tensor(out=modT[:, l], in0=pb[:, 0:96].rearrange("p (n c) -> p n c", c=2),
                                                              in1=bmod[:, l].unsqueeze(2).to_broadcast([128, 48, 2]), op=ALU.add),
                 [tpb, t_bmod], [t_modl[l]])
            P.op("dve", lambda h, l=l: h.scalar_tensor_tensor(out=A1s[:, l], in0=modT[:, l, 8:16, :], scalar=1.0,
                                                              in1=ngam[:, 0, l].unsqueeze(2).to_broadcast([128, 8, 2]),
                                                              op0=ALU.add, op1=ALU.mult), [t_modl[l], t_ng], [t_Al[l]])
            P.op("dve", lambda h, l=l: h.scalar_tensor_tensor(out=A2s[:, l], in0=modT[:, l, 32:40, :], scalar=1.0,
                                                              in1=ngam[:, 1, l].unsqueeze(2).to_broadcast([128, 8, 2]),
                                                              op0=ALU.add, op1=ALU.mult), [t_modl[l], t_ng], [t_Al[l]])

        def mod_finalize_part(l, pb, tpb, v0, v1):
            P.op("dve", lambda h, l=l, pb=pb: h.tensor_tensor(out=modT[:, l, v0 * 8:v1 * 8, :], in0=pb[:, v0 * 16:v1 * 16].rearrange("p (n c) -> p n c", c=2),
                                                              in1=bmod[:, l, v0 * 8:v1 * 8].unsqueeze(2).to_broadcast([128, (v1 - v0) * 8, 2]), op=ALU.add),
                 [tpb, t_bmod], [t_modl[l]])
            if v0 <= 1 < v1:
                P.op("dve", lambda h, l=l: h.scalar_tensor_tensor(out=A1s[:, l], in0=modT[:, l, 8:16, :], scalar=1.0,
                                                                  in1=ngam[:, 0, l].unsqueeze(2).to_broadcast([128, 8, 2]),
                                                                  op0=ALU.add, op1=ALU.mult), [t_modl[l], t_ng], [t_Al[l]])
            if v0 <= 4 < v1:
                P.op("dve", lambda h, l=l: h.scalar_tensor_tensor(out=A2s[:, l], in0=modT[:, l, 32:40, :], scalar=1.0,
                                                                  in1=ngam[:, 1, l].unsqueeze(2).to_broadcast([128, 8, 2]),
                                                                  op0=ALU.add, op1=ALU.mult), [t_modl[l], t_ng], [t_Al[l]])

        HIDE_MOD1 = (len(layers) == 2)
        with ExitStack() as st:
            wm = [sbt(st, "wm%d" % i, [128, 8, 1024], BF16) for i in range(2)]
            t_wm = [T(), T()]
            it = 0
            for l in (layers[:1] if HIDE_MOD1 else layers):
                pb, tpb = next_bank()
                for v in range(2 if HIDE_MOD1 else 6):
                    slot = it % 2
                    it += 1
                    mod_piece_dma(l, v, wm[slot], t_wm[slot])
                    mod_piece_mm(l, v, wm[slot], t_wm[slot], pb, tpb)
                if HIDE_MOD1:
                    mod_finalize_part(l, pb, tpb, 0, 2)
                else:
                    mod_finalize(l, pb, tpb)
            P.barrier()
        if "mod" in DUMP:
            P.dma("sp", DUMP["mod"], modT[:].rearrange("p l n c -> p (l n c)"), [t_modl[0], t_modl[1]], [], dk_out)

        def modv(l, v, k, c):
            return modT[:, l, v * 8 + k, c:c + 1]

        GROUPS = [(g * 512, 512, 0) for g in range(4)] + [(NTOK, LC, 1)]

        def x_ap(k, c0, n):
            if c0 < NTOK:
                return xT[:, k, c0:c0 + n]
            return xcT[:, k, c0 - NTOK:c0 - NTOK + n]

        def x_T(k, c0):
            if c0 < NTOK:
                return TX[k][c0 // 512]
            return TXC[k]

        def norm_mod(st_tiles, l, which, c0, n, isctx, dst, t_dst, bias_eng="act"):
            rstds, tmps = st_tiles
            rstd, t_rstd = rstds.next()
            As = A1s if which == 0 else A2s
            shv = 0 if which == 0 else 3
            pb, tpb = next_bank()
            for k in range(8):
                P.op("act", lambda h, k=k: h.activation(out=dst(k), in_=x_ap(k, c0, n), func=AF.Square),
                     [x_T(k, c0)], t_dst(k))
            for k in range(8):
                P.op("pe", lambda h, k=k: h.matmul(pb[:, 0:n], lhsT=ones128[:], rhs=dst(k), start=(k == 0), stop=(k == 7)),
                     t_dst(k) + [t_const], [tpb], inc=(k == 7))
            P.op("act", lambda h: h.activation(out=rstd[:, 0:n], in_=pb[:, 0:n], func=AF.Ln, bias=epsb[:, 0:1], scale=1.0 / D), [tpb, t_const], [t_rstd])
            P.op("act", lambda h: h.activation(out=rstd[:, 0:n], in_=rstd[:, 0:n], func=AF.Exp, scale=-0.5), [t_rstd], [t_rstd])
            for k in range(8):
                tmp, t_tmp = tmps.next()
                P.op("dve", lambda h, k=k, tmp=tmp: h.scalar_tensor_tensor(out=tmp[:, 0:n], in0=x_ap(k, c0, n), scalar=As[:, l, k, isctx:isctx + 1],
                                                                          in1=rstd[:, 0:n], op0=ALU.mult, op1=ALU.mult),
                     [x_T(k, c0), t_Al[l], t_rstd] + t_dst(k), [t_tmp])
                if bias_eng == "pool":
                    P.op("pool", lambda h, k=k, tmp=tmp: h.tensor_scalar(out=dst(k), in0=tmp[:, 0:n], scalar1=modv(l, shv, k, isctx), scalar2=None, op0=ALU.add),
                         [t_tmp, t_modl[l]], t_dst(k))
                else:
                    P.op("act", lambda h, k=k, tmp=tmp: h.activation(out=dst(k), in_=tmp[:, 0:n], func=AF.Identity,
                                                                     bias=modv(l, shv, k, isctx), scale=1.0),
                         [t_tmp, t_modl[l]], t_dst(k))

        for l in layers:
            last = (l == 1)
            with ExitStack() as mx:
                ABc = sbt(mx, "ABc", [128, 2, 512], BF16)
                qkg = sbt(mx, "qkg", [128, 2], F32)
                esink = sbt(mx, "esink", [128, 2, 4], F32)
                mxa = mx.enter_context(ExitStack())
                upad = sbt(mxa, "upad", [128, 2, UPAD + NTOK + UPAD], BF16)
                upadc = sbt(mxa, "upadc", [128, 2, UPAD + LC + UPAD], BF16)
                mxb = mxa.enter_context(ExitStack())
                Kz = [sbt(mxb, "Kz%d" % i, [128, NB * 128], BF16) for i in range(2)]
                Vaug = sbt(mxb, "Vaug", [128, NB, 2, 128], BF16)
                ropeC = sbt(mxb, "ropeC", [128, TOK], BF16)
                ropeS = sbt(mxb, "ropeS", [128, TOK], BF16)
                masks = sbt(mxb, "masks", [128, 4, 128], BF16)
                TK = [T("k%d" % b) for b in range(NB)]
                TV = [T("v%d" % b) for b in range(NB)]
                t_rope, t_masks, t_qkg, t_esink, t_ABc = T(), T(), T(), T(), T()
                TU = [T("u%d" % g) for g in range(4)]
                t_uh, t_uc = T("uh"), T("uc")
                P.dma("sp", ropeC[:], I["ropeC"], [], [t_rope], dk_misc)
                P.dma("sp", ropeS[:], I["ropeS"], [], [t_rope], dk_misc)
                P.dma("sp", masks[:], I["masks"], [], [t_masks], dk_misc)
                P.dma("sp", qkg[:, 0:1], I["qg"][l], [], [t_qkg], dk_misc)
                P.dma("sp", qkg[:, 1:2], I["kg"][l], [], [t_qkg], dk_misc)
                P.dma("sp", esink[:], I["sinkb"][l], [], [t_esink], dk_misc)
                P.op("act", lambda h: h.activation(out=esink[:], in_=esink[:], func=AF.Exp), [t_esink], [t_esink])
                P.op("pool", lambda h: h.memset(Kz[0][:], 0.0), [], TK)
                P.op("pool", lambda h: h.memset(Kz[1][:], 0.0), [], TK)
                P.op("pool", lambda h: h.memset(Vaug[:], 1.0), [], TV)
                P.op("pool", lambda h: h.memset(upad[:], 0.0), [], TU + [t_uh])
                P.op("pool", lambda h: h.memset(upadc[:], 0.0), [], [t_uc])

                with ExitStack() as st:
                    win = sbt(st, "win", [128, 8, 1536], BF16); t_win, t_wab = T(), T()
                    fs = st.enter_context(ExitStack())
                    wft = sbt(fs, "wft", [64, 4, D], F32); t_wft = T()
                    fw2 = sbt(fs, "fw2", [64, 4, 64], F32); t_fw2 = T()
                    d64 = sbt(fs, "d64", [64, 2, 64], F32); t_d64 = T()
                    m1 = sbt(fs, "m1", [64, 4, 2, 64], F32); t_m1 = T()
                    P.dma("pool", win[:, :, 0:1024], I["w_inp"][l].rearrange("(k p) c -> p k c", p=128), [], [t_win], dk_w[2])
                    P.dma("sp", wft[:], I["w_inFT"][l], [], [t_wft], dk_misc)
                    P.dma("sp", fw2[:], I["four_w2"][l], [], [t_fw2], dk_misc)
                    P.dma("sp", d64[:], I["dft64"], [], [t_d64], dk_misc)
                    pb, tpb = next_bank()
                    for g in range(4):
                        for cs in range(2):
                            P.op("pe", lambda h, g=g, cs=cs, pb=pb: h.matmul(pb[0:64, (g * 2 + cs) * 64:(g * 2 + cs + 1) * 64], lhsT=d64[:, cs, :], rhs=fw2[:, g, :],
                                                                             start=True, stop=True), [t_d64, t_fw2], [tpb], inc=(g == 3 and cs == 1))
                    P.op("dve", lambda h, pb=pb: h.tensor_copy(out=m1[:].rearrange("p g c d -> p (g c d)"), in_=pb[0:64, 0:512]), [tpb], [t_m1])
                    for k in range(8):
                        pb, tpb = next_bank()
                        for g in range(4):
                            P.op("pe", lambda h, g=g, k=k, pb=pb: h.matmul(pb[:, g * 128:(g + 1) * 128], lhsT=wft[:, g, k * 128:(k + 1) * 128],
                                                                           rhs=m1[:, g].rearrange("p c d -> p (c d)"), start=True, stop=True),
                                 [t_wft, t_m1], [tpb], inc=(g == 3))
                        P.op("act", lambda h, k=k, pb=pb: h.copy(out=win[:, k, 1024:1536].rearrange("p (c g d) -> p g c d", c=2, g=4),
                                                                 in_=pb[:, 0:512].rearrange("p (g c d) -> p g c d", g=4, c=2)), [tpb], [t_wab])

                    P.barrier()
                    fs.close()
                    AN = 256
                    hTs = [(sbt(st, "hT%d" % i, [128, 8, AN], BF16), T()) for i in range(2)]
                    rstds = Ring([(sbt(st, "rstd%d" % i, [128, AN], F32), T()) for i in range(2)])
                    tmps = Ring([(sbt(st, "ntmp%d" % i, [128, AN], F32), T()) for i in range(3)])
                    sqq = Ring([(sbt(st, "sqq%d" % i, [128, AN], BF16), T()) for i in range(3)])
                    sdr = Ring([(sbt(st, "sd%d" % i, [128, AN], F32), T()) for i in range(3)])
                    qnr = Ring([(sbt(st, "qn%d" % i, [128, AN], BF16), T()) for i in range(3)])
                    t1r = Ring([(sbt(st, "t1_%d" % i, [128, AN], F32), T()) for i in range(3)])
                    t2r = Ring([(sbt(st, "t2_%d" % i, [128, AN], F32), T()) for i in range(3)])
                    stg = Ring([(sbt(st, "stg%d" % i, [128, 512], BF16), T()) for i in range(3)])
                    kcol_of = lambda c0: (128 + c0) if c0 < NTOK else (18 * 128 + c0 - NTOK)
                    GROUPS_A1 = [(g * AN, AN, 0) for g in range(NTOK // AN)] + [(NTOK, LC, 1)]

                    def emit_norm(gi):
                        c0, n, isctx = GROUPS_A1[gi]
                        hT, t_hT = hTs[gi % 2]
                        norm_mod((rstds, tmps), l, 0, c0, n, isctx, lambda k, hT=hT, n=n: hT[:, k, 0:n], lambda k, t_hT=t_hT: [t_hT], bias_eng="pool")

                    def emit_chunks(gi):
                        c0, n, isctx = GROUPS_A1[gi]
                        hT, t_hT = hTs[gi % 2]
                        need_full = (not last) or (not isctx)
                        mlist = list(range(7)) if need_full else [6]
                        state = {}

                        def stage1(m):
                            pb, tpb = next_bank()
                            for k in range(8):
                                P.op("pe", lambda h, k=k, m=m, pb=pb: h.matmul(pb[:, 0:n], lhsT=win[:, k, m * 128:(m + 1) * 128], rhs=hT[:, k, 0:n],
                                                                               start=(k == 0), stop=(k == 7)), [t_win, t_hT], [tpb], inc=(k == 7))
                            if m < 2:
                                if isctx:
                                    P.op("act", lambda h, m=m, pb=pb: h.copy(out=upadc[:, m, UPAD:UPAD + n], in_=pb[:, 0:n]), [tpb], [t_uc])
                                else:
                                    P.op("act", lambda h, m=m, pb=pb: h.copy(out=upad[:, m, UPAD + c0:UPAD + c0 + n], in_=pb[:, 0:n]), [tpb], [TU[c0 // 512]])
                                return
                            sq, t_sq = sqq.next()
                            P.op("act", lambda h, pb=pb, sq=sq: h.activation(out=sq[:, 0:n], in_=pb[:, 0:n], func=AF.Square), [tpb], [t_sq])
                            state[m] = dict(pb=pb, tpb=tpb, sq=sq, t_sq=t_sq)

                        def stage2(m):
                            if m < 2:
                                return
                            S = state[m]
                            isk = (m == 6)
                            pb2, tpb2 = next_bank()
                            P.op("pe", lambda h, pb2=pb2, sq=S["sq"]: h.matmul(pb2[:, 0:n], lhsT=hsum[:], rhs=sq[:, 0:n], start=True, stop=True), [S["t_sq"], t_const], [tpb2])
                            sd, t_sd = sdr.next()
                            P.op("act", lambda h, pb2=pb2, sd=sd: h.activation(out=sd[:, 0:n], in_=pb2[:, 0:n], func=AF.Ln, bias=epsb[:, 0:1], scale=1.0), [tpb2, t_const], [t_sd])
                            P.op("act", lambda h, sd=sd: h.activation(out=sd[:, 0:n], in_=sd[:, 0:n], func=AF.Exp, scale=-0.5), [t_sd], [t_sd])
                            qn, t_qn = qnr.next()
                            P.op("dve", lambda h, pb=S["pb"], sd=sd, qn=qn, isk=isk: h.scalar_tensor_tensor(out=qn[:, 0:n], in0=pb[:, 0:n], scalar=qkg[:, (1 if isk else 0):(2 if isk else 1)],
                                                                                                          in1=sd[:, 0:n], op0=ALU.mult, op1=ALU.mult), [S["tpb"], t_sd, t_qkg], [t_qn])
                            S.update(qn=qn, t_qn=t_qn)

                        def stage3(m):
                            if m < 2:
                                return
                            S = state[m]
                            isk = (m == 6)
                            qn, t_qn = S["qn"], S["t_qn"]
                            pb3, tpb3 = next_bank()
                            P.op("pe", lambda h, pb3=pb3, qn=qn: h.matmul(pb3[:, 0:n], lhsT=rotm[:], rhs=qn[:, 0:n], start=True, stop=True), [t_qn, t_const], [tpb3])
                            t1, t_t1 = t1r.next()
                            t2, t_t2 = t2r.next()
                            P.op("pool", lambda h, t1=t1, qn=qn: h.tensor_tensor(out=t1[:, 0:n], in0=qn[:, 0:n], in1=ropeC[:, c0:c0 + n], op=ALU.mult), [t_qn, t_rope], [t_t1])
                            P.op("dve", lambda h, t2=t2, pb3=pb3: h.tensor_tensor(out=t2[:, 0:n], in0=pb3[:, 0:n], in1=ropeS[:, c0:c0 + n], op=ALU.mult), [tpb3, t_rope], [t_t2])
                            if isk:
                                kc = kcol_of(c0)
                                tks = [TK[b] for b in range(kc // 128, (kc + n) // 128)]
                                P.op("pool", lambda h, t1=t1, t2=t2, kc=kc: h.tensor_tensor(out=Kz[0][0:64, kc:kc + n], in0=t1[0:64, 0:n], in1=t2[0:64, 0:n], op=ALU.add), [t_t1, t_t2], tks)
                                P.op("pool", lambda h, t1=t1, t2=t2, kc=kc: h.tensor_tensor(out=Kz[1][64:128, kc:kc + n], in0=t1[64:128, 0:n], in1=t2[64:128, 0:n], op=ALU.add), [t_t1, t_t2], tks)
                            else:
                                P.op("pool", lambda h, t1=t1, t2=t2, m=m: h.tensor_tensor(out=catT[:, m, c0:c0 + n], in0=t1[:, 0:n], in1=t2[:, 0:n], op=ALU.add), [t_t1, t_t2], cat_T([m], c0, n))
                        nm = len(mlist)
                        for step in range(nm + 2):
                            if step < nm:
                                stage1(mlist[step])
                            if 0 <= step - 1 < nm:
                                stage2(mlist[step - 1])
                            if 0 <= step - 2 < nm:
                                stage3(mlist[step - 2])

                    def emit_tokmajor(gi):
                        c0, n, isctx = GROUPS_A1[gi]
                        hT, t_hT = hTs[gi % 2]
                        need_full = (not last) or (not isctx)
                        for tt in range(n // 128):
                            cc0 = tt * 128
                            blk = kcol_of(c0 + cc0) // 128
                            pb, tpb = next_bank()
                            for k in range(8):
                                P.op("pe", lambda h, k=k, pb=pb, cc0=cc0: h.matmul(pb[:, 0:128], lhsT=hT[:, k, cc0:cc0 + 128], rhs=win[:, k, 896:1024],
                                                                                   start=(k == 0), stop=(k == 7)), [t_win, t_hT], [tpb], inc=(k == 7))
                            P.op("act", lambda h, pb=pb, blk=blk: h.copy(out=Vaug[:, blk, 0, 0:64], in_=pb[:, 0:64]), [tpb], [TV[blk]])
                            P.op("act", lambda h, pb=pb, blk=blk: h.copy(out=Vaug[:, blk, 1, 64:128], in_=pb[:, 64:128]), [tpb], [TV[blk]])
                            if not need_full:
                                continue
                            pb, tpb = next_bank()
                            for k in range(8):
                                P.op("pe", lambda h, k=k, pb=pb, cc0=cc0: h.matmul(pb[:, 0:512], lhsT=hT[:, k, cc0:cc0 + 128], rhs=win[:, k, 1024:1536],
                                                                                   start=(k == 0), stop=(k == 7)), [t_win, t_wab, t_hT], [tpb], inc=(k == 7))
                            if isctx:
                                P.op("dve", lambda h, pb=pb, tt=tt: h.tensor_copy(out=ABc[:, tt, :], in_=pb[:, 0:512]), [tpb], [t_ABc])
                            else:
                                sg, t_sg = stg.next()
                                P.op("dve", lambda h, pb=pb, sg=sg: h.tensor_copy(out=sg[:], in_=pb[:, 0:512]), [tpb], [t_sg])
                                r0 = c0 + cc0
                                P.dma("sp", gin_ab[r0:r0 + 128, :], sg[:], [t_sg], [t_gin_ab], dk_gw)

                    emit_norm(0)
                    for gi in range(len(GROUPS_A1)):
                        if gi + 1 < len(GROUPS_A1):
                            emit_norm(gi + 1)
                        emit_tokmajor(gi)
                        emit_chunks(gi)
                    hv = lambda r0, nr: gin_h[r0:r0 + nr, :].rearrange("r (a j) -> (r a) j", a=4)
                    P.dma("sp", hv(0, 32)[0:64, :], Kz[0][0:64, 128:256], [TK[1]], [t_gin_h], dk_gw)
                    P.dma("sp", hv(0, 32)[64:128, :], Kz[1][64:128, 128:256], [TK[1]], [t_gin_h], dk_gw)
                    P.dma("sp", hv(32, 32)[0:64, :], Kz[0][0:64, 16 * 128:17 * 128], [TK[16]], [t_gin_h], dk_gw)
                    P.dma("sp", hv(32, 32)[64:128, :], Kz[1][64:128, 16 * 128:17 * 128], [TK[16]], [t_gin_h], dk_gw)
                    P.dma("sp", hv(64, 32)[:, 0:64], Vaug[:, 1, 0, 0:64], [TV[1]], [t_gin_h], dk_gw)
                    P.dma("sp", hv(64, 32)[:, 64:128], Vaug[:, 1, 1, 64:128], [TV[1]], [t_gin_h], dk_gw)
                    P.dma("sp", hv(96, 32)[:, 0:64], Vaug[:, 16, 0, 0:64], [TV[16]], [t_gin_h], dk_gw)
                    P.dma("sp", hv(96, 32)[:, 64:128], Vaug[:, 16, 1, 64:128], [TV[16]], [t_gin_h], dk_gw)
                    pv = lambda r0: gin_h[r0:r0 + 4, :].rearrange("r (a j) -> (r a) j", a=32)[:, 0:16].rearrange("p (c j) -> p c j", c=2)
                    P.dma("sp", pv(128), upad[:, :, UPAD:UPAD + 8], [TU[0]], [t_gin_h], dk_gw)
                    P.dma("sp", pv(132), upad[:, :, UPAD + NTOK - 8:UPAD + NTOK], [TU[3]], [t_gin_h], dk_gw)
                    P.collective("AllGather", PAIRS, gin_h.opt(), gout_h.opt(), t_gin_h, t_gout_h, dk_cc)
                    P.collective("AllGather", PAIRS, gin_ab.opt(), gout_ab.opt(), t_gin_ab, t_gout_ab, dk_cc2)
                    P.barrier(exclude=(dk_cc, dk_cc2))
                if stop == "A1":
                    break

                with ExitStack() as st:
                    hvo = lambda r0, nr: gout_h[r0:r0 + nr, :].rearrange("r (a j) -> (r a) j", a=4)
                    P.dma("sp", Kz[0][0:64, 0:128], hvo(32, 32)[0:64, :], [t_gout_h], [TK[0]], dk_gr)
                    P.dma("sp", Kz[1][64:128, 0:128], hvo(32, 32)[64:128, :], [t_gout_h], [TK[0]], dk_gr)
                    P.dma("sp", Kz[0][0:64, 17 * 128:18 * 128], hvo(256, 32)[0:64, :], [t_gout_h], [TK[17]], dk_gr)
                    P.dma("sp", Kz[1][64:128, 17 * 128:18 * 128], hvo(256, 32)[64:128, :], [t_gout_h], [TK[17]], dk_gr)
                    P.dma("sp", Vaug[:, 0, 0, 0:64], hvo(96, 32)[:, 0:64], [t_gout_h], [TV[0]], dk_gr)
                    P.dma("sp", Vaug[:, 0, 1, 64:128], hvo(96, 32)[:, 64:128], [t_gout_h], [TV[0]], dk_gr)
                    P.dma("sp", Vaug[:, 17, 0, 0:64], hvo(256 + 64, 32)[:, 0:64], [t_gout_h], [TV[17]], dk_gr)
                    P.dma("sp", Vaug[:, 17, 1, 64:128], hvo(256 + 64, 32)[:, 64:128], [t_gout_h], [TV[17]], dk_gr)
                    pvo = lambda r0: gout_h[r0:r0 + 4, :].rearrange("r (a j) -> (r a) j", a=32)[:, 0:16].rearrange("p (c j) -> p c j", c=2)
                    P.dma("sp", upad[:, :, UPAD - 8:UPAD], pvo(132), [t_gout_h], [t_uh], dk_gr)
                    P.dma("sp", upad[:, :, UPAD + NTOK:UPAD + NTOK + 8], pvo(256 + 128), [t_gout_h], [t_uh], dk_gr)

                    Eloc = Ring([(sbt(st, "Eloc%d" % i, [128, 3, 4, 128], BF16), T()) for i in range(2)])
                    Ectx = Ring([(sbt(st, "Ectx%d" % i, [128, 2, 4, 128], BF16), T()) for i in range(2)])
                    rdn = Ring([(sbt(st, "rdn%d" % i, [128, 4, 128], F32), T()) for i in range(2)])
                    SL = PSA[:, 0:1536].rearrange("p (j g q) -> p j g q", j=3, g=4); TSL = TP[0:3]
                    SC = PSB[:, 0:1024].rearrange("p (j g q) -> p j g q", j=2, g=4); TSC = TP[4:6]
                    OB = [(PSB[:, 1024:1536].rearrange("p (g q) -> p g q", g=4), TP[6]), (PSB[:, 1536:2048].rearrange("p (g q) -> p g q", g=4), TP[7])]
                    qblocks = list(range(1, 15)) + ([] if last else [16, 17]) + [0, 15]
                    hideA = HIDE_MOD1 and l == layers[0]
                    if hideA:
                        wmA = sbt(st, "wmA", [128, 8, 1024], BF16); t_wmA = T()
                        mod_piece_dma(l, 2, wmA, t_wmA)
                    qpos = 0
                    for i in qblocks:
                        isctx = i >= 16
                        qc0 = i * 128
                        tq = [TC[2 + g][i] for g in range(4)]
                        Es = {}

                        def ph_S(kvh):
                            KZ = Kz[kvh]
                            if not isctx:
                                for jj in range(3):
                                    blk = i + jj
                                    for g in range(4):
                                        P.op("pe", lambda h, jj=jj, g=g, blk=blk, KZ=KZ: h.matmul(SL[:, jj, g, :], lhsT=KZ[:, blk * 128:(blk + 1) * 128], rhs=catT[:, 2 + g, qc0:qc0 + 128],
                                                                                                 start=True, stop=True), [TK[blk]] + tq, TSL, inc=(jj == 2 and g == 3))
                            for jj in range(2):
                                blk = 18 + jj
                                for g in range(4):
                                    P.op("pe", lambda h, jj=jj, g=g, blk=blk, KZ=KZ: h.matmul(SC[:, jj, g, :], lhsT=KZ[:, blk * 128:(blk + 1) * 128], rhs=catT[:, 2 + g, qc0:qc0 + 128],
                                                                                             start=True, stop=True), [TK[blk]] + tq, TSC, inc=(jj == 1 and g == 3))

                        def ph_E(kvh):
                            ec, t_ec = Ectx.next()
                            seqs = []
                            if not isctx:
                                el, t_el = Eloc.next()
                                P.op("act", lambda h, el=el: h.activation(out=el[:], in_=SL, func=AF.Exp, scale=0.125), TSL, [t_el])
                                mp = 2 if i == 0 else 0
                                mn = 3 if i == 15 else 1
                                P.op("dve", lambda h, el=el, mp=mp: h.tensor_tensor(out=el[:, 0], in0=el[:, 0], in1=masks[:, mp, :].unsqueeze(1).to_broadcast([128, 4, 128]), op=ALU.mult), [t_el, t_masks], [t_el])
                                P.op("dve", lambda h, el=el, mn=mn: h.tensor_tensor(out=el[:, 2], in0=el[:, 2], in1=masks[:, mn, :].unsqueeze(1).to_broadcast([128, 4, 128]), op=ALU.mult), [t_el, t_masks], [t_el])
                                seqs += [(el, t_el, jj, i + jj) for jj in range(3)]
                            P.op("act", lambda h, ec=ec: h.activation(out=ec[:], in_=SC, func=AF.Exp, scale=0.125), TSC, [t_ec])
                            seqs += [(ec, t_ec, jj, 18 + jj) for jj in range(2)]
                            Es[kvh] = seqs

                        def ph_PV(kvh):
                            ob, tob = OB[kvh]
                            seqs = Es[kvh]
                            for si, (E, t_E, jj, blk) in enumerate(seqs):
                                P.op("pe", lambda h, E=E, jj=jj, blk=blk, ob=ob, si=si, ns=len(seqs), kvh=kvh: h.matmul(ob, lhsT=Vaug[:, blk, kvh, :], rhs=E[:, jj], start=(si == 0), stop=(si == ns - 1)),
                                     [t_E, TV[blk]], [tob], inc=(si == len(seqs) - 1))

                        def ph_N(kvh):
                            ob, tob = OB[kvh]
                            rd, t_rd = rdn.next()
                            dlo, dhi = (64, 128) if kvh == 0 else (0, 64)
                            olo, ohi = (0, 64) if kvh == 0 else (64, 128)
                            P.op("dve", lambda h, rd=rd, ob=ob, dlo=dlo, dhi=dhi, kvh=kvh: h.tensor_tensor(out=rd[dlo:dhi], in0=ob[dlo:dhi], in1=esink[dlo:dhi, kvh, :].unsqueeze(2).to_broadcast([64, 4, 128]), op=ALU.add),
                                 [tob, t_esink], [t_rd])
                            P.op("act", lambda h, rd=rd, dlo=dlo, dhi=dhi: h.activation(out=rd[dlo:dhi], in_=rd[dlo:dhi], func=AF.Ln), [t_rd], [t_rd])
                            P.op("act", lambda h, rd=rd, dlo=dlo, dhi=dhi: h.activation(out=rd[dlo:dhi], in_=rd[dlo:dhi], func=AF.Exp, scale=-1.0), [t_rd], [t_rd])
                            P.op("dve", lambda h, rd=rd, ob=ob, olo=olo, ohi=ohi, dlo=dlo, dhi=dhi: h.tensor_tensor(out=catT[olo:ohi, 2:6, qc0:qc0 + 128], in0=ob[olo:ohi], in1=rd[dlo:dhi], op=ALU.mult),
                                 [tob, t_rd], tq)
                        ph_S(0); ph_E(0); ph_S(1); ph_E(1); ph_PV(0); ph_N(0); ph_PV(1); ph_N(1)
                        qpos += 1
                        if hideA and qpos in (3, 6, 9, 12):
                            v_ = 2 + (qpos // 3 - 1)
                            mod_piece_mm(l, v_, wmA, t_wmA, bank(3), TP[3])
                            if v_ < 5:
                                mod_piece_dma(l, v_ + 1, wmA, t_wmA)
                            else:
                                mod_finalize_part(l, bank(3), TP[3], 2, 6)
                    P.barrier()
                if stop == "A2":
                    break
                mxb.close()
                wout = sbt(mxa, "wout", [128, 8, D], BF16); t_wout = T()
                P.dma("pool", wout[:], I["w_outp"][l].rearrange("(k p) c -> p k c", p=128), [], [t_wout], dk_w[3])

                with ExitStack() as st:
                    AB = sbt(st, "AB", [128, 32, 512], BF16); t_AB = [T() for _ in range(4)]
                    tabs = [(sbt(st, "tab%d" % i, [128, 4, 2, 512], BF16), T(), dk_tab[i]) for i in range(2)]
                    d256 = sbt(st, "d256", [128, 2, 2, 256], BF16); t_d256 = T()
                    P.dma("sp", d256[:], I["dft256"], [], [t_d256], dk_misc)
                    halov = sbt(st, "halov", [128, 2], F32); t_halov = T()
                    pfix = sbt(st, "pfix", [128, 2, 2, 8], F32)
                    pfixc = sbt(st, "pfixc", [128, 2, 2, 8], F32); t_pfix = T()
                    pwbd = sbt(st, "pwbd", [128, 2, 128], BF16); t_pwbd = T()
                    psc = sbt(st, "psc", [128, 2], F32); t_psc = T()
                    P.dma("sp", halov[:], I["halov"], [], [t_halov], dk_misc)
                    P.dma("sp", pfix[:], I["poolfix"], [], [t_pfix], dk_misc)
                    P.dma("sp", pfixc[:], I["poolfixc"], [], [t_pfix], dk_misc)
                    P.dma("pool", pwbd[:], I["poolw_bd"][l].rearrange("c p m -> p c m"), [], [t_pwbd], dk_misc)
                    P.dma("sp", psc[:], I["pscale"][l], [], [t_psc], dk_misc)
                    for q4 in range(4):
                        P.dma("sp", AB[:, q4 * 8:(q4 + 1) * 8, :], gout_ab[q4 * 1024:(q4 + 1) * 1024, :].rearrange("(n p) c -> p n c", p=128),
                              [t_gout_ab], [t_AB[q4]], dk_gr)
                    for hq in range(2):
                        lo = AB[:, hq * 8:(hq + 1) * 8, :]
                        hi = AB[:, 16 + hq * 8:16 + (hq + 1) * 8, :]
                        P.op("dve", lambda h, lo=lo, hi=hi: h.tensor_tensor(out=lo, in0=lo, in1=hi, op=ALU.add), [t_AB[hq], t_AB[2 + hq]], [t_AB[hq]])
                        P.op("dve", lambda h, lo=lo, hi=hi: h.scalar_tensor_tensor(out=hi, in0=hi, scalar=-2.0, in1=lo, op0=ALU.mult, op1=ALU.add), [t_AB[hq], t_AB[2 + hq]], [t_AB[2 + hq]])
                    pt = [(sbt(st, "pt%d" % i, [128, 512 + 32], F32), T()) for i in range(3)]
                    deferred = []
                    SEG = ([] if last else [(0, LC, 1)]) + [(512, 512, 0), (1024, 512, 0), (0, 512, 0), (1536, 512, 0)]
                    for si_, (c0, n, isctx) in enumerate(SEG):
                        if (not isctx) and c0 == 0:
                            P.op("dve", lambda h: h.tensor_scalar(out=upad[:, :, UPAD - 8:UPAD], in0=upad[:, :, UPAD - 8:UPAD], scalar1=halov[:, 0:1], scalar2=None, op0=ALU.mult),
                                 [t_uh, t_halov], [t_uh])
                            P.op("dve", lambda h: h.tensor_scalar(out=upad[:, :, UPAD + NTOK:UPAD + NTOK + 8], in0=upad[:, :, UPAD + NTOK:UPAD + NTOK + 8], scalar1=halov[:, 1:2], scalar2=None, op0=ALU.mult),
                                 [t_uh, t_halov], [t_uh])
                        U = upadc if isctx else upad
                        tus = [t_uc] if isctx else ([TU[c0 // 512]] + ([t_uh] if (c0 == 0 or c0 + 512 == NTOK) else []) + ([TU[c0 // 512 - 1]] if c0 > 0 else []) + ([TU[c0 // 512 + 1]] if c0 + 512 < NTOK else []))
                        fx = pfixc if isctx else pfix
                        seq_n = LC if isctx else NTOK
                        for ch in range(2):
                            (a, t_a), (b, t_b), (cbuf, t_c) = pt
                            base = UPAD + c0 - 16
                            W = n + 32
                            P.op("dve", lambda h, a=a, U=U, ch=ch, base=base, W=W: h.tensor_tensor(out=a[:, 1:W], in0=U[:, ch, base + 1:base + W], in1=U[:, ch, base:base + W - 1], op=ALU.add), tus, [t_a])
                            if ch == 0:
                                P.op("dve", lambda h, a=a, cbuf=cbuf: h.tensor_copy(out=cbuf[0:64, 0:n], in_=a[0:64, 16:16 + n]), [t_a], [t_c])
                                P.op("dve", lambda h, a=a, cbuf=cbuf: h.tensor_tensor(out=cbuf[64:128, 0:n], in0=a[64:128, 17:17 + n], in1=a[64:128, 15:15 + n], op=ALU.add), [t_a], [t_c])
                            else:
                                P.op("dve", lambda h, a=a, b=b, W=W: h.tensor_tensor(out=b[:, 3:W], in0=a[:, 3:W], in1=a[:, 1:W - 2], op=ALU.add), [t_a], [t_b])
                                P.op("dve", lambda h, a=a, b=b, W=W: h.tensor_tensor(out=a[:, 7:W], in0=b[:, 7:W], in1=b[:, 3:W - 4], op=ALU.add), [t_b, t_a], [t_a])
                                P.op("dve", lambda h, a=a, cbuf=cbuf: h.tensor_copy(out=cbuf[0:64, 0:n], in_=a[0:64, 19:19 + n]), [t_a], [t_c])
                                P.op("dve", lambda h, a=a, cbuf=cbuf: h.tensor_tensor(out=cbuf[64:128, 0:n], in0=a[64:128, 23:23 + n], in1=a[64:128, 15:15 + n], op=ALU.add), [t_a], [t_c])
                            if c0 == 0:
                                P.op("dve", lambda h, cbuf=cbuf, fx=fx, ch=ch: h.tensor_tensor(out=cbuf[:, 0:8], in0=cbuf[:, 0:8], in1=fx[:, ch, 0, :], op=ALU.mult), [t_c, t_pfix], [t_c])
                            if c0 + n == seq_n:
                                P.op("dve", lambda h, cbuf=cbuf, fx=fx, ch=ch: h.tensor_tensor(out=cbuf[:, n - 8:n], in0=cbuf[:, n - 8:n], in1=fx[:, ch, 1, :], op=ALU.mult), [t_c, t_pfix], [t_c])
                            wl, wh = (2, 4) if ch == 0 else (8, 16)
                            oc0 = NTOK if isctx else c0
                            tyc = cat_T([ch], oc0, n)
                            P.op("dve", lambda h, cbuf=cbuf, wl=wl: h.tensor_scalar(out=cbuf[0:64, 0:n], in0=cbuf[0:64, 0:n], scalar1=1.0 / wl, scalar2=None, op0=ALU.mult), [t_c], [t_c])
                            P.op("dve", lambda h, cbuf=cbuf, wh=wh: h.tensor_scalar(out=cbuf[64:128, 0:n], in0=cbuf[64:128, 0:n], scalar1=1.0 / wh, scalar2=None, op0=ALU.mult), [t_c], [t_c])
                            P.op("dve", lambda h, cbuf=cbuf, U=U, ch=ch, oc0=oc0: h.tensor_tensor(out=catT[:, ch, oc0:oc0 + n], in0=cbuf[:, 0:n], in1=U[:, ch, UPAD + c0:UPAD + c0 + n], op=ALU.subtract),
                                 [t_c] + tus, tyc)
                            deferred.append((ch, oc0, n))

                    ti = 0
                    for pi in range(2):
                        for jt in range(2):
                            accs = [next_bank(), next_bank()]
                            for ng_ in range(4):
                                tab, t_tab, dkt = tabs[ti % 2]
                                ti += 1
                                P.dma("sp", tab[:], I["dfttab"][pi, jt, ng_], [], [t_tab], dkt)
                                for nn in range(4):
                                    nch = ng_ * 4 + nn + 16 * pi
                                    for cs in range(2):
                                        for m in range(2):
                                            first = (ng_ == 0 and nn == 0 and cs == 0)
                                            lastmm = (ng_ == 3 and nn == 3 and cs == 1)
                                            P.op("pe", lambda h, m=m, nch=nch, cs=cs, tab=tab, nn=nn, first=first, lastmm=lastmm, acc=accs[m][0]:
                                                 h.matmul(acc, lhsT=AB[:, nch, cs * 256 + m * 128:cs * 256 + (m + 1) * 128], rhs=tab[:, nn, cs, :], start=first, stop=lastmm),
                                                 [t_AB[nch // 8], t_tab], [accs[m][1]], inc=(lastmm or (nn == 3 and cs == 1 and m == 1)))
                            for m in range(2):
                                P.op("act", lambda h, m=m, acc=accs[m][0], jt=jt, pi=pi: h.copy(out=catT[:, 6 + m, jt * 1024:(jt + 1) * 1024].rearrange("p (j two) -> p j two", two=2)[:, :, pi], in_=acc),
                                     [accs[m][1]], cat_T([6 + m], jt * 1024, 1024))
                    if not last:
                        accs = [next_bank(), next_bank()]
                        for nn in range(2):
                            for cs in range(2):
                                for m in range(2):
                                    first = (nn == 0 and cs == 0)
                                    lastmm = (nn == 1 and cs == 1)
                                    P.op("pe", lambda h, m=m, nn=nn, cs=cs, first=first, lastmm=lastmm, acc=accs[m][0]:
                                         h.matmul(acc[:, 0:256], lhsT=ABc[:, nn, cs * 256 + m * 128:cs * 256 + (m + 1) * 128], rhs=d256[:, nn, cs, :], start=first, stop=lastmm),
                                         [t_ABc, t_d256], [accs[m][1]], inc=lastmm)
                        for m in range(2):
                            P.op("act", lambda h, m=m, acc=accs[m][0]: h.copy(out=catT[:, 6 + m, NTOK:TOK], in_=acc[:, 0:256]), [accs[m][1]], cat_T([6 + m], NTOK, LC))
                    for (ch, oc0, n) in deferred:
                        pb, tpb = next_bank()
                        P.op("pe", lambda h, pb=pb, ch=ch, oc0=oc0, n=n: h.matmul(pb[:, 0:n], lhsT=pwbd[:, ch, :], rhs=catT[:, ch, oc0:oc0 + n], start=True, stop=True), cat_T([ch], oc0, n) + [t_pwbd], [tpb])
                        P.op("act", lambda h, pb=pb, ch=ch, oc0=oc0, n=n: h.activation(out=catT[:, ch, oc0:oc0 + n], in_=pb[:, 0:n], func=AF.Copy, scale=psc[:, ch:ch + 1]),
                             [tpb, t_psc], cat_T([ch], oc0, n))
                    P.barrier()
                if "catT" in DUMP and l == layers[-1] and stop in ("A3", "A2"):
                    pass
                if stop == "A3":
                    break

                rstds2 = Ring([(sbt(mxa, "rstd2_%d" % i, [128, 512], F32), T()) for i in range(2)])
                tmps2 = Ring([(sbt(mxa, "n2tmp%d" % i, [128, 512], F32), T()) for i in range(3)])
                G4 = [g for g in GROUPS if not (g[2] and last)]

                def emit_A4(c0, n, isctx):
                    for m in range(8):
                        pb, tpb = next_bank()
                        for k in range(8):
                            P.op("pe", lambda h, k=k, m=m, pb=pb: h.matmul(pb[:, 0:n], lhsT=wout[:, k, m * 128:(m + 1) * 128], rhs=catT[:, k, c0:c0 + n],
                                                                           start=(k == 0), stop=(k == 7)), [t_wout] + cat_T([k], c0, n), [tpb], inc=(k == 7))
                        P.op("dve", lambda h, m=m, pb=pb: h.scalar_tensor_tensor(out=x_ap(m, c0, n), in0=pb[:, 0:n], scalar=modv(l, 2, m, isctx), in1=x_ap(m, c0, n),
                                                                                 op0=ALU.mult, op1=ALU.add), [tpb, t_modl[l], x_T(m, c0)], [x_T(m, c0)])

                def emit_N2(c0, n, isctx):
                    norm_mod((rstds2, tmps2), l, 1, c0, n, isctx, lambda k, c0=c0, n=n: catT[:, k, c0:c0 + n], lambda k, c0=c0, n=n: cat_T([k], c0, n))
                for gi_, g_ in enumerate(G4):
                    emit_A4(*g_)
                    if gi_ >= 1:
                        emit_N2(*G4[gi_ - 1])
                emit_N2(*G4[-1])
                P.barrier()
            if stop in ("A1", "A2", "A3", "A4"):
                break

            MG = [g for g in GROUPS if not (g[2] and last)]
            ntile = sum(g[1] for g in MG) // 128
            h2T = catT
            with ExitStack() as st:
                gatesT = sbt(st, "gatesT", [16, TOK], BF16); t_gT = T()
                sel = sbt(st, "sel", [16, 16, 128], BF16); t_sel = T()
                P.dma("sp", sel[:], I["sel"], [], [t_sel], dk_misc)
                wg = [sbt(st, "wg%d" % i, [128, 8, 512], BF16) for i in range(2)]
                wu = [sbt(st, "wu%d" % i, [128, 8, 512], BF16) for i in range(2)]
                wd = [sbt(st, "wd%d" % i, [128, 4, D], BF16) for i in range(2)]
                t_wg, t_wu, t_wd = [T(), T()], [T(), T()], [T(), T()]
                def load_expert(e):
                    s = e % 2
                    P.dma("pool", wg[s][:], I["w_gate"][l, e].rearrange("(k p) c -> p k c", p=128), [], [t_wg[s]], dk_ex[s][0])
                    P.dma("pool", wu[s][:], I["w_up"][l, e].rearrange("(k p) c -> p k c", p=128), [], [t_wu[s]], dk_ex[s][1])
                    P.dma("pool", wd[s][:], I["w_down"][l, e].rearrange("(k p) c -> p k c", p=128), [], [t_wd[s]], dk_ex[s][2])
                load_expert(0)
                with ExitStack() as s2:
                    wr = sbt(s2, "wr", [128, 8, 20], BF16); t_wr = T()
                    brt = sbt(s2, "brt", [128, 20], F32); t_br = T()
                    P.dma("pool", wr[:], I["wr"][l].rearrange("(k p) c -> p k c", p=128), [], [t_wr], dk_misc)
                    P.dma("sp", brt[:], I["br"][l], [], [t_br], dk_misc)
                    pbr, tpbr = next_bank()
                    for tt in range(ntile):
                        for k in range(8):
                            P.op("pe", lambda h, tt=tt, k=k: h.matmul(pbr[:, tt * 20:(tt + 1) * 20], lhsT=h2T[:, k, tt * 128:(tt + 1) * 128], rhs=wr[:, k, :],
                                                                      start=(k == 0), stop=(k == 7)), [t_wr] + cat_T([k], tt * 128, 128), [tpbr], inc=(k == 7))
                    NT = ntile
                    rt = lambda nm, w: (sbt(s2, nm, [128, NT, w], F32), T())
                    Lg, t_L = rt("Lg", 20)
                    P.op("dve", lambda h: h.tensor_tensor(out=Lg[:], in0=pbr[:, 0:NT * 20].rearrange("p (t c) -> p t c", c=20), in1=brt[:].unsqueeze(1).to_broadcast([128, NT, 20]), op=ALU.add),
                         [tpbr, t_br], [t_L])
                    mg, t_mg = rt("mg", 1)
                    P.op("dve", lambda h: h.tensor_reduce(out=mg[:, :, 0], in_=Lg[:, :, 0:4], axis=AX.X, op=ALU.max), [t_L], [t_mg])
                    eg, t_eg = rt("eg", 4)
                    P.op("dve", lambda h: h.tensor_tensor(out=eg[:], in0=Lg[:, :, 0:4], in1=mg[:].to_broadcast([128, NT, 4]), op=ALU.subtract), [t_L, t_mg], [t_eg])
                    oh, t_oh = rt("oh", 4)
                    P.op("dve", lambda h: h.tensor_single_scalar(out=oh[:], in_=eg[:], scalar=0.0, op=ALU.is_ge), [t_eg], [t_oh])
                    P.op("act", lambda h: h.activation(out=eg[:], in_=eg[:], func=AF.Exp), [t_eg], [t_eg])
                    pg, t_pg = rt("pg", 1)
                    P.op("dve", lambda h: h.tensor_reduce(out=pg[:, :, 0], in_=eg[:], axis=AX.X, op=ALU.add), [t_eg], [t_pg])
                    P.op("dve", lambda h: h.reciprocal(out=pg[:], in_=pg[:]), [t_pg], [t_pg])
                    P.op("dve", lambda h: h.tensor_scalar(out=oh[:], in0=oh[:], scalar1=BIG, scalar2=-BIG, op0=ALU.mult, op1=ALU.add), [t_oh], [t_oh])
                    lm, t_lm = rt("lm", 16)
                    P.op("dve", lambda h: h.tensor_tensor(out=lm[:].rearrange("p t (g e) -> p t g e", g=4), in0=Lg[:, :, 4:20].rearrange("p t (g e) -> p t g e", g=4),
                                                          in1=oh[:].unsqueeze(3).to_broadcast([128, NT, 4, 4]), op=ALU.add), [t_L, t_oh], [t_lm])
                    m1_, t_m1_ = rt("m1_", 1)
                    P.op("dve", lambda h: h.tensor_reduce(out=m1_[:, :, 0], in_=lm[:], axis=AX.X, op=ALU.max), [t_lm], [t_m1_])
                    is1, t_is1 = rt("is1", 16)
                    P.op("dve", lambda h: h.tensor_tensor(out=is1[:], in0=lm[:], in1=m1_[:].to_broadcast([128, NT, 16]), op=ALU.is_ge), [t_lm, t_m1_], [t_is1])
                    lm2, t_lm2 = rt("lm2", 16)
                    P.op("dve", lambda h: h.scalar_tensor_tensor(out=lm2[:], in0=is1[:], scalar=-BIG, in1=lm[:], op0=ALU.mult, op1=ALU.add), [t_is1, t_lm], [t_lm2])
                    m2_, t_m2_ = rt("m2_", 1)
                    P.op("dve", lambda h: h.tensor_reduce(out=m2_[:, :, 0], in_=lm2[:], axis=AX.X, op=ALU.max), [t_lm2], [t_m2_])
                    selm, t_selm = rt("selm", 16)
                    P.op("dve", lambda h: h.tensor_tensor(out=selm[:], in0=lm[:], in1=m2_[:].to_broadcast([128, NT, 16]), op=ALU.is_ge), [t_lm, t_m2_], [t_selm])
                    P.op("dve", lambda h: h.tensor_tensor(out=lm2[:], in0=lm[:], in1=m1_[:].to_broadcast([128, NT, 16]), op=ALU.subtract), [t_lm, t_m1_, t_lm2], [t_lm2])
                    P.op("dve", lambda h: h.tensor_scalar(out=lm2[:], in0=lm2[:], scalar1=-80.0, scalar2=None, op0=ALU.max), [t_lm2], [t_lm2])
                    P.op("act", lambda h: h.activation(out=lm2[:], in_=lm2[:], func=AF.Exp), [t_lm2], [t_lm2])
                    P.op("dve", lambda h: h.tensor_tensor(out=lm2[:], in0=lm2[:], in1=selm[:], op=ALU.mult), [t_lm2, t_selm], [t_lm2])
                    den, t_den = rt("den", 1)
                    P.op("dve", lambda h: h.tensor_reduce(out=den[:, :, 0], in_=lm2[:], axis=AX.X, op=ALU.add), [t_lm2], [t_den])
                    P.op("dve", lambda h: h.reciprocal(out=den[:], in_=den[:]), [t_den], [t_den])
                    P.op("dve", lambda h: h.tensor_tensor(out=den[:], in0=den[:], in1=pg[:], op=ALU.mult), [t_den, t_pg], [t_den])
                    P.op("dve", lambda h: h.tensor_tensor(out=lm2[:], in0=lm2[:], in1=den[:].to_broadcast([128, NT, 16]), op=ALU.mult), [t_lm2, t_den], [t_lm2])
                    for tt in range(ntile):
                        pb, tpb = next_bank()
                        P.op("pe", lambda h, tt=tt, pb=pb: h.transpose(pb[0:16, 0:128], lm2[:, tt, :], ident32[:]), [t_lm2, t_const], [tpb])
                        P.op("act", lambda h, tt=tt, pb=pb: h.copy(out=gatesT[:, tt * 128:(tt + 1) * 128], in_=pb[0:16, 0:128]), [tpb], [t_gT])
                    P.barrier()
                if "gates" in DUMP:
                    P.dma("pool", DUMP["gates"], gatesT[:], [t_gT], [], dk_out)

                load_expert(1)
                aT = Ring([(sbt(st, "aT%d" % i, [128, 4, 512], BF16), T()) for i in range(2)])
                sgr = Ring([(sbt(st, "sg%d" % i, [128, 512], BF16), T()) for i in range(3)])
                sg2r = Ring([(sbt(st, "sg2_%d" % i, [128, 512], BF16), T()) for i in range(3)])
                gbr = Ring([(sbt(st, "gb%d" % i, [128, 512], BF16), T()) for i in range(2)])

                hide = HIDE_MOD1 and l == layers[0]
                if hide:
                    wm1 = sbt(st, "wm1", [128, 8, 1024], BF16); t_wm1 = T()
                    reserved.add(7)
                    mod_piece_dma(layers[1], 0, wm1, t_wm1)
                for e in range(16):
                    s = e % 2
                    if hide and e < 6:
                        mod_piece_mm(layers[1], e, wm1, t_wm1, bank(7), TP[7])
                        if e + 1 < 6:
                            mod_piece_dma(layers[1], e + 1, wm1, t_wm1)
                        else:
                            mod_finalize(layers[1], bank(7), TP[7])
                            reserved.discard(7)
                    astate = {}

                    def emit_GU(gi):
                        c0, n, isctx = MG[gi]
                        pbg, tpbg = next_bank()
                        P.op("pe", lambda h, e=e, pbg=pbg: h.matmul(pbg[:, 0:n], lhsT=sel[:, e, :], rhs=gatesT[:, c0:c0 + n], start=True, stop=True), [t_gT, t_sel], [tpbg])
                        gb, t_gb = gbr.next()
                        P.op("act", lambda h, gb=gb, pbg=pbg: h.copy(out=gb[:, 0:n], in_=pbg[:, 0:n]), [tpbg], [t_gb])
                        a, t_a = aT.next()
                        for dc in range(4):
                            pg_, tpg_ = next_bank()
                            for k in range(8):
                                P.op("pe", lambda h, k=k, dc=dc, pg_=pg_, s=s: h.matmul(pg_[:, 0:n], lhsT=wg[s][:, k, dc * 128:(dc + 1) * 128], rhs=h2T[:, k, c0:c0 + n],
                                                                                        start=(k == 0), stop=(k == 7)), [t_wg[s]] + cat_T([k], c0, n), [tpg_], inc=(k == 7))
                            pu_, tpu_ = next_bank()
                            for k in range(8):
                                P.op("pe", lambda h, k=k, dc=dc, pu_=pu_, s=s: h.matmul(pu_[:, 0:n], lhsT=wu[s][:, k, dc * 128:(dc + 1) * 128], rhs=h2T[:, k, c0:c0 + n],
                                                                                        start=(k == 0), stop=(k == 7)), [t_wu[s]] + cat_T([k], c0, n), [tpu_], inc=(k == 7))
                            sg, t_sg = sgr.next()
                            P.op("act", lambda h, sg=sg, pg_=pg_: h.activation(out=sg[:, 0:n], in_=pg_[:, 0:n], func=AF.Silu), [tpg_], [t_sg])
                            sg2, t_sg2 = sg2r.next()
                            P.op("pool", lambda h, sg=sg, sg2=sg2, gb=gb: h.tensor_tensor(out=sg2[:, 0:n], in0=sg[:, 0:n], in1=gb[:, 0:n], op=ALU.mult), [t_sg, t_gb], [t_sg2])
                            P.op("dve", lambda h, a=a, dc=dc, pu_=pu_, sg2=sg2: h.tensor_tensor(out=a[:, dc, 0:n], in0=pu_[:, 0:n], in1=sg2[:, 0:n], op=ALU.mult), [tpu_, t_sg2], [t_a])
                        astate[gi] = (a, t_a)

                    def emit_DOWN(gi):
                        c0, n, isctx = MG[gi]
                        a, t_a = astate.pop(gi)
                        for m in range(8):
                            py, tpy = next_bank()
                            for dc in range(4):
                                P.op("pe", lambda h, dc=dc, m=m, py=py, a=a, s=s: h.matmul(py[:, 0:n], lhsT=wd[s][:, dc, m * 128:(m + 1) * 128], rhs=a[:, dc, 0:n],
                                                                                           start=(dc == 0), stop=(dc == 3)), [t_wd[s], t_a], [tpy], inc=(dc == 3))
                            P.op("dve", lambda h, m=m, py=py: h.scalar_tensor_tensor(out=x_ap(m, c0, n), in0=py[:, 0:n], scalar=modv(l, 5, m, isctx), in1=x_ap(m, c0, n),
                                                                                     op0=ALU.mult, op1=ALU.add), [tpy, t_modl[l], x_T(m, c0)], [x_T(m, c0)])
                    emit_GU(0)
                    for gi in range(len(MG)):
                        if gi + 1 < len(MG):
                            emit_GU(gi + 1)
                        emit_DOWN(gi)
                    if e + 2 < 16:
                        load_expert(e + 2)
                P.barrier()
            if stop == "B%d" % l:
                break

        if "xT" in DUMP:
            for m in range(8):
                P.dma("sp", DUMP["xT"][m * 128:(m + 1) * 128, :], xT[:, m, :], TX[m], [], dk_out)
        if "xcT" in DUMP:
            for m in range(8):
                P.dma("sp", DUMP["xcT"][m * 128:(m + 1) * 128, :], xcT[:, m, :], [TXC[m]], [], dk_out)
        if "catT" in DUMP:
            for k in range(8):
                P.dma("pool", DUMP["catT"][k * 128:(k + 1) * 128, :], catT[:, k, :], TC[k], [], dk_out)
        for m in range(8):
            P.dma("sp", outT[m * 128:(m + 1) * 128, :], xT[:, m, :], TX[m], [], dk_out)
        P.barrier()
        P.emit()
    return nc


_CACHE = {}


def kernel(**inputs):
    in_maps = host_prep(**inputs)
    if "nc" not in _CACHE:
        _CACHE["nc"] = build()
    nc = _CACHE["nc"]
    res = run_bass_kernel_spmd(nc, in_maps, core_ids=list(range(8)))
    out = np.empty((4, 4096, D), np.float32)
    for core in range(8):
        b, par = core // 2, core % 2
        out[b, par * NTOK:(par + 1) * NTOK, :] = res.results[core]["outT"].T
    return out
```

```python
import types
import numpy as np
import ml_dtypes
from contextlib import ExitStack
import concourse.bass as bass
import concourse.mybir as mybir
from concourse.bass_utils import run_bass_kernel_spmd

F32 = mybir.dt.float32
BF16 = mybir.dt.bfloat16
AF = mybir.ActivationFunctionType
ALU = mybir.AluOpType
AX = mybir.AxisListType

D = 1024
NTOK = 2048
LC = 256
TOK = NTOK + LC
NB = 20
UPAD = 16
EPS = 1e-6
BIG = 30000.0
SAME_SYNC = True


class T:
    __slots__ = ("name", "w", "r")

    def __init__(self, name=""):
        self.name = name
        self.w = None
        self.r = []


def _freeze(fn):
    if fn.__closure__ is None:
        return fn
    cells = []
    for c in fn.__closure__:
        try:
            cells.append(types.CellType(c.cell_contents))
        except ValueError:
            cells.append(c)
    return types.FunctionType(fn.__code__, fn.__globals__, fn.__name__, fn.__defaults__, tuple(cells))


class Prog:
    ENG = ("pe", "act", "dve", "pool", "sp")

    def __init__(self, nc, stack, same_engine_sync=True):
        self.nc = nc
        self.stack = stack
        self.same = same_engine_sync
        self.sems = {}
        self.cnt = {}
        for e in self.ENG:
            self.sems[e] = stack.enter_context(nc.semaphore("s_" + e))
            self.cnt[e] = 0
        self.ops = {e: [] for e in self.ENG}
        self.waited = {e: {} for e in self.ENG}
        self.pending_silent = {e: False for e in self.ENG}
        self.ndsem = 0
        self.dkeys = []

    def dsem(self):
        k = "d%d" % self.ndsem
        self.ndsem += 1
        self.sems[k] = self.stack.enter_context(self.nc.semaphore("sd_" + k))
        self.cnt[k] = 0
        self.dkeys.append(k)
        return k

    def _need(self, eng, dep):
        if dep is None:
            return
        k, v = dep
        if k == eng:
            if not self.same or v > self.cnt[eng]:
                return
        if self.waited[eng].get(k, 0) >= v:
            return
        self.waited[eng][k] = v
        sem = self.sems[k]
        self.ops[eng].append(lambda h, sem=sem, v=v: h.wait_ge(sem, v))

    def _deps(self, eng, reads, writes):
        for t in reads:
            self._need(eng, t.w)
        for t in writes:
            self._need(eng, t.w)
            for d in t.r:
                self._need(eng, d)

    def _mark(self, tok, reads, writes):
        for t in reads:
            t.r.append(tok)
        for t in writes:
            t.w = tok
            t.r = []

    def op(self, eng, fn, reads=(), writes=(), inc=True):
        fn = _freeze(fn)
        self._deps(eng, reads, writes)
        seq = self.cnt[eng] + 1
        if inc:
            self.cnt[eng] = seq
            sem = self.sems[eng]
            self.ops[eng].append(lambda h, fn=fn, sem=sem: fn(h).then_inc(sem, 1))
            self.pending_silent[eng] = False
        else:
            self.ops[eng].append(lambda h, fn=fn: fn(h))
            self.pending_silent[eng] = True
        self._mark((eng, seq), reads, writes)

    def dma(self, q, out, in_, reads, writes, dk=None):
        if not hasattr(self, "dpool"):
            self.dpool = [self.dsem() for _ in range(40)]
            self.dnext = 0
        dk = self.dpool[self.dnext % len(self.dpool)]
        self.dnext += 1
        if self.cnt[dk] > 0:
            self._need(q, (dk, self.cnt[dk]))
        self._deps(q, reads, writes)
        self.cnt[dk] += 16
        sem = self.sems[dk]
        self.ops[q].append(lambda h, out=out, in_=in_, sem=sem: h.dma_start(out=out, in_=in_).then_inc(sem, 16))
        self._mark((dk, self.cnt[dk]), reads, writes)

    def collective(self, kind, groups, in_ap, out_ap, tin, tout, dk):
        q = "pool"
        if self.cnt[dk] > 0:
            self._need(q, (dk, self.cnt[dk]))
        self._deps(q, [tin], [tout])
        self.cnt[dk] += 1
        sem = self.sems[dk]
        self.ops[q].append(lambda h: h.collective_compute(kind, ALU.bypass, replica_groups=groups,
                                                          ins=[in_ap], outs=[out_ap]).then_inc(sem, 1))
        self._mark((dk, self.cnt[dk]), [tin], [tout])

    def barrier(self, exclude=()):
        for e in self.ENG:
            assert not self.pending_silent[e]
        keys = [k for k in list(self.ENG) + self.dkeys if k not in exclude]
        for e in self.ENG:
            for k in keys:
                if k != e and self.cnt[k] > 0:
                    self._need(e, (k, self.cnt[k]))

    def emit(self):
        for e in self.ENG:
            assert not self.pending_silent[e], "engine %s ends with silent op" % e
        nc = self.nc
        with nc.Block() as block:
            for e, deco in (("pe", block.tensor), ("act", block.scalar), ("dve", block.vector),
                            ("pool", block.gpsimd), ("sp", block.sync)):
                ops = self.ops[e]

                def body(h, ops=ops):
                    for f in ops:
                        f(h)
                deco(body)


class Ring:
    def __init__(self, items):
        self.items = items
        self.i = 0

    def next(self):
        it = self.items[self.i % len(self.items)]
        self.i += 1
        return it


def _bf(a):
    return np.ascontiguousarray(a.astype(ml_dtypes.bfloat16))


def _consts(par):
    c = {}
    pos = np.arange(NTOK) + NTOK * par
    r = (pos // 64).astype(np.float32)
    col = (pos % 64).astype(np.float32)
    half = 32
    inv = (1.0 / (np.float32(10000.0) ** (np.arange(0, half, 2, dtype=np.float32) / np.float32(half)))).astype(np.float32)
    ar = r[:, None] * inv
    ac = col[:, None] * inv
    ang = np.concatenate([ar, ar, ac, ac], axis=-1).astype(np.float32)
    cos = np.cos(ang).astype(np.float32).T
    sin = np.sin(ang).astype(np.float32).T
    cosT = np.ones((128, TOK), np.float32)
    sinT = np.zeros((128, TOK), np.float32)
    cosT[0:64, :NTOK] = cos; cosT[64:128, :NTOK] = cos
    sinT[0:64, :NTOK] = sin; sinT[64:128, :NTOK] = sin
    c["ropeC"] = _bf(cosT); c["ropeS"] = _bf(sinT)
    R = np.zeros((64, 64), np.float32)
    for i in range(16):
        R[i, i + 16] = -1.0
        R[i + 16, i] = 1.0
        R[i + 32, i + 48] = -1.0
        R[i + 48, i + 32] = 1.0
    R2 = np.zeros((128, 128), np.float32)
    R2[0:64, 0:64] = R; R2[64:, 64:] = R
    c["rotm"] = _bf(R2.T)
    hs = np.zeros((128, 128), np.float32)
    hs[0:64, 0:64] = 1.0 / 64; hs[64:, 64:] = 1.0 / 64
    c["hsum"] = _bf(hs)
    c["ones128"] = _bf(np.ones((128, 128), np.float32))
    c["ident32"] = np.eye(128, dtype=np.float32)
    kk = np.arange(128)[:, None]; qq = np.arange(128)[None, :]
    mprev = (kk >= qq).astype(np.float32)
    mnext = (kk <= qq).astype(np.float32)
    masks = np.zeros((128, 4, 128), np.float32)
    masks[:, 0] = mprev; masks[:, 1] = mnext
    masks[:, 2] = mprev if par == 1 else 0.0
    masks[:, 3] = mnext if par == 0 else 0.0
    c["masks"] = _bf(masks)
    wins = {(0, 0): 2, (0, 1): 4, (1, 0): 8, (1, 1): 16}
    fix = np.ones((128, 2, 2, 8), np.float32)
    Nn = 4096
    for (ch, hf), w in wins.items():
        rows = slice(hf * 64, hf * 64 + 64)
        for j in range(8):
            t = j
            lo = max(t - w // 2, 0); hi = min(t + w // 2 - 1, Nn - 1)
            fix[rows, ch, 0, j] = w / (hi - lo + 1)
            t = Nn - 8 + j
            lo = max(t - w // 2, 0); hi = min(t + w // 2 - 1, Nn - 1)
            fix[rows, ch, 1, j] = w / (hi - lo + 1)
    c["poolfixc"] = fix.copy()
    f2 = fix.copy()
    if par == 0:
        f2[:, :, 1, :] = 1.0
    else:
        f2[:, :, 0, :] = 1.0
    c["poolfix"] = f2
    hv = np.zeros((128, 2), np.float32)
    hv[:, 0] = 1.0 if par == 1 else 0.0
    hv[:, 1] = 1.0 if par == 0 else 0.0
    c["halov"] = hv
    n = np.arange(2048, dtype=np.int64)[:, None]
    k = (np.arange(NTOK, dtype=np.int64) + NTOK * par)[None, :]
    ph = ((n * k) % 4096).astype(np.float64) * (2.0 * np.pi / 4096.0)
    tab = np.stack([np.cos(ph), -np.sin(ph)], axis=1)
    tab = tab.reshape(4, 4, 128, 2, 2, 512, 2)
    tab = tab.transpose(6, 4, 0, 2, 1, 3, 5)
    c["dfttab"] = _bf(tab)
    n = np.arange(256, dtype=np.int64)[:, None]; k = np.arange(256, dtype=np.int64)[None, :]
    ph = ((n * k) % 256).astype(np.float64) * (2.0 * np.pi / 256.0)
    t2 = np.stack([np.cos(ph), -np.sin(ph)], axis=1)
    t2 = 4.0 * t2.reshape(2, 128, 2, 256).transpose(1, 0, 2, 3)
    c["dft256"] = _bf(t2)
    cc = np.arange(64, dtype=np.int64)
    ph = ((cc[:, None] * cc[None, :]) % 64).astype(np.float64) * (2.0 * np.pi / 64.0)
    d64 = np.stack([np.cos(ph) / 512.0, np.sin(ph) / 512.0], axis=1)
    c["dft64"] = d64.astype(np.float32)
    sel = np.zeros((16, 16, 128), np.float32)
    for e in range(16):
        sel[e, e, :] = 1.0
    c["sel"] = _bf(sel)
    return c


def host_prep(x, c, ctx, c_ctx, w_mod, b_mod, norm1_g, w_in, q_norm_g, k_norm_g, attn_sink,
              pool_w, pool_scale, four_w, w_out, norm2_g, w_grp, b_grp, w_rtr, b_rtr,
              w_gate, w_up, w_down):
    f = lambda a: np.ascontiguousarray(np.asarray(a, dtype=np.float32))
    x, c, ctx, c_ctx = f(x), f(c), f(ctx), f(c_ctx)
    w_in = f(w_in); w_out = f(w_out)
    sh = {}
    sh["w_mod"] = f(w_mod)
    sh["bmodT"] = f(np.asarray(b_mod).reshape(2, 48, 128).transpose(0, 2, 1))
    sh["n1g"] = f(np.asarray(norm1_g).reshape(2, 8, 128).transpose(0, 2, 1))
    sh["n2g"] = f(np.asarray(norm2_g).reshape(2, 8, 128).transpose(0, 2, 1))
    qcols = []
    for g in range(4):
        qcols += list(range(256 + g * 64, 256 + g * 64 + 64)) + list(range(256 + (4 + g) * 64, 256 + (4 + g) * 64 + 64))
    cols = list(range(0, 256)) + qcols + list(range(768, 896)) + list(range(896, 1024))
    sh["w_inp"] = f(w_in[:, :, cols])
    sh["w_inFT"] = f(w_in[:, :, 1024:1280].reshape(2, 1024, 4, 64).transpose(0, 3, 2, 1))
    sh["four_w2"] = f(np.asarray(four_w).transpose(0, 2, 1, 3))
    sh["qg"] = f(np.tile(np.asarray(q_norm_g), (1, 2))[:, :, None])
    sh["kg"] = f(np.tile(np.asarray(k_norm_g), (1, 2))[:, :, None])
    sh["sinkb"] = f(np.broadcast_to(np.asarray(attn_sink).reshape(2, 1, 2, 4), (2, 128, 2, 4)))
    pw = np.asarray(pool_w, dtype=np.float32)
    bd = np.zeros((2, 2, 128, 128), np.float32)
    for l in range(2):
        for ch in range(2):
            bd[l, ch, 0:64, 0:64] = pw[l, 2 * ch]
            bd[l, ch, 64:, 64:] = pw[l, 2 * ch + 1]
    sh["poolw_bd"] = bd
    sh["pscale"] = f(np.asarray(pool_scale).reshape(2, 2, 128).transpose(0, 2, 1))
    arows = []
    for g in range(4):
        arows += list(range(256 + g * 64, 256 + g * 64 + 64)) + list(range(256 + (4 + g) * 64, 256 + (4 + g) * 64 + 64))
    rows = list(range(0, 256)) + arows + list(range(768, 1024))
    sh["w_outp"] = f(w_out[:, rows, :])
    sh["wr"] = f(np.concatenate([np.asarray(w_grp), np.asarray(w_rtr)], axis=-1))
    br = np.concatenate([np.asarray(b_grp), np.asarray(b_rtr)], axis=-1)
    sh["br"] = f(np.broadcast_to(br[:, None, :], (2, 128, 20)))
    sh["w_gate"] = f(w_gate); sh["w_up"] = f(w_up); sh["w_down"] = f(w_down)
    cons = [_consts(0), _consts(1)]
    in_maps = []
    for core in range(8):
        b, par = core // 2, core % 2
        m = dict(sh)
        m.update(cons[par])
        m["xT"] = f(x[b, par * NTOK:(par + 1) * NTOK, :].T)
        m["ctxT"] = f(ctx[b].T)
        m["sT"] = f(np.stack([c[b], c_ctx], axis=1))
        in_maps.append(m)
    return in_maps


IN_SPECS = [
    ("xT", [D, NTOK], F32), ("ctxT", [D, LC], F32), ("sT", [D, 2], F32),
    ("w_mod", [2, D, 6 * D], F32), ("bmodT", [2, 128, 48], F32), ("n1g", [2, 128, 8], F32), ("n2g", [2, 128, 8], F32),
    ("w_inp", [2, D, 1024], F32), ("w_inFT", [2, 64, 4, D], F32), ("four_w2", [2, 64, 4, 64], F32),
    ("qg", [2, 128, 1], F32), ("kg", [2, 128, 1], F32), ("sinkb", [2, 128, 2, 4], F32),
    ("poolw_bd", [2, 2, 128, 128], F32), ("pscale", [2, 128, 2], F32), ("w_outp", [2, D, D], F32),
    ("wr", [2, D, 20], F32), ("br", [2, 128, 20], F32),
    ("w_gate", [2, 16, D, 512], F32), ("w_up", [2, 16, D, 512], F32), ("w_down", [2, 16, 512, D], F32),
    ("ropeC", [128, TOK], BF16), ("ropeS", [128, TOK], BF16), ("rotm", [128, 128], BF16), ("hsum", [128, 128], BF16),
    ("ones128", [128, 128], BF16), ("ident32", [128, 128], F32), ("masks", [128, 4, 128], BF16),
    ("poolfix", [128, 2, 2, 8], F32), ("poolfixc", [128, 2, 2, 8], F32), ("halov", [128, 2], F32),
    ("dfttab", [2, 2, 4, 128, 4, 2, 512], BF16), ("dft256", [128, 2, 2, 256], BF16), ("dft64", [64, 2, 64], F32),
    ("sel", [16, 16, 128], BF16),
]


def build(layers=(0, 1), stop=None, dumps=()):
    nc = bass.Bass("TRN2", target_bir_lowering=False)
    I = {}
    for name, shape, dt in IN_SPECS:
        I[name] = nc.dram_tensor(name, shape, dt, kind="ExternalInput").ap()
    outT = nc.dram_tensor("outT", [D, NTOK], F32, kind="ExternalOutput").ap()
    DUMP = {}
    dump_specs = {"xT": [D, NTOK], "xcT": [D, LC], "catT": [D, TOK], "mod": [128, 2 * 48 * 2], "kz": [128, 2 * NB * 128],
                  "vaug": [128, NB * 256], "gates": [16, TOK]}
    for dn in dumps:
        DUMP[dn] = nc.dram_tensor("dump_" + dn, dump_specs[dn], F32, kind="ExternalOutput").ap()
    gin_ab = nc.dram_tensor("gin_ab", [NTOK, 512], BF16).ap()
    gout_ab = nc.dram_tensor("gout_ab", [2 * NTOK, 512], BF16).ap()
    gin_h = nc.dram_tensor("gin_h", [256, 512], BF16).ap()
    gout_h = nc.dram_tensor("gout_h", [512, 512], BF16).ap()
    t_gin_ab, t_gout_ab, t_gin_h, t_gout_h = T("gin_ab"), T("gout_ab"), T("gin_h"), T("gout_h")
    PAIRS = [[0, 1], [2, 3], [4, 5], [6, 7]]

    with ExitStack() as top:
        P = Prog(nc, top, same_engine_sync=SAME_SYNC)

        uid = [0]

        def sbt(st, name, shape, dt):
            uid[0] += 1
            return st.enter_context(nc.sbuf_tensor("sb%d_%s" % (uid[0], name), shape, dt))

        xT = sbt(top, "xT", [128, 8, NTOK], F32)
        xcT = sbt(top, "xcT", [128, 8, LC], F32)
        catT = sbt(top, "catT", [128, 8, TOK], BF16)
        modT = sbt(top, "modT", [128, 2, 48, 2], F32)
        A1s = sbt(top, "A1s", [128, 2, 8, 2], F32)
        A2s = sbt(top, "A2s", [128, 2, 8, 2], F32)
        rotm = sbt(top, "rotm", [128, 128], BF16)
        hsum = sbt(top, "hsum", [128, 128], BF16)
        ones128 = sbt(top, "ones128", [128, 128], BF16)
        ident32 = sbt(top, "ident32", [128, 128], F32)
        epsb = sbt(top, "epsb", [128, 1], F32)
        TX = [[T("x%d_%d" % (m, g)) for g in range(4)] for m in range(8)]
        TXC = [T("xc%d" % m) for m in range(8)]
        TC = [[T("c%d_%d" % (k, b)) for b in range(18)] for k in range(8)]
        t_modl = {0: T("mod0"), 1: T("mod1")}
        t_Al = {0: T("A0"), 1: T("A1")}
        t_const = T("const")
        PSA = top.enter_context(nc.psum_tensor("PSA", [128, 2048], F32))
        PSB = top.enter_context(nc.psum_tensor("PSB", [128, 2048], F32))
        TP = [T("bank%d" % i) for i in range(8)]

        def bank(i):
            big = PSA if i < 4 else PSB
            j = i % 4
            return big[:, j * 512:(j + 1) * 512]
        bank_rr = [0]

        reserved = set()

        def next_bank():
            while True:
                i = bank_rr[0] % 8
                bank_rr[0] += 1
                if i not in reserved:
                    return bank(i), TP[i]

        dk_const = P.dsem()
        dk_x = P.dsem()
        dk_w = [P.dsem() for _ in range(4)]
        dk_misc = P.dsem()
        dk_out = P.dsem()
        dk_gw = P.dsem()
        dk_cc = P.dsem()
        dk_cc2 = P.dsem()
        dk_gr = P.dsem()
        dk_tab = [P.dsem() for _ in range(3)]
        dk_ex = [[P.dsem() for _ in range(3)] for _ in range(2)]

        for nm, tl in (("rotm", rotm), ("hsum", hsum), ("ones128", ones128), ("ident32", ident32)):
            P.dma("sp", tl[:], I[nm], [], [t_const], dk_const)
        P.op("pool", lambda h: h.memset(epsb[:], EPS), [], [t_const])
        for m in range(8):
            for g in range(4):
                P.dma("sp", xT[:, m, g * 512:(g + 1) * 512], I["xT"][m * 128:(m + 1) * 128, g * 512:(g + 1) * 512], [], [TX[m][g]], dk_x)
            P.dma("sp", xcT[:, m, :], I["ctxT"][m * 128:(m + 1) * 128, :], [], [TXC[m]], dk_x)

        def cat_T(ks, c0, n):
            return [TC[k][b] for k in ks for b in range(c0 // 128, (c0 + n + 127) // 128)]

        s32 = sbt(top, "s32", [128, 8, 2], F32); t_s32 = T()
        sbf = sbt(top, "sbf", [128, 8, 2], BF16); t_sbf = T()
        bmod = sbt(top, "bmod", [128, 2, 48], F32); t_bmod = T()
        ngam = sbt(top, "ngam", [128, 2, 2, 8], F32); t_ng = T()
        P.dma("sp", s32[:], I["sT"].rearrange("(k p) c -> p k c", p=128), [], [t_s32], dk_misc)
        P.dma("sp", bmod[:], I["bmodT"].rearrange("l p n -> p l n"), [], [t_bmod], dk_misc)
        P.dma("sp", ngam[:, 0], I["n1g"].rearrange("l p k -> p l k"), [], [t_ng], dk_misc)
        P.dma("sp", ngam[:, 1], I["n2g"].rearrange("l p k -> p l k"), [], [t_ng], dk_misc)
        P.op("act", lambda h: h.activation(out=sbf[:], in_=s32[:], func=AF.Silu), [t_s32], [t_sbf])

        def mod_piece_dma(l, v, wmt, t_wmt):
            P.dma("pool", wmt[:], I["w_mod"][l, :, v * 1024:(v + 1) * 1024].rearrange("(k p) c -> p k c", p=128), [], [t_wmt], dk_misc)

        def mod_piece_mm(l, v, wmt, t_wmt, pb, tpb):
            for c8 in range(8):
                n = v * 8 + c8
                for k in range(8):
                    P.op("pe", lambda h, o=pb[:, 2 * n:2 * n + 2], w=wmt[:, k, c8 * 128:(c8 + 1) * 128], r=sbf[:, k, :], k=k:
                         h.matmul(o, lhsT=w, rhs=r, start=(k == 0), stop=(k == 7)),
                         [t_wmt, t_sbf], [tpb], inc=(k == 7))

        def mod_finalize(l, pb, tpb):
            P.op("dve", lambda h, l=l, pb=pb: h.tensor_tensor(out=modT[:, l], in0=pb[:, 0:96].rearrange("p (n c) -> p n c", c=2),
                                                              in1=bmod[:, l].unsqueeze(2).to_broadcast([128, 48, 2]), op=ALU.add),
                 [tpb, t_bmod], [t_modl[l]])
            P.op("dve", lambda h, l=l: h.scalar_tensor_tensor(out=A1s[:, l], in0=modT[:, l, 8:16, :], scalar=1.0,
                                                              in1=ngam[:, 0, l].unsqueeze(2).to_broadcast([128, 8, 2]),
                                                              op0=ALU.add, op1=ALU.mult), [t_modl[l], t_ng], [t_Al[l]])
            P.op("dve", lambda h, l=l: h.scalar_tensor_tensor(out=A2s[:, l], in0=modT[:, l, 32:40, :], scalar=1.0,
                                                              in1=ngam[:, 1, l].unsqueeze(2).to_broadcast([128, 8, 2]),
                                                              op0=ALU.add, op1=ALU.mult), [t_modl[l], t_ng], [t_Al[l]])

        def mod_finalize_part(l, pb, tpb, v0, v1):
            P.op("dve", lambda h, l=l, pb=pb: h.tensor_tensor(out=modT[:, l, v0 * 8:v1 * 8, :], in0=pb[:, v0 * 16:v1 * 16].rearrange("p (n c) -> p n c", c=2),
                                                              in1=bmod[:, l, v0 * 8:v1 * 8].unsqueeze(2).to_broadcast([128, (v1 - v0) * 8, 2]), op=ALU.add),
                 [tpb, t_bmod], [t_modl[l]])
            if v0 <= 1 < v1:
                P.op("dve", lambda h, l=l: h.scalar_tensor_tensor(out=A1s[:, l], in0=modT[:, l, 8:16, :], scalar=1.0,
                                                                  in1=ngam[:, 0, l].unsqueeze(2).to_broadcast([128, 8, 2]),
                                                                  op0=ALU.add, op1=ALU.mult), [t_modl[l], t_ng], [t_Al[l]])
            if v0 <= 4 < v1:
                P.op("dve", lambda h, l=l: h.scalar_tensor_tensor(out=A2s[:, l], in0=modT[:, l, 32:40, :], scalar=1.0,
                                                                  in1=ngam[:, 1, l].unsqueeze(2).to_broadcast([128, 8, 2]),
                                                                  op0=ALU.add, op1=ALU.mult), [t_modl[l], t_ng], [t_Al[l]])

        HIDE_MOD1 = (len(layers) == 2)
        with ExitStack() as st:
            wm = [sbt(st, "wm%d" % i, [128, 8, 1024], BF16) for i in range(2)]
            t_wm = [T(), T()]
            it = 0
            for l in (layers[:1] if HIDE_MOD1 else layers):
                pb, tpb = next_bank()
                for v in range(2 if HIDE_MOD1 else 6):
                    slot = it % 2
                    it += 1
                    mod_piece_dma(l, v, wm[slot], t_wm[slot])
                    mod_piece_mm(l, v, wm[slot], t_wm[slot], pb, tpb)
                if HIDE_MOD1:
                    mod_finalize_part(l, pb, tpb, 0, 2)
                else:
                    mod_finalize(l, pb, tpb)
            P.barrier()
        if "mod" in DUMP:
            P.dma("sp", DUMP["mod"], modT[:].rearrange("p l n c -> p (l n c)"), [t_modl[0], t_modl[1]], [], dk_out)

        def modv(l, v, k, c):
            return modT[:, l, v * 8 + k, c:c + 1]

        GROUPS = [(g * 512, 512, 0) for g in range(4)] + [(NTOK, LC, 1)]

        def x_ap(k, c0, n):
            if c0 < NTOK:
                return xT[:, k, c0:c0 + n]
            return xcT[:, k, c0 - NTOK:c0 - NTOK + n]

        def x_T(k, c0):
            if c0 < NTOK:
                return TX[k][c0 // 512]
            return TXC[k]

        def norm_mod(st_tiles, l, which, c0, n, isctx, dst, t_dst):
            rstds, tmps = st_tiles
            rstd, t_rstd = rstds.next()
            As = A1s if which == 0 else A2s
            shv = 0 if which == 0 else 3
            pb, tpb = next_bank()
            for k in range(8):
                P.op("act", lambda h, k=k: h.activation(out=dst(k), in_=x_ap(k, c0, n), func=AF.Square),
                     [x_T(k, c0)], t_dst(k))
            for k in range(8):
                P.op("pe", lambda h, k=k: h.matmul(pb[:, 0:n], lhsT=ones128[:], rhs=dst(k), start=(k == 0), stop=(k == 7)),
                     t_dst(k) + [t_const], [tpb], inc=(k == 7))
            P.op("act", lambda h: h.activation(out=rstd[:, 0:n], in_=pb[:, 0:n], func=AF.Ln, bias=epsb[:, 0:1], scale=1.0 / D), [tpb, t_const], [t_rstd])
            P.op("act", lambda h: h.activation(out=rstd[:, 0:n], in_=rstd[:, 0:n], func=AF.Exp, scale=-0.5), [t_rstd], [t_rstd])
            for k in range(8):
                tmp, t_tmp = tmps.next()
                P.op("dve", lambda h, k=k, tmp=tmp: h.scalar_tensor_tensor(out=tmp[:, 0:n], in0=x_ap(k, c0, n), scalar=As[:, l, k, isctx:isctx + 1],
                                                                          in1=rstd[:, 0:n], op0=ALU.mult, op1=ALU.mult),
                     [x_T(k, c0), t_Al[l], t_rstd] + t_dst(k), [t_tmp])
                P.op("act", lambda h, k=k, tmp=tmp: h.activation(out=dst(k), in_=tmp[:, 0:n], func=AF.Identity,
                                                                 bias=modv(l, shv, k, isctx), scale=1.0),
                     [t_tmp, t_modl[l]], t_dst(k))

        for l in layers:
            last = (l == 1)
            with ExitStack() as mx:
                ABc = sbt(mx, "ABc", [128, 2, 512], BF16)
                qkg = sbt(mx, "qkg", [128, 2], F32)
                esink = sbt(mx, "esink", [128, 2, 4], F32)
                mxa = mx.enter_context(ExitStack())
                upad = sbt(mxa, "upad", [128, 2, UPAD + NTOK + UPAD], BF16)
                upadc = sbt(mxa, "upadc", [128, 2, UPAD + LC + UPAD], BF16)
                mxb = mxa.enter_context(ExitStack())
                Kz = [sbt(mxb, "Kz%d" % i, [128, NB * 128], BF16) for i in range(2)]
                Vaug = sbt(mxb, "Vaug", [128, NB, 2, 128], BF16)
                ropeC = sbt(mxb, "ropeC", [128, TOK], BF16)
                ropeS = sbt(mxb, "ropeS", [128, TOK], BF16)
                masks = sbt(mxb, "masks", [128, 4, 128], BF16)
                TK = [T("k%d" % b) for b in range(NB)]
                TV = [T("v%d" % b) for b in range(NB)]
                t_rope, t_masks, t_qkg, t_esink, t_ABc = T(), T(), T(), T(), T()
                TU = [T("u%d" % g) for g in range(4)]
                t_uh, t_uc = T("uh"), T("uc")
                P.dma("sp", ropeC[:], I["ropeC"], [], [t_rope], dk_misc)
                P.dma("sp", ropeS[:], I["ropeS"], [], [t_rope], dk_misc)
                P.dma("sp", masks[:], I["masks"], [], [t_masks], dk_misc)
                P.dma("sp", qkg[:, 0:1], I["qg"][l], [], [t_qkg], dk_misc)
                P.dma("sp", qkg[:, 1:2], I["kg"][l], [], [t_qkg], dk_misc)
                P.dma("sp", esink[:], I["sinkb"][l], [], [t_esink], dk_misc)
                P.op("act", lambda h: h.activation(out=esink[:], in_=esink[:], func=AF.Exp), [t_esink], [t_esink])
                P.op("pool", lambda h: h.memset(Kz[0][:], 0.0), [], TK)
                P.op("pool", lambda h: h.memset(Kz[1][:], 0.0), [], TK)
                P.op("pool", lambda h: h.memset(Vaug[:], 1.0), [], TV)
                P.op("pool", lambda h: h.memset(upad[:], 0.0), [], TU + [t_uh])
                P.op("pool", lambda h: h.memset(upadc[:], 0.0), [], [t_uc])

                with ExitStack() as st:
                    win = sbt(st, "win", [128, 8, 1536], BF16); t_win, t_wab = T(), T()
                    fs = st.enter_context(ExitStack())
                    wft = sbt(fs, "wft", [64, 4, D], F32); t_wft = T()
                    fw2 = sbt(fs, "fw2", [64, 4, 64], F32); t_fw2 = T()
                    d64 = sbt(fs, "d64", [64, 2, 64], F32); t_d64 = T()
                    m1 = sbt(fs, "m1", [64, 4, 2, 64], F32); t_m1 = T()
                    P.dma("pool", win[:, :, 0:1024], I["w_inp"][l].rearrange("(k p) c -> p k c", p=128), [], [t_win], dk_w[2])
                    P.dma("sp", wft[:], I["w_inFT"][l], [], [t_wft], dk_misc)
                    P.dma("sp", fw2[:], I["four_w2"][l], [], [t_fw2], dk_misc)
                    P.dma("sp", d64[:], I["dft64"], [], [t_d64], dk_misc)
                    pb, tpb = next_bank()
                    for g in range(4):
                        for cs in range(2):
                            P.op("pe", lambda h, g=g, cs=cs, pb=pb: h.matmul(pb[0:64, (g * 2 + cs) * 64:(g * 2 + cs + 1) * 64], lhsT=d64[:, cs, :], rhs=fw2[:, g, :],
                                                                             start=True, stop=True), [t_d64, t_fw2], [tpb], inc=(g == 3 and cs == 1))
                    P.op("dve", lambda h, pb=pb: h.tensor_copy(out=m1[:].rearrange("p g c d -> p (g c d)"), in_=pb[0:64, 0:512]), [tpb], [t_m1])
                    for k in range(8):
                        pb, tpb = next_bank()
                        for g in range(4):
                            P.op("pe", lambda h, g=g, k=k, pb=pb: h.matmul(pb[:, g * 128:(g + 1) * 128], lhsT=wft[:, g, k * 128:(k + 1) * 128],
                                                                           rhs=m1[:, g].rearrange("p c d -> p (c d)"), start=True, stop=True),
                                 [t_wft, t_m1], [tpb], inc=(g == 3))
                        P.op("act", lambda h, k=k, pb=pb: h.copy(out=win[:, k, 1024:1536].rearrange("p (c g d) -> p g c d", c=2, g=4),
                                                                 in_=pb[:, 0:512].rearrange("p (g c d) -> p g c d", g=4, c=2)), [tpb], [t_wab])

                    P.barrier()
                    fs.close()
                    AN = 256
                    hTs = [(sbt(st, "hT%d" % i, [128, 8, AN], BF16), T()) for i in range(2)]
                    rstds = Ring([(sbt(st, "rstd%d" % i, [128, AN], F32), T()) for i in range(2)])
                    tmps = Ring([(sbt(st, "ntmp%d" % i, [128, AN], F32), T()) for i in range(3)])
                    sqq = Ring([(sbt(st, "sqq%d" % i, [128, AN], BF16), T()) for i in range(3)])
                    sdr = Ring([(sbt(st, "sd%d" % i, [128, AN], F32), T()) for i in range(3)])
                    qnr = Ring([(sbt(st, "qn%d" % i, [128, AN], BF16), T()) for i in range(3)])
                    t1r = Ring([(sbt(st, "t1_%d" % i, [128, AN], F32), T()) for i in range(3)])
                    t2r = Ring([(sbt(st, "t2_%d" % i, [128, AN], F32), T()) for i in range(3)])
                    stg = Ring([(sbt(st, "stg%d" % i, [128, 512], BF16), T()) for i in range(3)])
                    kcol_of = lambda c0: (128 + c0) if c0 < NTOK else (18 * 128 + c0 - NTOK)
                    GROUPS_A1 = [(g * AN, AN, 0) for g in range(NTOK // AN)] + [(NTOK, LC, 1)]

                    def emit_norm(gi):
                        c0, n, isctx = GROUPS_A1[gi]
                        hT, t_hT = hTs[gi % 2]
                        norm_mod((rstds, tmps), l, 0, c0, n, isctx, lambda k, hT=hT, n=n: hT[:, k, 0:n], lambda k, t_hT=t_hT: [t_hT])

                    def emit_chunks(gi):
                        c0, n, isctx = GROUPS_A1[gi]
                        hT, t_hT = hTs[gi % 2]
                        need_full = (not last) or (not isctx)
                        mlist = list(range(7)) if need_full else [6]
                        state = {}

                        def stage1(m):
                            pb, tpb = next_bank()
                            for k in range(8):
                                P.op("pe", lambda h, k=k, m=m, pb=pb: h.matmul(pb[:, 0:n], lhsT=win[:, k, m * 128:(m + 1) * 128], rhs=hT[:, k, 0:n],
                                                                               start=(k == 0), stop=(k == 7)), [t_win, t_hT], [tpb], inc=(k == 7))
                            if m < 2:
                                if isctx:
                                    P.op("act", lambda h, m=m, pb=pb: h.copy(out=upadc[:, m, UPAD:UPAD + n], in_=pb[:, 0:n]), [tpb], [t_uc])
                                else:
                                    P.op("act", lambda h, m=m, pb=pb: h.copy(out=upad[:, m, UPAD + c0:UPAD + c0 + n], in_=pb[:, 0:n]), [tpb], [TU[c0 // 512]])
                                return
                            sq, t_sq = sqq.next()
                            P.op("act", lambda h, pb=pb, sq=sq: h.activation(out=sq[:, 0:n], in_=pb[:, 0:n], func=AF.Square), [tpb], [t_sq])
                            state[m] = dict(pb=pb, tpb=tpb, sq=sq, t_sq=t_sq)

                        def stage2(m):
                            if m < 2:
                                return
                            S = state[m]
                            isk = (m == 6)
                            pb2, tpb2 = next_bank()
                            P.op("pe", lambda h, pb2=pb2, sq=S["sq"]: h.matmul(pb2[:, 0:n], lhsT=hsum[:], rhs=sq[:, 0:n], start=True, stop=True), [S["t_sq"], t_const], [tpb2])
                            sd, t_sd = sdr.next()
                            P.op("act", lambda h, pb2=pb2, sd=sd: h.activation(out=sd[:, 0:n], in_=pb2[:, 0:n], func=AF.Ln, bias=epsb[:, 0:1], scale=1.0), [tpb2, t_const], [t_sd])
                            P.op("act", lambda h, sd=sd: h.activation(out=sd[:, 0:n], in_=sd[:, 0:n], func=AF.Exp, scale=-0.5), [t_sd], [t_sd])
                            qn, t_qn = qnr.next()
                            P.op("dve", lambda h, pb=S["pb"], sd=sd, qn=qn, isk=isk: h.scalar_tensor_tensor(out=qn[:, 0:n], in0=pb[:, 0:n], scalar=qkg[:, (1 if isk else 0):(2 if isk else 1)],
                                                                                                          in1=sd[:, 0:n], op0=ALU.mult, op1=ALU.mult), [S["tpb"], t_sd, t_qkg], [t_qn])
                            S.update(qn=qn, t_qn=t_qn)

                        def stage3(m):
                            if m < 2:
                                return
                            S = state[m]
                            isk = (m == 6)
                            qn, t_qn = S["qn"], S["t_qn"]
                            pb3, tpb3 = next_bank()
                            P.op("pe", lambda h, pb3=pb3, qn=qn: h.matmul(pb3[:, 0:n], lhsT=rotm[:], rhs=qn[:, 0:n], start=True, stop=True), [t_qn, t_const], [tpb3])
                            t1, t_t1 = t1r.next()
                            t2, t_t2 = t2r.next()
                            P.op("dve", lambda h, t1=t1, qn=qn: h.tensor_tensor(out=t1[:, 0:n], in0=qn[:, 0:n], in1=ropeC[:, c0:c0 + n], op=ALU.mult), [t_qn, t_rope], [t_t1])
                            P.op("dve", lambda h, t2=t2, pb3=pb3: h.tensor_tensor(out=t2[:, 0:n], in0=pb3[:, 0:n], in1=ropeS[:, c0:c0 + n], op=ALU.mult), [tpb3, t_rope], [t_t2])
                            if isk:
                                kc = kcol_of(c0)
                                tks = [TK[b] for b in range(kc // 128, (kc + n) // 128)]
                                P.op("dve", lambda h, t1=t1, t2=t2, kc=kc: h.tensor_tensor(out=Kz[0][0:64, kc:kc + n], in0=t1[0:64, 0:n], in1=t2[0:64, 0:n], op=ALU.add), [t_t1, t_t2], tks)
                                P.op("dve", lambda h, t1=t1, t2=t2, kc=kc: h.tensor_tensor(out=Kz[1][64:128, kc:kc + n], in0=t1[64:128, 0:n], in1=t2[64:128, 0:n], op=ALU.add), [t_t1, t_t2], tks)
                            else:
                                P.op("dve", lambda h, t1=t1, t2=t2, m=m: h.tensor_tensor(out=catT[:, m, c0:c0 + n], in0=t1[:, 0:n], in1=t2[:, 0:n], op=ALU.add), [t_t1, t_t2], cat_T([m], c0, n))
                        nm = len(mlist)
                        for step in range(nm + 2):
                            if step < nm:
                                stage1(mlist[step])
                            if 0 <= step - 1 < nm:
                                stage2(mlist[step - 1])
                            if 0 <= step - 2 < nm:
                                stage3(mlist[step - 2])

                    def emit_tokmajor(gi):
                        c0, n, isctx = GROUPS_A1[gi]
                        hT, t_hT = hTs[gi % 2]
                        need_full = (not last) or (not isctx)
                        for tt in range(n // 128):
                            cc0 = tt * 128
                            blk = kcol_of(c0 + cc0) // 128
                            pb, tpb = next_bank()
                            for k in range(8):
                                P.op("pe", lambda h, k=k, pb=pb, cc0=cc0: h.matmul(pb[:, 0:128], lhsT=hT[:, k, cc0:cc0 + 128], rhs=win[:, k, 896:1024],
                                                                                   start=(k == 0), stop=(k == 7)), [t_win, t_hT], [tpb], inc=(k == 7))
                            P.op("act", lambda h, pb=pb, blk=blk: h.copy(out=Vaug[:, blk, 0, 0:64], in_=pb[:, 0:64]), [tpb], [TV[blk]])
                            P.op("act", lambda h, pb=pb, blk=blk: h.copy(out=Vaug[:, blk, 1, 64:128], in_=pb[:, 64:128]), [tpb], [TV[blk]])
                            if not need_full:
                                continue
                            pb, tpb = next_bank()
                            for k in range(8):
                                P.op("pe", lambda h, k=k, pb=pb, cc0=cc0: h.matmul(pb[:, 0:512], lhsT=hT[:, k, cc0:cc0 + 128], rhs=win[:, k, 1024:1536],
                                                                                   start=(k == 0), stop=(k == 7)), [t_win, t_wab, t_hT], [tpb], inc=(k == 7))
                            if isctx:
                                P.op("dve", lambda h, pb=pb, tt=tt: h.tensor_copy(out=ABc[:, tt, :], in_=pb[:, 0:512]), [tpb], [t_ABc])
                            else:
                                sg, t_sg = stg.next()
                                P.op("dve", lambda h, pb=pb, sg=sg: h.tensor_copy(out=sg[:], in_=pb[:, 0:512]), [tpb], [t_sg])
                                r0 = c0 + cc0
                                P.dma("sp", gin_ab[r0:r0 + 128, :], sg[:], [t_sg], [t_gin_ab], dk_gw)

                    emit_norm(0)
                    for gi in range(len(GROUPS_A1)):
                        if gi + 1 < len(GROUPS_A1):
                            emit_norm(gi + 1)
                        emit_tokmajor(gi)
                        emit_chunks(gi)
                    hv = lambda r0, nr: gin_h[r0:r0 + nr, :].rearrange("r (a j) -> (r a) j", a=4)
                    P.dma("sp", hv(0, 32)[0:64, :], Kz[0][0:64, 128:256], [TK[1]], [t_gin_h], dk_gw)
                    P.dma("sp", hv(0, 32)[64:128, :], Kz[1][64:128, 128:256], [TK[1]], [t_gin_h], dk_gw)
                    P.dma("sp", hv(32, 32)[0:64, :], Kz[0][0:64, 16 * 128:17 * 128], [TK[16]], [t_gin_h], dk_gw)
                    P.dma("sp", hv(32, 32)[64:128, :], Kz[1][64:128, 16 * 128:17 * 128], [TK[16]], [t_gin_h], dk_gw)
                    P.dma("sp", hv(64, 32)[:, 0:64], Vaug[:, 1, 0, 0:64], [TV[1]], [t_gin_h], dk_gw)
                    P.dma("sp", hv(64, 32)[:, 64:128], Vaug[:, 1, 1, 64:128], [TV[1]], [t_gin_h], dk_gw)
                    P.dma("sp", hv(96, 32)[:, 0:64], Vaug[:, 16, 0, 0:64], [TV[16]], [t_gin_h], dk_gw)
                    P.dma("sp", hv(96, 32)[:, 64:128], Vaug[:, 16, 1, 64:128], [TV[16]], [t_gin_h], dk_gw)
                    pv = lambda r0: gin_h[r0:r0 + 4, :].rearrange("r (a j) -> (r a) j", a=32)[:, 0:16].rearrange("p (c j) -> p c j", c=2)
                    P.dma("sp", pv(128), upad[:, :, UPAD:UPAD + 8], [TU[0]], [t_gin_h], dk_gw)
                    P.dma("sp", pv(132), upad[:, :, UPAD + NTOK - 8:UPAD + NTOK], [TU[3]], [t_gin_h], dk_gw)
                    P.collective("AllGather", PAIRS, gin_h.opt(), gout_h.opt(), t_gin_h, t_gout_h, dk_cc)
                    P.collective("AllGather", PAIRS, gin_ab.opt(), gout_ab.opt(), t_gin_ab, t_gout_ab, dk_cc2)
                    P.barrier(exclude=(dk_cc, dk_cc2))
                if stop == "A1":
                    break

                with ExitStack() as st:
                    hvo = lambda r0, nr: gout_h[r0:r0 + nr, :].rearrange("r (a j) -> (r a) j", a=4)
                    P.dma("sp", Kz[0][0:64, 0:128], hvo(32, 32)[0:64, :], [t_gout_h], [TK[0]], dk_gr)
                    P.dma("sp", Kz[1][64:128, 0:128], hvo(32, 32)[64:128, :], [t_gout_h], [TK[0]], dk_gr)
                    P.dma("sp", Kz[0][0:64, 17 * 128:18 * 128], hvo(256, 32)[0:64, :], [t_gout_h], [TK[17]], dk_gr)
                    P.dma("sp", Kz[1][64:128, 17 * 128:18 * 128], hvo(256, 32)[64:128, :], [t_gout_h], [TK[17]], dk_gr)
                    P.dma("sp", Vaug[:, 0, 0, 0:64], hvo(96, 32)[:, 0:64], [t_gout_h], [TV[0]], dk_gr)
                    P.dma("sp", Vaug[:, 0, 1, 64:128], hvo(96, 32)[:, 64:128], [t_gout_h], [TV[0]], dk_gr)
                    P.dma("sp", Vaug[:, 17, 0, 0:64], hvo(256 + 64, 32)[:, 0:64], [t_gout_h], [TV[17]], dk_gr)
                    P.dma("sp", Vaug[:, 17, 1, 64:128], hvo(256 + 64, 32)[:, 64:128], [t_gout_h], [TV[17]], dk_gr)
                    pvo = lambda r0: gout_h[r0:r0 + 4, :].rearrange("r (a j) -> (r a) j", a=32)[:, 0:16].rearrange("p (c j) -> p c j", c=2)
                    P.dma("sp", upad[:, :, UPAD - 8:UPAD], pvo(132), [t_gout_h], [t_uh], dk_gr)
                    P.dma("sp", upad[:, :, UPAD + NTOK:UPAD + NTOK + 8], pvo(256 + 128), [t_gout_h], [t_uh], dk_gr)

                    Eloc = Ring([(sbt(st, "Eloc%d" % i, [128, 3, 4, 128], BF16), T()) for i in range(2)])
                    Ectx = Ring([(sbt(st, "Ectx%d" % i, [128, 2, 4, 128], BF16), T()) for i in range(2)])
                    rdn = Ring([(sbt(st, "rdn%d" % i, [128, 4, 128], F32), T()) for i in range(2)])
                    SL = PSA[:, 0:1536].rearrange("p (j g q) -> p j g q", j=3, g=4); TSL = TP[0:3]
                    SC = PSB[:, 0:1024].rearrange("p (j g q) -> p j g q", j=2, g=4); TSC = TP[4:6]
                    OB = [(PSB[:, 1024:1536].rearrange("p (g q) -> p g q", g=4), TP[6]), (PSB[:, 1536:2048].rearrange("p (g q) -> p g q", g=4), TP[7])]
                    qblocks = list(range(1, 15)) + ([] if last else [16, 17]) + [0, 15]
                    hideA = HIDE_MOD1 and l == layers[0]
                    if hideA:
                        wmA = sbt(st, "wmA", [128, 8, 1024], BF16); t_wmA = T()
                        mod_piece_dma(l, 2, wmA, t_wmA)
                    qpos = 0
                    for i in qblocks:
                        isctx = i >= 16
                        qc0 = i * 128
                        tq = [TC[2 + g][i] for g in range(4)]
                        Es = {}

                        def ph_S(kvh):
                            KZ = Kz[kvh]
                            if not isctx:
                                for jj in range(3):
                                    blk = i + jj
                                    for g in range(4):
                                        P.op("pe", lambda h, jj=jj, g=g, blk=blk, KZ=KZ: h.matmul(SL[:, jj, g, :], lhsT=KZ[:, blk * 128:(blk + 1) * 128], rhs=catT[:, 2 + g, qc0:qc0 + 128],
                                                                                                 start=True, stop=True), [TK[blk]] + tq, TSL, inc=(jj == 2 and g == 3))
                            for jj in range(2):
                                blk = 18 + jj
                                for g in range(4):
                                    P.op("pe", lambda h, jj=jj, g=g, blk=blk, KZ=KZ: h.matmul(SC[:, jj, g, :], lhsT=KZ[:, blk * 128:(blk + 1) * 128], rhs=catT[:, 2 + g, qc0:qc0 + 128],
                                                                                             start=True, stop=True), [TK[blk]] + tq, TSC, inc=(jj == 1 and g == 3))

                        def ph_E(kvh):
                            ec, t_ec = Ectx.next()
                            seqs = []
                            if not isctx:
                                el, t_el = Eloc.next()
                                P.op("act", lambda h, el=el: h.activation(out=el[:], in_=SL, func=AF.Exp, scale=0.125), TSL, [t_el])
                                mp = 2 if i == 0 else 0
                                mn = 3 if i == 15 else 1
                                P.op("dve", lambda h, el=el, mp=mp: h.tensor_tensor(out=el[:, 0], in0=el[:, 0], in1=masks[:, mp, :].unsqueeze(1).to_broadcast([128, 4, 128]), op=ALU.mult), [t_el, t_masks], [t_el])
                                P.op("dve", lambda h, el=el, mn=mn: h.tensor_tensor(out=el[:, 2], in0=el[:, 2], in1=masks[:, mn, :].unsqueeze(1).to_broadcast([128, 4, 128]), op=ALU.mult), [t_el, t_masks], [t_el])
                                seqs += [(el, t_el, jj, i + jj) for jj in range(3)]
                            P.op("act", lambda h, ec=ec: h.activation(out=ec[:], in_=SC, func=AF.Exp, scale=0.125), TSC, [t_ec])
                            seqs += [(ec, t_ec, jj, 18 + jj) for jj in range(2)]
                            Es[kvh] = seqs

                        def ph_PV(kvh):
                            ob, tob = OB[kvh]
                            seqs = Es[kvh]
                            for si, (E, t_E, jj, blk) in enumerate(seqs):
                                P.op("pe", lambda h, E=E, jj=jj, blk=blk, ob=ob, si=si, ns=len(seqs), kvh=kvh: h.matmul(ob, lhsT=Vaug[:, blk, kvh, :], rhs=E[:, jj], start=(si == 0), stop=(si == ns - 1)),
                                     [t_E, TV[blk]], [tob], inc=(si == len(seqs) - 1))

                        def ph_N(kvh):
                            ob, tob = OB[kvh]
                            rd, t_rd = rdn.next()
                            dlo, dhi = (64, 128) if kvh == 0 else (0, 64)
                            olo, ohi = (0, 64) if kvh == 0 else (64, 128)
                            P.op("dve", lambda h, rd=rd, ob=ob, dlo=dlo, dhi=dhi, kvh=kvh: h.tensor_tensor(out=rd[dlo:dhi], in0=ob[dlo:dhi], in1=esink[dlo:dhi, kvh, :].unsqueeze(2).to_broadcast([64, 4, 128]), op=ALU.add),
                                 [tob, t_esink], [t_rd])
                            P.op("act", lambda h, rd=rd, dlo=dlo, dhi=dhi: h.activation(out=rd[dlo:dhi], in_=rd[dlo:dhi], func=AF.Ln), [t_rd], [t_rd])
                            P.op("act", lambda h, rd=rd, dlo=dlo, dhi=dhi: h.activation(out=rd[dlo:dhi], in_=rd[dlo:dhi], func=AF.Exp, scale=-1.0), [t_rd], [t_rd])
                            P.op("dve", lambda h, rd=rd, ob=ob, olo=olo, ohi=ohi, dlo=dlo, dhi=dhi: h.tensor_tensor(out=catT[olo:ohi, 2:6, qc0:qc0 + 128], in0=ob[olo:ohi], in1=rd[dlo:dhi], op=ALU.mult),
                                 [tob, t_rd], tq)
                        ph_S(0); ph_E(0); ph_S(1); ph_E(1); ph_PV(0); ph_N(0); ph_PV(1); ph_N(1)
                        qpos += 1
                        if hideA and qpos in (3, 6, 9, 12):
                            v_ = 2 + (qpos // 3 - 1)
                            mod_piece_mm(l, v_, wmA, t_wmA, bank(3), TP[3])
                            if v_ < 5:
                                mod_piece_dma(l, v_ + 1, wmA, t_wmA)
                            else:
                                mod_finalize_part(l, bank(3), TP[3], 2, 6)
                    P.barrier()
                if stop == "A2":
                    break
                mxb.close()
                wout = sbt(mxa, "wout", [128, 8, D], BF16); t_wout = T()
                P.dma("pool", wout[:], I["w_outp"][l].rearrange("(k p) c -> p k c", p=128), [], [t_wout], dk_w[3])

                with ExitStack() as st:
                    AB = sbt(st, "AB", [128, 32, 512], BF16); t_AB = [T() for _ in range(4)]
                    tabs = [(sbt(st, "tab%d" % i, [128, 4, 2, 512], BF16), T(), dk_tab[i]) for i in range(2)]
                    d256 = sbt(st, "d256", [128, 2, 2, 256], BF16); t_d256 = T()
                    P.dma("sp", d256[:], I["dft256"], [], [t_d256], dk_misc)
                    halov = sbt(st, "halov", [128, 2], F32); t_halov = T()
                    pfix = sbt(st, "pfix", [128, 2, 2, 8], F32)
                    pfixc = sbt(st, "pfixc", [128, 2, 2, 8], F32); t_pfix = T()
                    pwbd = sbt(st, "pwbd", [128, 2, 128], BF16); t_pwbd = T()
                    psc = sbt(st, "psc", [128, 2], F32); t_psc = T()
                    P.dma("sp", halov[:], I["halov"], [], [t_halov], dk_misc)
                    P.dma("sp", pfix[:], I["poolfix"], [], [t_pfix], dk_misc)
                    P.dma("sp", pfixc[:], I["poolfixc"], [], [t_pfix], dk_misc)
                    P.dma("pool", pwbd[:], I["poolw_bd"][l].rearrange("c p m -> p c m"), [], [t_pwbd], dk_misc)
                    P.dma("sp", psc[:], I["pscale"][l], [], [t_psc], dk_misc)
                    for q4 in range(4):
                        P.dma("sp", AB[:, q4 * 8:(q4 + 1) * 8, :], gout_ab[q4 * 1024:(q4 + 1) * 1024, :].rearrange("(n p) c -> p n c", p=128),
                              [t_gout_ab], [t_AB[q4]], dk_gr)
                    for hq in range(2):
                        lo = AB[:, hq * 8:(hq + 1) * 8, :]
                        hi = AB[:, 16 + hq * 8:16 + (hq + 1) * 8, :]
                        P.op("dve", lambda h, lo=lo, hi=hi: h.tensor_tensor(out=lo, in0=lo, in1=hi, op=ALU.add), [t_AB[hq], t_AB[2 + hq]], [t_AB[hq]])
                        P.op("dve", lambda h, lo=lo, hi=hi: h.scalar_tensor_tensor(out=hi, in0=hi, scalar=-2.0, in1=lo, op0=ALU.mult, op1=ALU.add), [t_AB[hq], t_AB[2 + hq]], [t_AB[2 + hq]])
                    pt = [(sbt(st, "pt%d" % i, [128, 512 + 32], F32), T()) for i in range(3)]
                    deferred = []
                    SEG = ([] if last else [(0, LC, 1)]) + [(512, 512, 0), (1024, 512, 0), (0, 512, 0), (1536, 512, 0)]
                    for si_, (c0, n, isctx) in enumerate(SEG):
                        if (not isctx) and c0 == 0:
                            P.op("dve", lambda h: h.tensor_scalar(out=upad[:, :, UPAD - 8:UPAD], in0=upad[:, :, UPAD - 8:UPAD], scalar1=halov[:, 0:1], scalar2=None, op0=ALU.mult),
                                 [t_uh, t_halov], [t_uh])
                            P.op("dve", lambda h: h.tensor_scalar(out=upad[:, :, UPAD + NTOK:UPAD + NTOK + 8], in0=upad[:, :, UPAD + NTOK:UPAD + NTOK + 8], scalar1=halov[:, 1:2], scalar2=None, op0=ALU.mult),
                                 [t_uh, t_halov], [t_uh])
                        U = upadc if isctx else upad
                        tus = [t_uc] if isctx else ([TU[c0 // 512]] + ([t_uh] if (c0 == 0 or c0 + 512 == NTOK) else []) + ([TU[c0 // 512 - 1]] if c0 > 0 else []) + ([TU[c0 // 512 + 1]] if c0 + 512 < NTOK else []))
                        fx = pfixc if isctx else pfix
                        seq_n = LC if isctx else NTOK
                        for ch in range(2):
                            (a, t_a), (b, t_b), (cbuf, t_c) = pt
                            base = UPAD + c0 - 16
                            W = n + 32
                            P.op("dve", lambda h, a=a, U=U, ch=ch, base=base, W=W: h.tensor_tensor(out=a[:, 1:W], in0=U[:, ch, base + 1:base + W], in1=U[:, ch, base:base + W - 1], op=ALU.add), tus, [t_a])
                            if ch == 0:
                                P.op("dve", lambda h, a=a, cbuf=cbuf: h.tensor_copy(out=cbuf[0:64, 0:n], in_=a[0:64, 16:16 + n]), [t_a], [t_c])
                                P.op("dve", lambda h, a=a, cbuf=cbuf: h.tensor_tensor(out=cbuf[64:128, 0:n], in0=a[64:128, 17:17 + n], in1=a[64:128, 15:15 + n], op=ALU.add), [t_a], [t_c])
                            else:
                                P.op("dve", lambda h, a=a, b=b, W=W: h.tensor_tensor(out=b[:, 3:W], in0=a[:, 3:W], in1=a[:, 1:W - 2], op=ALU.add), [t_a], [t_b])
                                P.op("dve", lambda h, a=a, b=b, W=W: h.tensor_tensor(out=a[:, 7:W], in0=b[:, 7:W], in1=b[:, 3:W - 4], op=ALU.add), [t_b, t_a], [t_a])
                                P.op("dve", lambda h, a=a, cbuf=cbuf: h.tensor_copy(out=cbuf[0:64, 0:n], in_=a[0:64, 19:19 + n]), [t_a], [t_c])
                                P.op("dve", lambda h, a=a, cbuf=cbuf: h.tensor_tensor(out=cbuf[64:128, 0:n], in0=a[64:128, 23:23 + n], in1=a[64:128, 15:15 + n], op=ALU.add), [t_a], [t_c])
                            if c0 == 0:
                                P.op("dve", lambda h, cbuf=cbuf, fx=fx, ch=ch: h.tensor_tensor(out=cbuf[:, 0:8], in0=cbuf[:, 0:8], in1=fx[:, ch, 0, :], op=ALU.mult), [t_c, t_pfix], [t_c])
                            if c0 + n == seq_n:
                                P.op("dve", lambda h, cbuf=cbuf, fx=fx, ch=ch: h.tensor_tensor(out=cbuf[:, n - 8:n], in0=cbuf[:, n - 8:n], in1=fx[:, ch, 1, :], op=ALU.mult), [t_c, t_pfix], [t_c])
                            wl, wh = (2, 4) if ch == 0 else (8, 16)
                            oc0 = NTOK if isctx else c0
                            tyc = cat_T([ch], oc0, n)
                            P.op("dve", lambda h, cbuf=cbuf, wl=wl: h.tensor_scalar(out=cbuf[0:64, 0:n], in0=cbuf[0:64, 0:n], scalar1=1.0 / wl, scalar2=None, op0=ALU.mult), [t_c], [t_c])
                            P.op("dve", lambda h, cbuf=cbuf, wh=wh: h.tensor_scalar(out=cbuf[64:128, 0:n], in0=cbuf[64:128, 0:n], scalar1=1.0 / wh, scalar2=None, op0=ALU.mult), [t_c], [t_c])
                            P.op("dve", lambda h, cbuf=cbuf, U=U, ch=ch, oc0=oc0: h.tensor_tensor(out=catT[:, ch, oc0:oc0 + n], in0=cbuf[:, 0:n], in1=U[:, ch, UPAD + c0:UPAD + c0 + n], op=ALU.subtract),
                                 [t_c] + tus, tyc)
                            deferred.append((ch, oc0, n))

                    ti = 0
                    for pi in range(2):
                        for jt in range(2):
                            accs = [next_bank(), next_bank()]
                            for ng_ in range(4):
                                tab, t_tab, dkt = tabs[ti % 2]
                                ti += 1
                                P.dma("sp", tab[:], I["dfttab"][pi, jt, ng_], [], [t_tab], dkt)
                                for nn in range(4):
                                    nch = ng_ * 4 + nn + 16 * pi
                                    for cs in range(2):
                                        for m in range(2):
                                            first = (ng_ == 0 and nn == 0 and cs == 0)
                                            lastmm = (ng_ == 3 and nn == 3 and cs == 1)
                                            P.op("pe", lambda h, m=m, nch=nch, cs=cs, tab=tab, nn=nn, first=first, lastmm=lastmm, acc=accs[m][0]:
                                                 h.matmul(acc, lhsT=AB[:, nch, cs * 256 + m * 128:cs * 256 + (m + 1) * 128], rhs=tab[:, nn, cs, :], start=first, stop=lastmm),
                                                 [t_AB[nch // 8], t_tab], [accs[m][1]], inc=(lastmm or (nn == 3 and cs == 1 and m == 1)))
                            for m in range(2):
                                P.op("act", lambda h, m=m, acc=accs[m][0], jt=jt, pi=pi: h.copy(out=catT[:, 6 + m, jt * 1024:(jt + 1) * 1024].rearrange("p (j two) -> p j two", two=2)[:, :, pi], in_=acc),
                                     [accs[m][1]], cat_T([6 + m], jt * 1024, 1024))
                    if not last:
                        accs = [next_bank(), next_bank()]
                        for nn in range(2):
                            for cs in range(2):
                                for m in range(2):
                                    first = (nn == 0 and cs == 0)
                                    lastmm = (nn == 1 and cs == 1)
                                    P.op("pe", lambda h, m=m, nn=nn, cs=cs, first=first, lastmm=lastmm, acc=accs[m][0]:
                                         h.matmul(acc[:, 0:256], lhsT=ABc[:, nn, cs * 256 + m * 128:cs * 256 + (m + 1) * 128], rhs=d256[:, nn, cs, :], start=first, stop=lastmm),
                                         [t_ABc, t_d256], [accs[m][1]], inc=lastmm)
                        for m in range(2):
                            P.op("act", lambda h, m=m, acc=accs[m][0]: h.copy(out=catT[:, 6 + m, NTOK:TOK], in_=acc[:, 0:256]), [accs[m][1]], cat_T([6 + m], NTOK, LC))
                    for (ch, oc0, n) in deferred:
                        pb, tpb = next_bank()
                        P.op("pe", lambda h, pb=pb, ch=ch, oc0=oc0, n=n: h.matmul(pb[:, 0:n], lhsT=pwbd[:, ch, :], rhs=catT[:, ch, oc0:oc0 + n], start=True, stop=True), cat_T([ch], oc0, n) + [t_pwbd], [tpb])
                        P.op("act", lambda h, pb=pb, ch=ch, oc0=oc0, n=n: h.activation(out=catT[:, ch, oc0:oc0 + n], in_=pb[:, 0:n], func=AF.Copy, scale=psc[:, ch:ch + 1]),
                             [tpb, t_psc], cat_T([ch], oc0, n))
                    P.barrier()
                if "catT" in DUMP and l == layers[-1] and stop in ("A3", "A2"):
                    pass
                if stop == "A3":
                    break

                rstds2 = Ring([(sbt(mxa, "rstd2_%d" % i, [128, 512], F32), T()) for i in range(2)])
                tmps2 = Ring([(sbt(mxa, "n2tmp%d" % i, [128, 512], F32), T()) for i in range(3)])
                G4 = [g for g in GROUPS if not (g[2] and last)]

                def emit_A4(c0, n, isctx):
                    for m in range(8):
                        pb, tpb = next_bank()
                        for k in range(8):
                            P.op("pe", lambda h, k=k, m=m, pb=pb: h.matmul(pb[:, 0:n], lhsT=wout[:, k, m * 128:(m + 1) * 128], rhs=catT[:, k, c0:c0 + n],
                                                                           start=(k == 0), stop=(k == 7)), [t_wout] + cat_T([k], c0, n), [tpb], inc=(k == 7))
                        P.op("dve", lambda h, m=m, pb=pb: h.scalar_tensor_tensor(out=x_ap(m, c0, n), in0=pb[:, 0:n], scalar=modv(l, 2, m, isctx), in1=x_ap(m, c0, n),
                                                                                 op0=ALU.mult, op1=ALU.add), [tpb, t_modl[l], x_T(m, c0)], [x_T(m, c0)])

                def emit_N2(c0, n, isctx):
                    norm_mod((rstds2, tmps2), l, 1, c0, n, isctx, lambda k, c0=c0, n=n: catT[:, k, c0:c0 + n], lambda k, c0=c0, n=n: cat_T([k], c0, n))
                for gi_, g_ in enumerate(G4):
                    emit_A4(*g_)
                    if gi_ >= 1:
                        emit_N2(*G4[gi_ - 1])
                emit_N2(*G4[-1])
                P.barrier()
            if stop in ("A1", "A2", "A3", "A4"):
                break

            MG = [g for g in GROUPS if not (g[2] and last)]
            ntile = sum(g[1] for g in MG) // 128
            h2T = catT
            with ExitStack() as st:
                gatesT = sbt(st, "gatesT", [16, TOK], BF16); t_gT = T()
                sel = sbt(st, "sel", [16, 16, 128], BF16); t_sel = T()
                P.dma("sp", sel[:], I["sel"], [], [t_sel], dk_misc)
                wg = [sbt(st, "wg%d" % i, [128, 8, 512], BF16) for i in range(2)]
                wu = [sbt(st, "wu%d" % i, [128, 8, 512], BF16) for i in range(2)]
                wd = [sbt(st, "wd%d" % i, [128, 4, D], BF16) for i in range(2)]
                t_wg, t_wu, t_wd = [T(), T()], [T(), T()], [T(), T()]
                def load_expert(e):
                    s = e % 2
                    P.dma("pool", wg[s][:], I["w_gate"][l, e].rearrange("(k p) c -> p k c", p=128), [], [t_wg[s]], dk_ex[s][0])
                    P.dma("pool", wu[s][:], I["w_up"][l, e].rearrange("(k p) c -> p k c", p=128), [], [t_wu[s]], dk_ex[s][1])
                    P.dma("pool", wd[s][:], I["w_down"][l, e].rearrange("(k p) c -> p k c", p=128), [], [t_wd[s]], dk_ex[s][2])
                load_expert(0)
                with ExitStack() as s2:
                    wr = sbt(s2, "wr", [128, 8, 20], BF16); t_wr = T()
                    brt = sbt(s2, "brt", [128, 20], F32); t_br = T()
                    P.dma("pool", wr[:], I["wr"][l].rearrange("(k p) c -> p k c", p=128), [], [t_wr], dk_misc)
                    P.dma("sp", brt[:], I["br"][l], [], [t_br], dk_misc)
                    pbr, tpbr = next_bank()
                    for tt in range(ntile):
                        for k in range(8):
                            P.op("pe", lambda h, tt=tt, k=k: h.matmul(pbr[:, tt * 20:(tt + 1) * 20], lhsT=h2T[:, k, tt * 128:(tt + 1) * 128], rhs=wr[:, k, :],
                                                                      start=(k == 0), stop=(k == 7)), [t_wr] + cat_T([k], tt * 128, 128), [tpbr], inc=(k == 7))
                    NT = ntile
                    rt = lambda nm, w: (sbt(s2, nm, [128, NT, w], F32), T())
                    Lg, t_L = rt("Lg", 20)
                    P.op("dve", lambda h: h.tensor_tensor(out=Lg[:], in0=pbr[:, 0:NT * 20].rearrange("p (t c) -> p t c", c=20), in1=brt[:].unsqueeze(1).to_broadcast([128, NT, 20]), op=ALU.add),
                         [tpbr, t_br], [t_L])
                    mg, t_mg = rt("mg", 1)
                    P.op("dve", lambda h: h.tensor_reduce(out=mg[:, :, 0], in_=Lg[:, :, 0:4], axis=AX.X, op=ALU.max), [t_L], [t_mg])
                    eg, t_eg = rt("eg", 4)
                    P.op("dve", lambda h: h.tensor_tensor(out=eg[:], in0=Lg[:, :, 0:4], in1=mg[:].to_broadcast([128, NT, 4]), op=ALU.subtract), [t_L, t_mg], [t_eg])
                    oh, t_oh = rt("oh", 4)
                    P.op("dve", lambda h: h.tensor_single_scalar(out=oh[:], in_=eg[:], scalar=0.0, op=ALU.is_ge), [t_eg], [t_oh])
                    P.op("act", lambda h: h.activation(out=eg[:], in_=eg[:], func=AF.Exp), [t_eg], [t_eg])
                    pg, t_pg = rt("pg", 1)
                    P.op("dve", lambda h: h.tensor_reduce(out=pg[:, :, 0], in_=eg[:], axis=AX.X, op=ALU.add), [t_eg], [t_pg])
                    P.op("dve", lambda h: h.reciprocal(out=pg[:], in_=pg[:]), [t_pg], [t_pg])
                    P.op("dve", lambda h: h.tensor_scalar(out=oh[:], in0=oh[:], scalar1=BIG, scalar2=-BIG, op0=ALU.mult, op1=ALU.add), [t_oh], [t_oh])
                    lm, t_lm = rt("lm", 16)
                    P.op("dve", lambda h: h.tensor_tensor(out=lm[:].rearrange("p t (g e) -> p t g e", g=4), in0=Lg[:, :, 4:20].rearrange("p t (g e) -> p t g e", g=4),
                                                          in1=oh[:].unsqueeze(3).to_broadcast([128, NT, 4, 4]), op=ALU.add), [t_L, t_oh], [t_lm])
                    m1_, t_m1_ = rt("m1_", 1)
                    P.op("dve", lambda h: h.tensor_reduce(out=m1_[:, :, 0], in_=lm[:], axis=AX.X, op=ALU.max), [t_lm], [t_m1_])
                    is1, t_is1 = rt("is1", 16)
                    P.op("dve", lambda h: h.tensor_tensor(out=is1[:], in0=lm[:], in1=m1_[:].to_broadcast([128, NT, 16]), op=ALU.is_ge), [t_lm, t_m1_], [t_is1])
                    lm2, t_lm2 = rt("lm2", 16)
                    P.op("dve", lambda h: h.scalar_tensor_tensor(out=lm2[:], in0=is1[:], scalar=-BIG, in1=lm[:], op0=ALU.mult, op1=ALU.add), [t_is1, t_lm], [t_lm2])
                    m2_, t_m2_ = rt("m2_", 1)
                    P.op("dve", lambda h: h.tensor_reduce(out=m2_[:, :, 0], in_=lm2[:], axis=AX.X, op=ALU.max), [t_lm2], [t_m2_])
                    selm, t_selm = rt("selm", 16)
                    P.op("dve", lambda h: h.tensor_tensor(out=selm[:], in0=lm[:], in1=m2_[:].to_broadcast([128, NT, 16]), op=ALU.is_ge), [t_lm, t_m2_], [t_selm])
                    P.op("dve", lambda h: h.tensor_tensor(out=lm2[:], in0=lm[:], in1=m1_[:].to_broadcast([128, NT, 16]), op=ALU.subtract), [t_lm, t_m1_, t_lm2], [t_lm2])
                    P.op("dve", lambda h: h.tensor_scalar(out=lm2[:], in0=lm2[:], scalar1=-80.0, scalar2=None, op0=ALU.max), [t_lm2], [t_lm2])
                    P.op("act", lambda h: h.activation(out=lm2[:], in_=lm2[:], func=AF.Exp), [t_lm2], [t_lm2])
                    P.op("dve", lambda h: h.tensor_tensor(out=lm2[:], in0=lm2[:], in1=selm[:], op=ALU.mult), [t_lm2, t_selm], [t_lm2])
                    den, t_den = rt("den", 1)
                    P.op("dve", lambda h: h.tensor_reduce(out=den[:, :, 0], in_=lm2[:], axis=AX.X, op=ALU.add), [t_lm2], [t_den])
                    P.op("dve", lambda h: h.reciprocal(out=den[:], in_=den[:]), [t_den], [t_den])
                    P.op("dve", lambda h: h.tensor_tensor(out=den[:], in0=den[:], in1=pg[:], op=ALU.mult), [t_den, t_pg], [t_den])
                    P.op("dve", lambda h: h.tensor_tensor(out=lm2[:], in0=lm2[:], in1=den[:].to_broadcast([128, NT, 16]), op=ALU.mult), [t_lm2, t_den], [t_lm2])
                    for tt in range(ntile):
                        pb, tpb = next_bank()
                        P.op("pe", lambda h, tt=tt, pb=pb: h.transpose(pb[0:16, 0:128], lm2[:, tt, :], ident32[:]), [t_lm2, t_const], [tpb])
                        P.op("act", lambda h, tt=tt, pb=pb: h.copy(out=gatesT[:, tt * 128:(tt + 1) * 128], in_=pb[0:16, 0:128]), [tpb], [t_gT])
                    P.barrier()
                if "gates" in DUMP:
                    P.dma("pool", DUMP["gates"], gatesT[:], [t_gT], [], dk_out)

                load_expert(1)
                aT = Ring([(sbt(st, "aT%d" % i, [128, 4, 512], BF16), T()) for i in range(2)])
                sgr = Ring([(sbt(st, "sg%d" % i, [128, 512], BF16), T()) for i in range(3)])
                sg2r = Ring([(sbt(st, "sg2_%d" % i, [128, 512], BF16), T()) for i in range(3)])
                gbr = Ring([(sbt(st, "gb%d" % i, [128, 512], BF16), T()) for i in range(2)])

                hide = HIDE_MOD1 and l == layers[0]
                if hide:
                    wm1 = sbt(st, "wm1", [128, 8, 1024], BF16); t_wm1 = T()
                    reserved.add(7)
                    mod_piece_dma(layers[1], 0, wm1, t_wm1)
                for e in range(16):
                    s = e % 2
                    if hide and e < 6:
                        mod_piece_mm(layers[1], e, wm1, t_wm1, bank(7), TP[7])
                        if e + 1 < 6:
                            mod_piece_dma(layers[1], e + 1, wm1, t_wm1)
                        else:
                            mod_finalize(layers[1], bank(7), TP[7])
                            reserved.discard(7)
                    astate = {}

                    def emit_GU(gi):
                        c0, n, isctx = MG[gi]
                        pbg, tpbg = next_bank()
                        P.op("pe", lambda h, e=e, pbg=pbg: h.matmul(pbg[:, 0:n], lhsT=sel[:, e, :], rhs=gatesT[:, c0:c0 + n], start=True, stop=True), [t_gT, t_sel], [tpbg])
                        gb, t_gb = gbr.next()
                        P.op("act", lambda h, gb=gb, pbg=pbg: h.copy(out=gb[:, 0:n], in_=pbg[:, 0:n]), [tpbg], [t_gb])
                        a, t_a = aT.next()
                        for dc in range(4):
                            pg_, tpg_ = next_bank()
                            for k in range(8):
                                P.op("pe", lambda h, k=k, dc=dc, pg_=pg_, s=s: h.matmul(pg_[:, 0:n], lhsT=wg[s][:, k, dc * 128:(dc + 1) * 128], rhs=h2T[:, k, c0:c0 + n],
                                                                                        start=(k == 0), stop=(k == 7)), [t_wg[s]] + cat_T([k], c0, n), [tpg_], inc=(k == 7))
                            pu_, tpu_ = next_bank()
                            for k in range(8):
                                P.op("pe", lambda h, k=k, dc=dc, pu_=pu_, s=s: h.matmul(pu_[:, 0:n], lhsT=wu[s][:, k, dc * 128:(dc + 1) * 128], rhs=h2T[:, k, c0:c0 + n],
                                                                                        start=(k == 0), stop=(k == 7)), [t_wu[s]] + cat_T([k], c0, n), [tpu_], inc=(k == 7))
                            sg, t_sg = sgr.next()
                            P.op("act", lambda h, sg=sg, pg_=pg_: h.activation(out=sg[:, 0:n], in_=pg_[:, 0:n], func=AF.Silu), [tpg_], [t_sg])
                            sg2, t_sg2 = sg2r.next()
                            P.op("pool", lambda h, sg=sg, sg2=sg2, gb=gb: h.tensor_tensor(out=sg2[:, 0:n], in0=sg[:, 0:n], in1=gb[:, 0:n], op=ALU.mult), [t_sg, t_gb], [t_sg2])
                            P.op("dve", lambda h, a=a, dc=dc, pu_=pu_, sg2=sg2: h.tensor_tensor(out=a[:, dc, 0:n], in0=pu_[:, 0:n], in1=sg2[:, 0:n], op=ALU.mult), [tpu_, t_sg2], [t_a])
                        astate[gi] = (a, t_a)

                    def emit_DOWN(gi):
                        c0, n, isctx = MG[gi]
                        a, t_a = astate.pop(gi)
                        for m in range(8):
                            py, tpy = next_bank()
                            for dc in range(4):
                                P.op("pe", lambda h, dc=dc, m=m, py=py, a=a, s=s: h.matmul(py[:, 0:n], lhsT=wd[s][:, dc, m * 128:(m + 1) * 128], rhs=a[:, dc, 0:n],
                                                                                           start=(dc == 0), stop=(dc == 3)), [t_wd[s], t_a], [tpy], inc=(dc == 3))
                            P.op("dve", lambda h, m=m, py=py: h.scalar_tensor_tensor(out=x_ap(m, c0, n), in0=py[:, 0:n], scalar=modv(l, 5, m, isctx), in1=x_ap(m, c0, n),
                                                                                     op0=ALU.mult, op1=ALU.add), [tpy, t_modl[l], x_T(m, c0)], [x_T(m, c0)])
                    emit_GU(0)
                    for gi in range(len(MG)):
                        if gi + 1 < len(MG):
                            emit_GU(gi + 1)
                        emit_DOWN(gi)
                    if e + 2 < 16:
                        load_expert(e + 2)
                P.barrier()
            if stop == "B%d" % l:
                break

        if "xT" in DUMP:
            for m in range(8):
                P.dma("sp", DUMP["xT"][m * 128:(m + 1) * 128, :], xT[:, m, :], TX[m], [], dk_out)
        if "xcT" in DUMP:
            for m in range(8):
                P.dma("sp", DUMP["xcT"][m * 128:(m + 1) * 128, :], xcT[:, m, :], [TXC[m]], [], dk_out)
        if "catT" in DUMP:
            for k in range(8):
                P.dma("pool", DUMP["catT"][k * 128:(k + 1) * 128, :], catT[:, k, :], TC[k], [], dk_out)
        for m in range(8):
            P.dma("sp", outT[m * 128:(m + 1) * 128, :], xT[:, m, :], TX[m], [], dk_out)
        P.barrier()
        P.emit()
    return nc


_CACHE = {}


def kernel(**inputs):
    in_maps = host_prep(**inputs)
    if "nc" not in _CACHE:
        _CACHE["nc"] = build()
    nc = _CACHE["nc"]
    res = run_bass_kernel_spmd(nc, in_maps, core_ids=list(range(8)))
    out = np.empty((4, 4096, D), np.float32)
    for core in range(8):
        b, par = core // 2, core % 2
        out[b, par * NTOK:(par + 1) * NTOK, :] = res.results[core]["outT"].T
    return out
```

```python
import types
import numpy as np
import ml_dtypes
from contextlib import ExitStack
import concourse.bass as bass
import concourse.mybir as mybir
from concourse.bass_utils import run_bass_kernel_spmd

F32 = mybir.dt.float32
BF16 = mybir.dt.bfloat16
AF = mybir.ActivationFunctionType
ALU = mybir.AluOpType
AX = mybir.AxisListType

D = 1024
NTOK = 2048
LC = 256
TOK = NTOK + LC
NB = 20
UPAD = 16
EPS = 1e-6
BIG = 30000.0
SAME_SYNC = True


class T:
    __slots__ = ("name", "w", "r")

    def __init__(self, name=""):
        self.name = name
        self.w = None
        self.r = []


def _freeze(fn):
    if fn.__closure__ is None:
        return fn
    cells = []
    for c in fn.__closure__:
        try:
            cells.append(types.CellType(c.cell_contents))
        except ValueError:
            cells.append(c)
    return types.FunctionType(fn.__code__, fn.__globals__, fn.__name__, fn.__defaults__, tuple(cells))


class Prog:
    ENG = ("pe", "act", "dve", "pool", "sp")

    def __init__(self, nc, stack, same_engine_sync=True):
        self.nc = nc
        self.stack = stack
        self.same = same_engine_sync
        self.sems = {}
        self.cnt = {}
        for e in self.ENG:
            self.sems[e] = stack.enter_context(nc.semaphore("s_" + e))
            self.cnt[e] = 0
        self.ops = {e: [] for e in self.ENG}
        self.waited = {e: {} for e in self.ENG}
        self.pending_silent = {e: False for e in self.ENG}
        self.ndsem = 0
        self.dkeys = []

    def dsem(self):
        k = "d%d" % self.ndsem
        self.ndsem += 1
        self.sems[k] = self.stack.enter_context(self.nc.semaphore("sd_" + k))
        self.cnt[k] = 0
        self.dkeys.append(k)
        return k

    def _need(self, eng, dep):
        if dep is None:
            return
        k, v = dep
        if k == eng:
            if not self.same or v > self.cnt[eng]:
                return
        if self.waited[eng].get(k, 0) >= v:
            return
        self.waited[eng][k] = v
        sem = self.sems[k]
        self.ops[eng].append(lambda h, sem=sem, v=v: h.wait_ge(sem, v))

    def _deps(self, eng, reads, writes):
        for t in reads:
            self._need(eng, t.w)
        for t in writes:
            self._need(eng, t.w)
            for d in t.r:
                self._need(eng, d)

    def _mark(self, tok, reads, writes):
        for t in reads:
            t.r.append(tok)
        for t in writes:
            t.w = tok
            t.r = []

    def op(self, eng, fn, reads=(), writes=(), inc=True):
        fn = _freeze(fn)
        self._deps(eng, reads, writes)
        seq = self.cnt[eng] + 1
        if inc:
            self.cnt[eng] = seq
            sem = self.sems[eng]
            self.ops[eng].append(lambda h, fn=fn, sem=sem: fn(h).then_inc(sem, 1))
            self.pending_silent[eng] = False
        else:
            self.ops[eng].append(lambda h, fn=fn: fn(h))
            self.pending_silent[eng] = True
        self._mark((eng, seq), reads, writes)

    def dma(self, q, out, in_, reads, writes, dk=None):
        if not hasattr(self, "dpool"):
            self.dpool = [self.dsem() for _ in range(40)]
            self.dnext = 0
        dk = self.dpool[self.dnext % len(self.dpool)]
        self.dnext += 1
        if self.cnt[dk] > 0:
            self._need(q, (dk, self.cnt[dk]))
        self._deps(q, reads, writes)
        self.cnt[dk] += 16
        sem = self.sems[dk]
        self.ops[q].append(lambda h, out=out, in_=in_, sem=sem: h.dma_start(out=out, in_=in_).then_inc(sem, 16))
        self._mark((dk, self.cnt[dk]), reads, writes)

    def collective(self, kind, groups, in_ap, out_ap, tin, tout, dk):
        q = "pool"
        if self.cnt[dk] > 0:
            self._need(q, (dk, self.cnt[dk]))
        self._deps(q, [tin], [tout])
        self.cnt[dk] += 1
        sem = self.sems[dk]
        self.ops[q].append(lambda h: h.collective_compute(kind, ALU.bypass, replica_groups=groups,
                                                          ins=[in_ap], outs=[out_ap]).then_inc(sem, 1))
        self._mark((dk, self.cnt[dk]), [tin], [tout])

    def barrier(self, exclude=()):
        for e in self.ENG:
            assert not self.pending_silent[e]
        keys = [k for k in list(self.ENG) + self.dkeys if k not in exclude]
        for e in self.ENG:
            for k in keys:
                if k != e and self.cnt[k] > 0:
                    self._need(e, (k, self.cnt[k]))

    def emit(self):
        for e in self.ENG:
            assert not self.pending_silent[e], "engine %s ends with silent op" % e
        nc = self.nc
        with nc.Block() as block:
            for e, deco in (("pe", block.tensor), ("act", block.scalar), ("dve", block.vector),
                            ("pool", block.gpsimd), ("sp", block.sync)):
                ops = self.ops[e]

                def body(h, ops=ops):
                    for f in ops:
                        f(h)
                deco(body)


class Ring:
    def __init__(self, items):
        self.items = items
        self.i = 0

    def next(self):
        it = self.items[self.i % len(self.items)]
        self.i += 1
        return it


def _bf(a):
    return np.ascontiguousarray(a.astype(ml_dtypes.bfloat16))


def _consts(par):
    c = {}
    pos = np.arange(NTOK) + NTOK * par
    r = (pos // 64).astype(np.float32)
    col = (pos % 64).astype(np.float32)
    half = 32
    inv = (1.0 / (np.float32(10000.0) ** (np.arange(0, half, 2, dtype=np.float32) / np.float32(half)))).astype(np.float32)
    ar = r[:, None] * inv
    ac = col[:, None] * inv
    ang = np.concatenate([ar, ar, ac, ac], axis=-1).astype(np.float32)
    cos = np.cos(ang).astype(np.float32).T
    sin = np.sin(ang).astype(np.float32).T
    cosT = np.ones((128, TOK), np.float32)
    sinT = np.zeros((128, TOK), np.float32)
    cosT[0:64, :NTOK] = cos; cosT[64:128, :NTOK] = cos
    sinT[0:64, :NTOK] = sin; sinT[64:128, :NTOK] = sin
    c["ropeC"] = _bf(cosT); c["ropeS"] = _bf(sinT)
    R = np.zeros((64, 64), np.float32)
    for i in range(16):
        R[i, i + 16] = -1.0
        R[i + 16, i] = 1.0
        R[i + 32, i + 48] = -1.0
        R[i + 48, i + 32] = 1.0
    R2 = np.zeros((128, 128), np.float32)
    R2[0:64, 0:64] = R; R2[64:, 64:] = R
    c["rotm"] = _bf(R2.T)
    hs = np.zeros((128, 128), np.float32)
    hs[0:64, 0:64] = 1.0 / 64; hs[64:, 64:] = 1.0 / 64
    c["hsum"] = _bf(hs)
    c["ones128"] = _bf(np.ones((128, 128), np.float32))
    c["ident32"] = np.eye(128, dtype=np.float32)
    kk = np.arange(128)[:, None]; qq = np.arange(128)[None, :]
    mprev = (kk >= qq).astype(np.float32)
    mnext = (kk <= qq).astype(np.float32)
    masks = np.zeros((128, 4, 128), np.float32)
    masks[:, 0] = mprev; masks[:, 1] = mnext
    masks[:, 2] = mprev if par == 1 else 0.0
    masks[:, 3] = mnext if par == 0 else 0.0
    c["masks"] = _bf(masks)
    wins = {(0, 0): 2, (0, 1): 4, (1, 0): 8, (1, 1): 16}
    fix = np.ones((128, 2, 2, 8), np.float32)
    Nn = 4096
    for (ch, hf), w in wins.items():
        rows = slice(hf * 64, hf * 64 + 64)
        for j in range(8):
            t = j
            lo = max(t - w // 2, 0); hi = min(t + w // 2 - 1, Nn - 1)
            fix[rows, ch, 0, j] = w / (hi - lo + 1)
            t = Nn - 8 + j
            lo = max(t - w // 2, 0); hi = min(t + w // 2 - 1, Nn - 1)
            fix[rows, ch, 1, j] = w / (hi - lo + 1)
    c["poolfixc"] = fix.copy()
    f2 = fix.copy()
    if par == 0:
        f2[:, :, 1, :] = 1.0
    else:
        f2[:, :, 0, :] = 1.0
    c["poolfix"] = f2
    hv = np.zeros((128, 2), np.float32)
    hv[:, 0] = 1.0 if par == 1 else 0.0
    hv[:, 1] = 1.0 if par == 0 else 0.0
    c["halov"] = hv
    n = np.arange(2048, dtype=np.int64)[:, None]
    k = (np.arange(NTOK, dtype=np.int64) + NTOK * par)[None, :]
    ph = ((n * k) % 4096).astype(np.float64) * (2.0 * np.pi / 4096.0)
    tab = np.stack([np.cos(ph), -np.sin(ph)], axis=1)
    tab = tab.reshape(4, 4, 128, 2, 2, 512, 2)
    tab = tab.transpose(6, 4, 0, 2, 1, 3, 5)
    c["dfttab"] = _bf(tab)
    n = np.arange(256, dtype=np.int64)[:, None]; k = np.arange(256, dtype=np.int64)[None, :]
    ph = ((n * k) % 256).astype(np.float64) * (2.0 * np.pi / 256.0)
    t2 = np.stack([np.cos(ph), -np.sin(ph)], axis=1)
    t2 = 4.0 * t2.reshape(2, 128, 2, 256).transpose(1, 0, 2, 3)
    c["dft256"] = _bf(t2)
    cc = np.arange(64, dtype=np.int64)
    ph = ((cc[:, None] * cc[None, :]) % 64).astype(np.float64) * (2.0 * np.pi / 64.0)
    d64 = np.stack([np.cos(ph) / 512.0, np.sin(ph) / 512.0], axis=1)
    c["dft64"] = d64.astype(np.float32)
    sel = np.zeros((16, 16, 128), np.float32)
    for e in range(16):
        sel[e, e, :] = 1.0
    c["sel"] = _bf(sel)
    return c


def host_prep(x, c, ctx, c_ctx, w_mod, b_mod, norm1_g, w_in, q_norm_g, k_norm_g, attn_sink,
              pool_w, pool_scale, four_w, w_out, norm2_g, w_grp, b_grp, w_rtr, b_rtr,
              w_gate, w_up, w_down):
    f = lambda a: np.ascontiguousarray(np.asarray(a, dtype=np.float32))
    x, c, ctx, c_ctx = f(x), f(c), f(ctx), f(c_ctx)
    w_in = f(w_in); w_out = f(w_out)
    sh = {}
    sh["w_mod"] = f(w_mod)
    sh["bmodT"] = f(np.asarray(b_mod).reshape(2, 48, 128).transpose(0, 2, 1))
    sh["n1g"] = f(np.asarray(norm1_g).reshape(2, 8, 128).transpose(0, 2, 1))
    sh["n2g"] = f(np.asarray(norm2_g).reshape(2, 8, 128).transpose(0, 2, 1))
    qcols = []
    for g in range(4):
        qcols += list(range(256 + g * 64, 256 + g * 64 + 64)) + list(range(256 + (4 + g) * 64, 256 + (4 + g) * 64 + 64))
    cols = list(range(0, 256)) + qcols + list(range(768, 896)) + list(range(896, 1024))
    sh["w_inp"] = f(w_in[:, :, cols])
    sh["w_inFT"] = f(w_in[:, :, 1024:1280].reshape(2, 1024, 4, 64).transpose(0, 3, 2, 1))
    sh["four_w2"] = f(np.asarray(four_w).transpose(0, 2, 1, 3))
    sh["qg"] = f(np.tile(np.asarray(q_norm_g), (1, 2))[:, :, None])
    sh["kg"] = f(np.tile(np.asarray(k_norm_g), (1, 2))[:, :, None])
    sh["sinkb"] = f(np.broadcast_to(np.asarray(attn_sink).reshape(2, 1, 2, 4), (2, 128, 2, 4)))
    pw = np.asarray(pool_w, dtype=np.float32)
    bd = np.zeros((2, 2, 128, 128), np.float32)
    for l in range(2):
        for ch in range(2):
            bd[l, ch, 0:64, 0:64] = pw[l, 2 * ch]
            bd[l, ch, 64:, 64:] = pw[l, 2 * ch + 1]
    sh["poolw_bd"] = bd
    sh["pscale"] = f(np.asarray(pool_scale).reshape(2, 2, 128).transpose(0, 2, 1))
    arows = []
    for g in range(4):
        arows += list(range(256 + g * 64, 256 + g * 64 + 64)) + list(range(256 + (4 + g) * 64, 256 + (4 + g) * 64 + 64))
    rows = list(range(0, 256)) + arows + list(range(768, 1024))
    sh["w_outp"] = f(w_out[:, rows, :])
    sh["wr"] = f(np.concatenate([np.asarray(w_grp), np.asarray(w_rtr)], axis=-1))
    br = np.concatenate([np.asarray(b_grp), np.asarray(b_rtr)], axis=-1)
    sh["br"] = f(np.broadcast_to(br[:, None, :], (2, 128, 20)))
    sh["w_gate"] = f(w_gate); sh["w_up"] = f(w_up); sh["w_down"] = f(w_down)
    cons = [_consts(0), _consts(1)]
    in_maps = []
    for core in range(8):
        b, par = core // 2, core % 2
        m = dict(sh)
        m.update(cons[par])
        m["xT"] = f(x[b, par * NTOK:(par + 1) * NTOK, :].T)
        m["ctxT"] = f(ctx[b].T)
        m["sT"] = f(np.stack([c[b], c_ctx], axis=1))
        in_maps.append(m)
    return in_maps


IN_SPECS = [
    ("xT", [D, NTOK], F32), ("ctxT", [D, LC], F32), ("sT", [D, 2], F32),
    ("w_mod", [2, D, 6 * D], F32), ("bmodT", [2, 128, 48], F32), ("n1g", [2, 128, 8], F32), ("n2g", [2, 128, 8], F32),
    ("w_inp", [2, D, 1024], F32), ("w_inFT", [2, 64, 4, D], F32), ("four_w2", [2, 64, 4, 64], F32),
    ("qg", [2, 128, 1], F32), ("kg", [2, 128, 1], F32), ("sinkb", [2, 128, 2, 4], F32),
    ("poolw_bd", [2, 2, 128, 128], F32), ("pscale", [2, 128, 2], F32), ("w_outp", [2, D, D], F32),
    ("wr", [2, D, 20], F32), ("br", [2, 128, 20], F32),
    ("w_gate", [2, 16, D, 512], F32), ("w_up", [2, 16, D, 512], F32), ("w_down", [2, 16, 512, D], F32),
    ("ropeC", [128, TOK], BF16), ("ropeS", [128, TOK], BF16), ("rotm", [128, 128], BF16), ("hsum", [128, 128], BF16),
    ("ones128", [128, 128], BF16), ("ident32", [128, 128], F32), ("masks", [128, 4, 128], BF16),
    ("poolfix", [128, 2, 2, 8], F32), ("poolfixc", [128, 2, 2, 8], F32), ("halov", [128, 2], F32),
    ("dfttab", [2, 2, 4, 128, 4, 2, 512], BF16), ("dft256", [128, 2, 2, 256], BF16), ("dft64", [64, 2, 64], F32),
    ("sel", [16, 16, 128], BF16),
]


def build(layers=(0, 1), stop=None, dumps=()):
    nc = bass.Bass("TRN2", target_bir_lowering=False)
    I = {}
    for name, shape, dt in IN_SPECS:
        I[name] = nc.dram_tensor(name, shape, dt, kind="ExternalInput").ap()
    outT = nc.dram_tensor("outT", [D, NTOK], F32, kind="ExternalOutput").ap()
    DUMP = {}
    dump_specs = {"xT": [D, NTOK], "xcT": [D, LC], "catT": [D, TOK], "mod": [128, 2 * 48 * 2], "kz": [128, 2 * NB * 128],
                  "vaug": [128, NB * 256], "gates": [16, TOK]}
    for dn in dumps:
        DUMP[dn] = nc.dram_tensor("dump_" + dn, dump_specs[dn], F32, kind="ExternalOutput").ap()
    gin_ab = nc.dram_tensor("gin_ab", [NTOK, 512], BF16).ap()
    gout_ab = nc.dram_tensor("gout_ab", [2 * NTOK, 512], BF16).ap()
    gin_h = nc.dram_tensor("gin_h", [256, 512], BF16).ap()
    gout_h = nc.dram_tensor("gout_h", [512, 512], BF16).ap()
    t_gin_ab, t_gout_ab, t_gin_h, t_gout_h = T("gin_ab"), T("gout_ab"), T("gin_h"), T("gout_h")
    PAIRS = [[0, 1], [2, 3], [4, 5], [6, 7]]

    with ExitStack() as top:
        P = Prog(nc, top, same_engine_sync=SAME_SYNC)

        uid = [0]

        def sbt(st, name, shape, dt):
            uid[0] += 1
            return st.enter_context(nc.sbuf_tensor("sb%d_%s" % (uid[0], name), shape, dt))

        xT = sbt(top, "xT", [128, 8, NTOK], F32)
        xcT = sbt(top, "xcT", [128, 8, LC], F32)
        catT = sbt(top, "catT", [128, 8, TOK], BF16)
        modT = sbt(top, "modT", [128, 2, 48, 2], F32)
        A1s = sbt(top, "A1s", [128, 2, 8, 2], F32)
        A2s = sbt(top, "A2s", [128, 2, 8, 2], F32)
        rotm = sbt(top, "rotm", [128, 128], BF16)
        hsum = sbt(top, "hsum", [128, 128], BF16)
        ones128 = sbt(top, "ones128", [128, 128], BF16)
        ident32 = sbt(top, "ident32", [128, 128], F32)
        epsb = sbt(top, "epsb", [128, 1], F32)
        TX = [[T("x%d_%d" % (m, g)) for g in range(4)] for m in range(8)]
        TXC = [T("xc%d" % m) for m in range(8)]
        TC = [[T("c%d_%d" % (k, b)) for b in range(18)] for k in range(8)]
        t_modl = {0: T("mod0"), 1: T("mod1")}
        t_Al = {0: T("A0"), 1: T("A1")}
        t_const = T("const")
        PSA = top.enter_context(nc.psum_tensor("PSA", [128, 2048], F32))
        PSB = top.enter_context(nc.psum_tensor("PSB", [128, 2048], F32))
        TP = [T("bank%d" % i) for i in range(8)]

        def bank(i):
            big = PSA if i < 4 else PSB
            j = i % 4
            return big[:, j * 512:(j + 1) * 512]
        bank_rr = [0]

        reserved = set()

        def next_bank():
            while True:
                i = bank_rr[0] % 8
                bank_rr[0] += 1
                if i not in reserved:
                    return bank(i), TP[i]

        dk_const = P.dsem()
        dk_x = P.dsem()
        dk_w = [P.dsem() for _ in range(4)]
        dk_misc = P.dsem()
        dk_out = P.dsem()
        dk_gw = P.dsem()
        dk_cc = P.dsem()
        dk_cc2 = P.dsem()
        dk_gr = P.dsem()
        dk_tab = [P.dsem() for _ in range(3)]
        dk_ex = [[P.dsem() for _ in range(3)] for _ in range(2)]

        for nm, tl in (("rotm", rotm), ("hsum", hsum), ("ones128", ones128), ("ident32", ident32)):
            P.dma("sp", tl[:], I[nm], [], [t_const], dk_const)
        P.op("pool", lambda h: h.memset(epsb[:], EPS), [], [t_const])
        for g in range(4):
            for m in range(8):
                P.dma("sp", xT[:, m, g * 512:(g + 1) * 512], I["xT"][m * 128:(m + 1) * 128, g * 512:(g + 1) * 512], [], [TX[m][g]], dk_x)
        for m in range(8):
            P.dma("sp", xcT[:, m, :], I["ctxT"][m * 128:(m + 1) * 128, :], [], [TXC[m]], dk_x)

        def cat_T(ks, c0, n):
            return [TC[k][b] for k in ks for b in range(c0 // 128, (c0 + n + 127) // 128)]

        s32 = sbt(top, "s32", [128, 8, 2], F32); t_s32 = T()
        sbf = sbt(top, "sbf", [128, 8, 2], BF16); t_sbf = T()
        bmod = sbt(top, "bmod", [128, 2, 48], F32); t_bmod = T()
        ngam = sbt(top, "ngam", [128, 2, 2, 8], F32); t_ng = T()
        P.dma("sp", s32[:], I["sT"].rearrange("(k p) c -> p k c", p=128), [], [t_s32], dk_misc)
        P.dma("sp", bmod[:], I["bmodT"].rearrange("l p n -> p l n"), [], [t_bmod], dk_misc)
        P.dma("sp", ngam[:, 0], I["n1g"].rearrange("l p k -> p l k"), [], [t_ng], dk_misc)
        P.dma("sp", ngam[:, 1], I["n2g"].rearrange("l p k -> p l k"), [], [t_ng], dk_misc)
        P.op("act", lambda h: h.activation(out=sbf[:], in_=s32[:], func=AF.Silu), [t_s32], [t_sbf])

        def mod_piece_dma(l, v, wmt, t_wmt):
            P.dma("pool", wmt[:], I["w_mod"][l, :, v * 1024:(v + 1) * 1024].rearrange("(k p) c -> p k c", p=128), [], [t_wmt], dk_misc)

        def mod_piece_mm(l, v, wmt, t_wmt, pb, tpb):
            for c8 in range(8):
                n = v * 8 + c8
                for k in range(8):
                    P.op("pe", lambda h, o=pb[:, 2 * n:2 * n + 2], w=wmt[:, k, c8 * 128:(c8 + 1) * 128], r=sbf[:, k, :], k=k:
                         h.matmul(o, lhsT=w, rhs=r, start=(k == 0), stop=(k == 7)),
                         [t_wmt, t_sbf], [tpb], inc=(k == 7))

        def mod_finalize(l, pb, tpb):
            P.op("dve", lambda h, l=l, pb=pb: h.tensor_tensor(out=modT[:, l], in0=pb[:, 0:96].rearrange("p (n c) -> p n c", c=2),
                                                              in1=bmod[:, l].unsqueeze(2).to_broadcast([128, 48, 2]), op=ALU.add),
                 [tpb, t_bmod], [t_modl[l]])
            P.op("dve", lambda h, l=l: h.scalar_tensor_tensor(out=A1s[:, l], in0=modT[:, l, 8:16, :], scalar=1.0,
                                                              in1=ngam[:, 0, l].unsqueeze(2).to_broadcast([128, 8, 2]),
                                                              op0=ALU.add, op1=ALU.mult), [t_modl[l], t_ng], [t_Al[l]])
            P.op("dve", lambda h, l=l: h.scalar_tensor_tensor(out=A2s[:, l], in0=modT[:, l, 32:40, :], scalar=1.0,
                                                              in1=ngam[:, 1, l].unsqueeze(2).to_broadcast([128, 8, 2]),
                                                              op0=ALU.add, op1=ALU.mult), [t_modl[l], t_ng], [t_Al[l]])

        def mod_finalize_part(l, pb, tpb, v0, v1):
            P.op("dve", lambda h, l=l, pb=pb: h.tensor_tensor(out=modT[:, l, v0 * 8:v1 * 8, :], in0=pb[:, v0 * 16:v1 * 16].rearrange("p (n c) -> p n c", c=2),
                                                              in1=bmod[:, l, v0 * 8:v1 * 8].unsqueeze(2).to_broadcast([128, (v1 - v0) * 8, 2]), op=ALU.add),
                 [tpb, t_bmod], [t_modl[l]])
            if v0 <= 1 < v1:
                P.op("dve", lambda h, l=l: h.scalar_tensor_tensor(out=A1s[:, l], in0=modT[:, l, 8:16, :], scalar=1.0,
                                                                  in1=ngam[:, 0, l].unsqueeze(2).to_broadcast([128, 8, 2]),
                                                                  op0=ALU.add, op1=ALU.mult), [t_modl[l], t_ng], [t_Al[l]])
            if v0 <= 4 < v1:
                P.op("dve", lambda h, l=l: h.scalar_tensor_tensor(out=A2s[:, l], in0=modT[:, l, 32:40, :], scalar=1.0,
                                                                  in1=ngam[:, 1, l].unsqueeze(2).to_broadcast([128, 8, 2]),
                                                                  op0=ALU.add, op1=ALU.mult), [t_modl[l], t_ng], [t_Al[l]])

        HIDE_MOD1 = (len(layers) == 2)
        with ExitStack() as st:
            wm = [sbt(st, "wm%d" % i, [128, 8, 1024], BF16) for i in range(2)]
            t_wm = [T(), T()]
            it = 0
            for l in (layers[:1] if HIDE_MOD1 else layers):
                pb, tpb = next_bank()
                for v in range(2 if HIDE_MOD1 else 6):
                    slot = it % 2
                    it += 1
                    mod_piece_dma(l, v, wm[slot], t_wm[slot])
                    mod_piece_mm(l, v, wm[slot], t_wm[slot], pb, tpb)
                if HIDE_MOD1:
                    mod_finalize_part(l, pb, tpb, 0, 2)
                else:
                    mod_finalize(l, pb, tpb)
            P.barrier()
        if "mod" in DUMP:
            P.dma("sp", DUMP["mod"], modT[:].rearrange("p l n c -> p (l n c)"), [t_modl[0], t_modl[1]], [], dk_out)

        def modv(l, v, k, c):
            return modT[:, l, v * 8 + k, c:c + 1]

        GROUPS = [(g * 512, 512, 0) for g in range(4)] + [(NTOK, LC, 1)]

        def x_ap(k, c0, n):
            if c0 < NTOK:
                return xT[:, k, c0:c0 + n]
            return xcT[:, k, c0 - NTOK:c0 - NTOK + n]

        def x_T(k, c0):
            if c0 < NTOK:
                return TX[k][c0 // 512]
            return TXC[k]

        def norm_mod(st_tiles, l, which, c0, n, isctx, dst, t_dst):
            rstds, tmps = st_tiles
            rstd, t_rstd = rstds.next()
            As = A1s if which == 0 else A2s
            shv = 0 if which == 0 else 3
            pb, tpb = next_bank()
            for k in range(8):
                P.op("act", lambda h, k=k: h.activation(out=dst(k), in_=x_ap(k, c0, n), func=AF.Square),
                     [x_T(k, c0)], t_dst(k))
            for k in range(8):
                P.op("pe", lambda h, k=k: h.matmul(pb[:, 0:n], lhsT=ones128[:], rhs=dst(k), start=(k == 0), stop=(k == 7)),
                     t_dst(k) + [t_const], [tpb], inc=(k == 7))
            P.op("act", lambda h: h.activation(out=rstd[:, 0:n], in_=pb[:, 0:n], func=AF.Ln, bias=epsb[:, 0:1], scale=1.0 / D), [tpb, t_const], [t_rstd])
            P.op("act", lambda h: h.activation(out=rstd[:, 0:n], in_=rstd[:, 0:n], func=AF.Exp, scale=-0.5), [t_rstd], [t_rstd])
            for k in range(8):
                tmp, t_tmp = tmps.next()
                P.op("dve", lambda h, k=k, tmp=tmp: h.scalar_tensor_tensor(out=tmp[:, 0:n], in0=x_ap(k, c0, n), scalar=As[:, l, k, isctx:isctx + 1],
                                                                          in1=rstd[:, 0:n], op0=ALU.mult, op1=ALU.mult),
                     [x_T(k, c0), t_Al[l], t_rstd] + t_dst(k), [t_tmp])
                P.op("act", lambda h, k=k, tmp=tmp: h.activation(out=dst(k), in_=tmp[:, 0:n], func=AF.Identity,
                                                                 bias=modv(l, shv, k, isctx), scale=1.0),
                     [t_tmp, t_modl[l]], t_dst(k))

        for l in layers:
            last = (l == 1)
            with ExitStack() as mx:
                ABc = sbt(mx, "ABc", [128, 2, 512], BF16)
                qkg = sbt(mx, "qkg", [128, 2], F32)
                esink = sbt(mx, "esink", [128, 2, 4], F32)
                mxa = mx.enter_context(ExitStack())
                upad = sbt(mxa, "upad", [128, 2, UPAD + NTOK + UPAD], BF16)
                upadc = sbt(mxa, "upadc", [128, 2, UPAD + LC + UPAD], BF16)
                mxb = mxa.enter_context(ExitStack())
                Kz = [sbt(mxb, "Kz%d" % i, [128, NB * 128], BF16) for i in range(2)]
                Vaug = sbt(mxb, "Vaug", [128, NB, 2, 128], BF16)
                ropeC = sbt(mxb, "ropeC", [128, TOK], BF16)
                ropeS = sbt(mxb, "ropeS", [128, TOK], BF16)
                masks = sbt(mxb, "masks", [128, 4, 128], BF16)
                TK = [T("k%d" % b) for b in range(NB)]
                TV = [T("v%d" % b) for b in range(NB)]
                t_rope, t_masks, t_qkg, t_esink, t_ABc = T(), T(), T(), T(), T()
                TU = [T("u%d" % g) for g in range(4)]
                t_uh, t_uc = T("uh"), T("uc")
                P.dma("sp", ropeC[:], I["ropeC"], [], [t_rope], dk_misc)
                P.dma("sp", ropeS[:], I["ropeS"], [], [t_rope], dk_misc)
                P.dma("sp", masks[:], I["masks"], [], [t_masks], dk_misc)
                P.dma("sp", qkg[:, 0:1], I["qg"][l], [], [t_qkg], dk_misc)
                P.dma("sp", qkg[:, 1:2], I["kg"][l], [], [t_qkg], dk_misc)
                P.dma("sp", esink[:], I["sinkb"][l], [], [t_esink], dk_misc)
                P.op("act", lambda h: h.activation(out=esink[:], in_=esink[:], func=AF.Exp), [t_esink], [t_esink])
                P.op("pool", lambda h: h.memset(Kz[0][:], 0.0), [], TK)
                P.op("pool", lambda h: h.memset(Kz[1][:], 0.0), [], TK)
                P.op("pool", lambda h: h.memset(Vaug[:], 1.0), [], TV)
                P.op("pool", lambda h: h.memset(upad[:], 0.0), [], TU + [t_uh])
                P.op("pool", lambda h: h.memset(upadc[:], 0.0), [], [t_uc])

                with ExitStack() as st:
                    win = sbt(st, "win", [128, 8, 1536], BF16); t_win, t_wab = T(), T()
                    fs = st.enter_context(ExitStack())
                    wft = sbt(fs, "wft", [64, 4, D], F32); t_wft = T()
                    fw2 = sbt(fs, "fw2", [64, 4, 64], F32); t_fw2 = T()
                    d64 = sbt(fs, "d64", [64, 2, 64], F32); t_d64 = T()
                    m1 = sbt(fs, "m1", [64, 4, 2, 64], F32); t_m1 = T()
                    P.dma("pool", win[:, :, 0:1024], I["w_inp"][l].rearrange("(k p) c -> p k c", p=128), [], [t_win], dk_w[2])
                    P.dma("sp", wft[:], I["w_inFT"][l], [], [t_wft], dk_misc)
                    P.dma("sp", fw2[:], I["four_w2"][l], [], [t_fw2], dk_misc)
                    P.dma("sp", d64[:], I["dft64"], [], [t_d64], dk_misc)
                    pb, tpb = next_bank()
                    for g in range(4):
                        for cs in range(2):
                            P.op("pe", lambda h, g=g, cs=cs, pb=pb: h.matmul(pb[0:64, (g * 2 + cs) * 64:(g * 2 + cs + 1) * 64], lhsT=d64[:, cs, :], rhs=fw2[:, g, :],
                                                                             start=True, stop=True), [t_d64, t_fw2], [tpb], inc=(g == 3 and cs == 1))
                    P.op("dve", lambda h, pb=pb: h.tensor_copy(out=m1[:].rearrange("p g c d -> p (g c d)"), in_=pb[0:64, 0:512]), [tpb], [t_m1])
                    for k in range(8):
                        pb, tpb = next_bank()
                        for g in range(4):
                            P.op("pe", lambda h, g=g, k=k, pb=pb: h.matmul(pb[:, g * 128:(g + 1) * 128], lhsT=wft[:, g, k * 128:(k + 1) * 128],
                                                                           rhs=m1[:, g].rearrange("p c d -> p (c d)"), start=True, stop=True),
                                 [t_wft, t_m1], [tpb], inc=(g == 3))
                        P.op("act", lambda h, k=k, pb=pb: h.copy(out=win[:, k, 1024:1536].rearrange("p (c g d) -> p g c d", c=2, g=4),
                                                                 in_=pb[:, 0:512].rearrange("p (g c d) -> p g c d", g=4, c=2)), [tpb], [t_wab])

                    P.barrier()
                    fs.close()
                    AN = 256
                    hTs = [(sbt(st, "hT%d" % i, [128, 8, AN], BF16), T()) for i in range(2)]
                    rstds = Ring([(sbt(st, "rstd%d" % i, [128, AN], F32), T()) for i in range(2)])
                    tmps = Ring([(sbt(st, "ntmp%d" % i, [128, AN], F32), T()) for i in range(3)])
                    sqq = Ring([(sbt(st, "sqq%d" % i, [128, AN], BF16), T()) for i in range(3)])
                    sdr = Ring([(sbt(st, "sd%d" % i, [128, AN], F32), T()) for i in range(3)])
                    qnr = Ring([(sbt(st, "qn%d" % i, [128, AN], BF16), T()) for i in range(3)])
                    t1r = Ring([(sbt(st, "t1_%d" % i, [128, AN], F32), T()) for i in range(3)])
                    t2r = Ring([(sbt(st, "t2_%d" % i, [128, AN], F32), T()) for i in range(3)])
                    stg = Ring([(sbt(st, "stg%d" % i, [128, 512], BF16), T()) for i in range(3)])
                    kcol_of = lambda c0: (128 + c0) if c0 < NTOK else (18 * 128 + c0 - NTOK)
                    GROUPS_A1 = [(g * AN, AN, 0) for g in range(NTOK // AN)] + [(NTOK, LC, 1)]

                    def emit_norm(gi):
                        c0, n, isctx = GROUPS_A1[gi]
                        hT, t_hT = hTs[gi % 2]
                        norm_mod((rstds, tmps), l, 0, c0, n, isctx, lambda k, hT=hT, n=n: hT[:, k, 0:n], lambda k, t_hT=t_hT: [t_hT])

                    def emit_chunks(gi):
                        c0, n, isctx = GROUPS_A1[gi]
                        hT, t_hT = hTs[gi % 2]
                        need_full = (not last) or (not isctx)
                        mlist = list(range(7)) if need_full else [6]
                        state = {}

                        def stage1(m):
                            pb, tpb = next_bank()
                            for k in range(8):
                                P.op("pe", lambda h, k=k, m=m, pb=pb: h.matmul(pb[:, 0:n], lhsT=win[:, k, m * 128:(m + 1) * 128], rhs=hT[:, k, 0:n],
                                                                               start=(k == 0), stop=(k == 7)), [t_win, t_hT], [tpb], inc=(k == 7))
                            if m < 2:
                                if isctx:
                                    P.op("act", lambda h, m=m, pb=pb: h.copy(out=upadc[:, m, UPAD:UPAD + n], in_=pb[:, 0:n]), [tpb], [t_uc])
                                else:
                                    P.op("act", lambda h, m=m, pb=pb: h.copy(out=upad[:, m, UPAD + c0:UPAD + c0 + n], in_=pb[:, 0:n]), [tpb], [TU[c0 // 512]])
                                return
                            sq, t_sq = sqq.next()
                            P.op("act", lambda h, pb=pb, sq=sq: h.activation(out=sq[:, 0:n], in_=pb[:, 0:n], func=AF.Square), [tpb], [t_sq])
                            state[m] = dict(pb=pb, tpb=tpb, sq=sq, t_sq=t_sq)

                        def stage2(m):
                            if m < 2:
                                return
                            S = state[m]
                            isk = (m == 6)
                            pb2, tpb2 = next_bank()
                            P.op("pe", lambda h, pb2=pb2, sq=S["sq"]: h.matmul(pb2[:, 0:n], lhsT=hsum[:], rhs=sq[:, 0:n], start=True, stop=True), [S["t_sq"], t_const], [tpb2])
                            sd, t_sd = sdr.next()
                            P.op("act", lambda h, pb2=pb2, sd=sd: h.activation(out=sd[:, 0:n], in_=pb2[:, 0:n], func=AF.Ln, bias=epsb[:, 0:1], scale=1.0), [tpb2, t_const], [t_sd])
                            P.op("act", lambda h, sd=sd: h.activation(out=sd[:, 0:n], in_=sd[:, 0:n], func=AF.Exp, scale=-0.5), [t_sd], [t_sd])
                            qn, t_qn = qnr.next()
                            P.op("dve", lambda h, pb=S["pb"], sd=sd, qn=qn, isk=isk: h.scalar_tensor_tensor(out=qn[:, 0:n], in0=pb[:, 0:n], scalar=qkg[:, (1 if isk else 0):(2 if isk else 1)],
                                                                                                          in1=sd[:, 0:n], op0=ALU.mult, op1=ALU.mult), [S["tpb"], t_sd, t_qkg], [t_qn])
                            S.update(qn=qn, t_qn=t_qn)

                        def stage3(m):
                            if m < 2:
                                return
                            S = state[m]
                            isk = (m == 6)
                            qn, t_qn = S["qn"], S["t_qn"]
                            pb3, tpb3 = next_bank()
                            P.op("pe", lambda h, pb3=pb3, qn=qn: h.matmul(pb3[:, 0:n], lhsT=rotm[:], rhs=qn[:, 0:n], start=True, stop=True), [t_qn, t_const], [tpb3])
                            t1, t_t1 = t1r.next()
                            t2, t_t2 = t2r.next()
                            P.op("pool", lambda h, t1=t1, qn=qn: h.tensor_tensor(out=t1[:, 0:n], in0=qn[:, 0:n], in1=ropeC[:, c0:c0 + n], op=ALU.mult), [t_qn, t_rope], [t_t1])
                            P.op("dve", lambda h, t2=t2, pb3=pb3: h.tensor_tensor(out=t2[:, 0:n], in0=pb3[:, 0:n], in1=ropeS[:, c0:c0 + n], op=ALU.mult), [tpb3, t_rope], [t_t2])
                            if isk:
                                kc = kcol_of(c0)
                                tks = [TK[b] for b in range(kc // 128, (kc + n) // 128)]
                                P.op("pool", lambda h, t1=t1, t2=t2, kc=kc: h.tensor_tensor(out=Kz[0][0:64, kc:kc + n], in0=t1[0:64, 0:n], in1=t2[0:64, 0:n], op=ALU.add), [t_t1, t_t2], tks)
                                P.op("pool", lambda h, t1=t1, t2=t2, kc=kc: h.tensor_tensor(out=Kz[1][64:128, kc:kc + n], in0=t1[64:128, 0:n], in1=t2[64:128, 0:n], op=ALU.add), [t_t1, t_t2], tks)
                            else:
                                P.op("pool", lambda h, t1=t1, t2=t2, m=m: h.tensor_tensor(out=catT[:, m, c0:c0 + n], in0=t1[:, 0:n], in1=t2[:, 0:n], op=ALU.add), [t_t1, t_t2], cat_T([m], c0, n))
                        nm = len(mlist)
                        for step in range(nm + 2):
                            if step < nm:
                                stage1(mlist[step])
                            if 0 <= step - 1 < nm:
                                stage2(mlist[step - 1])
                            if 0 <= step - 2 < nm:
                                stage3(mlist[step - 2])

                    def emit_tokmajor(gi):
                        c0, n, isctx = GROUPS_A1[gi]
                        hT, t_hT = hTs[gi % 2]
                        need_full = (not last) or (not isctx)
                        for tt in range(n // 128):
                            cc0 = tt * 128
                            blk = kcol_of(c0 + cc0) // 128
                            pb, tpb = next_bank()
                            for k in range(8):
                                P.op("pe", lambda h, k=k, pb=pb, cc0=cc0: h.matmul(pb[:, 0:128], lhsT=hT[:, k, cc0:cc0 + 128], rhs=win[:, k, 896:1024],
                                                                                   start=(k == 0), stop=(k == 7)), [t_win, t_hT], [tpb], inc=(k == 7))
                            P.op("act", lambda h, pb=pb, blk=blk: h.copy(out=Vaug[:, blk, 0, 0:64], in_=pb[:, 0:64]), [tpb], [TV[blk]])
                            P.op("act", lambda h, pb=pb, blk=blk: h.copy(out=Vaug[:, blk, 1, 64:128], in_=pb[:, 64:128]), [tpb], [TV[blk]])
                            if not need_full:
                                continue
                            pb, tpb = next_bank()
                            for k in range(8):
                                P.op("pe", lambda h, k=k, pb=pb, cc0=cc0: h.matmul(pb[:, 0:512], lhsT=hT[:, k, cc0:cc0 + 128], rhs=win[:, k, 1024:1536],
                                                                                   start=(k == 0), stop=(k == 7)), [t_win, t_wab, t_hT], [tpb], inc=(k == 7))
                            if isctx:
                                P.op("dve", lambda h, pb=pb, tt=tt: h.tensor_copy(out=ABc[:, tt, :], in_=pb[:, 0:512]), [tpb], [t_ABc])
                            else:
                                sg, t_sg = stg.next()
                                P.op("dve", lambda h, pb=pb, sg=sg: h.tensor_copy(out=sg[:], in_=pb[:, 0:512]), [tpb], [t_sg])
                                r0 = c0 + cc0
                                P.dma("sp", gin_ab[r0:r0 + 128, :], sg[:], [t_sg], [t_gin_ab], dk_gw)

                    emit_norm(0)
                    for gi in range(len(GROUPS_A1)):
                        if gi + 1 < len(GROUPS_A1):
                            emit_norm(gi + 1)
                        emit_tokmajor(gi)
                        emit_chunks(gi)
                    hv = lambda r0, nr: gin_h[r0:r0 + nr, :].rearrange("r (a j) -> (r a) j", a=4)
                    P.dma("sp", hv(0, 32)[0:64, :], Kz[0][0:64, 128:256], [TK[1]], [t_gin_h], dk_gw)
                    P.dma("sp", hv(0, 32)[64:128, :], Kz[1][64:128, 128:256], [TK[1]], [t_gin_h], dk_gw)
                    P.dma("sp", hv(32, 32)[0:64, :], Kz[0][0:64, 16 * 128:17 * 128], [TK[16]], [t_gin_h], dk_gw)
                    P.dma("sp", hv(32, 32)[64:128, :], Kz[1][64:128, 16 * 128:17 * 128], [TK[16]], [t_gin_h], dk_gw)
                    P.dma("sp", hv(64, 32)[:, 0:64], Vaug[:, 1, 0, 0:64], [TV[1]], [t_gin_h], dk_gw)
                    P.dma("sp", hv(64, 32)[:, 64:128], Vaug[:, 1, 1, 64:128], [TV[1]], [t_gin_h], dk_gw)
                    P.dma("sp", hv(96, 32)[:, 0:64], Vaug[:, 16, 0, 0:64], [TV[16]], [t_gin_h], dk_gw)
                    P.dma("sp", hv(96, 32)[:, 64:128], Vaug[:, 16, 1, 64:128], [TV[16]], [t_gin_h], dk_gw)
                    pv = lambda r0: gin_h[r0:r0 + 4, :].rearrange("r (a j) -> (r a) j", a=32)[:, 0:16].rearrange("p (c j) -> p c j", c=2)
                    P.dma("sp", pv(128), upad[:, :, UPAD:UPAD + 8], [TU[0]], [t_gin_h], dk_gw)
                    P.dma("sp", pv(132), upad[:, :, UPAD + NTOK - 8:UPAD + NTOK], [TU[3]], [t_gin_h], dk_gw)
                    P.collective("AllGather", PAIRS, gin_h.opt(), gout_h.opt(), t_gin_h, t_gout_h, dk_cc)
                    P.collective("AllGather", PAIRS, gin_ab.opt(), gout_ab.opt(), t_gin_ab, t_gout_ab, dk_cc2)
                    P.barrier(exclude=(dk_cc, dk_cc2))
                if stop == "A1":
                    break

                with ExitStack() as st:
                    hvo = lambda r0, nr: gout_h[r0:r0 + nr, :].rearrange("r (a j) -> (r a) j", a=4)
                    P.dma("sp", Kz[0][0:64, 0:128], hvo(32, 32)[0:64, :], [t_gout_h], [TK[0]], dk_gr)
                    P.dma("sp", Kz[1][64:128, 0:128], hvo(32, 32)[64:128, :], [t_gout_h], [TK[0]], dk_gr)
                    P.dma("sp", Kz[0][0:64, 17 * 128:18 * 128], hvo(256, 32)[0:64, :], [t_gout_h], [TK[17]], dk_gr)
                    P.dma("sp", Kz[1][64:128, 17 * 128:18 * 128], hvo(256, 32)[64:128, :], [t_gout_h], [TK[17]], dk_gr)
                    P.dma("sp", Vaug[:, 0, 0, 0:64], hvo(96, 32)[:, 0:64], [t_gout_h], [TV[0]], dk_gr)
                    P.dma("sp", Vaug[:, 0, 1, 64:128], hvo(96, 32)[:, 64:128], [t_gout_h], [TV[0]], dk_gr)
                    P.dma("sp", Vaug[:, 17, 0, 0:64], hvo(256 + 64, 32)[:, 0:64], [t_gout_h], [TV[17]], dk_gr)
                    P.dma("sp", Vaug[:, 17, 1, 64:128], hvo(256 + 64, 32)[:, 64:128], [t_gout_h], [TV[17]], dk_gr)
                    pvo = lambda r0: gout_h[r0:r0 + 4, :].rearrange("r (a j) -> (r a) j", a=32)[:, 0:16].rearrange("p (c j) -> p c j", c=2)
                    P.dma("sp", upad[:, :, UPAD - 8:UPAD], pvo(132), [t_gout_h], [t_uh], dk_gr)
                    P.dma("sp", upad[:, :, UPAD + NTOK:UPAD + NTOK + 8], pvo(256 + 128), [t_gout_h], [t_uh], dk_gr)

                    Eloc = Ring([(sbt(st, "Eloc%d" % i, [128, 3, 4, 128], BF16), T()) for i in range(2)])
                    Ectx = Ring([(sbt(st, "Ectx%d" % i, [128, 2, 4, 128], BF16), T()) for i in range(2)])
                    rdn = Ring([(sbt(st, "rdn%d" % i, [128, 4, 128], F32), T()) for i in range(2)])
                    SL = PSA[:, 0:1536].rearrange("p (j g q) -> p j g q", j=3, g=4); TSL = TP[0:3]
                    SC = PSB[:, 0:1024].rearrange("p (j g q) -> p j g q", j=2, g=4); TSC = TP[4:6]
                    OB = [(PSB[:, 1024:1536].rearrange("p (g q) -> p g q", g=4), TP[6]), (PSB[:, 1536:2048].rearrange("p (g q) -> p g q", g=4), TP[7])]
                    qblocks = list(range(1, 15)) + ([] if last else [16, 17]) + [0, 15]
                    hideA = HIDE_MOD1 and l == layers[0]
                    if hideA:
                        wmA = sbt(st, "wmA", [128, 8, 1024], BF16); t_wmA = T()
                        mod_piece_dma(l, 2, wmA, t_wmA)
                    qpos = 0
                    for i in qblocks:
                        isctx = i >= 16
                        qc0 = i * 128
                        tq = [TC[2 + g][i] for g in range(4)]
                        Es = {}

                        def ph_S(kvh):
                            KZ = Kz[kvh]
                            if not isctx:
                                for jj in range(3):
                                    blk = i + jj
                                    for g in range(4):
                                        P.op("pe", lambda h, jj=jj, g=g, blk=blk, KZ=KZ: h.matmul(SL[:, jj, g, :], lhsT=KZ[:, blk * 128:(blk + 1) * 128], rhs=catT[:, 2 + g, qc0:qc0 + 128],
                                                                                                 start=True, stop=True), [TK[blk]] + tq, TSL, inc=(jj == 2 and g == 3))
                            for jj in range(2):
                                blk = 18 + jj
                                for g in range(4):
                                    P.op("pe", lambda h, jj=jj, g=g, blk=blk, KZ=KZ: h.matmul(SC[:, jj, g, :], lhsT=KZ[:, blk * 128:(blk + 1) * 128], rhs=catT[:, 2 + g, qc0:qc0 + 128],
                                                                                             start=True, stop=True), [TK[blk]] + tq, TSC, inc=(jj == 1 and g == 3))

                        def ph_E(kvh):
                            ec, t_ec = Ectx.next()
                            seqs = []
                            if not isctx:
                                el, t_el = Eloc.next()
                                P.op("act", lambda h, el=el: h.activation(out=el[:], in_=SL, func=AF.Exp, scale=0.125), TSL, [t_el])
                                mp = 2 if i == 0 else 0
                                mn = 3 if i == 15 else 1
                                P.op("dve", lambda h, el=el, mp=mp: h.tensor_tensor(out=el[:, 0], in0=el[:, 0], in1=masks[:, mp, :].unsqueeze(1).to_broadcast([128, 4, 128]), op=ALU.mult), [t_el, t_masks], [t_el])
                                P.op("dve", lambda h, el=el, mn=mn: h.tensor_tensor(out=el[:, 2], in0=el[:, 2], in1=masks[:, mn, :].unsqueeze(1).to_broadcast([128, 4, 128]), op=ALU.mult), [t_el, t_masks], [t_el])
                                seqs += [(el, t_el, jj, i + jj) for jj in range(3)]
                            P.op("act", lambda h, ec=ec: h.activation(out=ec[:], in_=SC, func=AF.Exp, scale=0.125), TSC, [t_ec])
                            seqs += [(ec, t_ec, jj, 18 + jj) for jj in range(2)]
                            Es[kvh] = seqs

                        def ph_PV(kvh):
                            ob, tob = OB[kvh]
                            seqs = Es[kvh]
                            for si, (E, t_E, jj, blk) in enumerate(seqs):
                                P.op("pe", lambda h, E=E, jj=jj, blk=blk, ob=ob, si=si, ns=len(seqs), kvh=kvh: h.matmul(ob, lhsT=Vaug[:, blk, kvh, :], rhs=E[:, jj], start=(si == 0), stop=(si == ns - 1)),
                                     [t_E, TV[blk]], [tob], inc=(si == len(seqs) - 1))

                        def ph_N(kvh):
                            ob, tob = OB[kvh]
                            rd, t_rd = rdn.next()
                            dlo, dhi = (64, 128) if kvh == 0 else (0, 64)
                            olo, ohi = (0, 64) if kvh == 0 else (64, 128)
                            P.op("dve", lambda h, rd=rd, ob=ob, dlo=dlo, dhi=dhi, kvh=kvh: h.tensor_tensor(out=rd[dlo:dhi], in0=ob[dlo:dhi], in1=esink[dlo:dhi, kvh, :].unsqueeze(2).to_broadcast([64, 4, 128]), op=ALU.add),
                                 [tob, t_esink], [t_rd])
                            P.op("act", lambda h, rd=rd, dlo=dlo, dhi=dhi: h.activation(out=rd[dlo:dhi], in_=rd[dlo:dhi], func=AF.Ln), [t_rd], [t_rd])
                            P.op("act", lambda h, rd=rd, dlo=dlo, dhi=dhi: h.activation(out=rd[dlo:dhi], in_=rd[dlo:dhi], func=AF.Exp, scale=-1.0), [t_rd], [t_rd])
                            P.op("dve", lambda h, rd=rd, ob=ob, olo=olo, ohi=ohi, dlo=dlo, dhi=dhi: h.tensor_tensor(out=catT[olo:ohi, 2:6, qc0:qc0 + 128], in0=ob[olo:ohi], in1=rd[dlo:dhi], op=ALU.mult),
                                 [tob, t_rd], tq)
                        ph_S(0); ph_E(0); ph_S(1); ph_E(1); ph_PV(0); ph_N(0); ph_PV(1); ph_N(1)
                        qpos += 1
                        if hideA and qpos in (3, 6, 9, 12):
                            v_ = 2 + (qpos // 3 - 1)
                            mod_piece_mm(l, v_, wmA, t_wmA, bank(3), TP[3])
                            if v_ < 5:
                                mod_piece_dma(l, v_ + 1, wmA, t_wmA)
                            else:
                                mod_finalize_part(l, bank(3), TP[3], 2, 6)
                    P.barrier()
                if stop == "A2":
                    break
                mxb.close()
                wout = sbt(mxa, "wout", [128, 8, D], BF16); t_wout = T()
                P.dma("pool", wout[:], I["w_outp"][l].rearrange("(k p) c -> p k c", p=128), [], [t_wout], dk_w[3])

                with ExitStack() as st:
                    AB = sbt(st, "AB", [128, 32, 512], BF16); t_AB = [T() for _ in range(4)]
                    tabs = [(sbt(st, "tab%d" % i, [128, 4, 2, 512], BF16), T(), dk_tab[i]) for i in range(2)]
                    d256 = sbt(st, "d256", [128, 2, 2, 256], BF16); t_d256 = T()
                    P.dma("sp", d256[:], I["dft256"], [], [t_d256], dk_misc)
                    halov = sbt(st, "halov", [128, 2], F32); t_halov = T()
                    pfix = sbt(st, "pfix", [128, 2, 2, 8], F32)
                    pfixc = sbt(st, "pfixc", [128, 2, 2, 8], F32); t_pfix = T()
                    pwbd = sbt(st, "pwbd", [128, 2, 128], BF16); t_pwbd = T()
                    psc = sbt(st, "psc", [128, 2], F32); t_psc = T()
                    P.dma("sp", halov[:], I["halov"], [], [t_halov], dk_misc)
                    P.dma("sp", pfix[:], I["poolfix"], [], [t_pfix], dk_misc)
                    P.dma("sp", pfixc[:], I["poolfixc"], [], [t_pfix], dk_misc)
                    P.dma("pool", pwbd[:], I["poolw_bd"][l].rearrange("c p m -> p c m"), [], [t_pwbd], dk_misc)
                    P.dma("sp", psc[:], I["pscale"][l], [], [t_psc], dk_misc)
                    for q4 in range(4):
                        P.dma("sp", AB[:, q4 * 8:(q4 + 1) * 8, :], gout_ab[q4 * 1024:(q4 + 1) * 1024, :].rearrange("(n p) c -> p n c", p=128),
                              [t_gout_ab], [t_AB[q4]], dk_gr)
                    for hq in range(2):
                        lo = AB[:, hq * 8:(hq + 1) * 8, :]
                        hi = AB[:, 16 + hq * 8:16 + (hq + 1) * 8, :]
                        P.op("dve", lambda h, lo=lo, hi=hi: h.tensor_tensor(out=lo, in0=lo, in1=hi, op=ALU.add), [t_AB[hq], t_AB[2 + hq]], [t_AB[hq]])
                        P.op("dve", lambda h, lo=lo, hi=hi: h.scalar_tensor_tensor(out=hi, in0=hi, scalar=-2.0, in1=lo, op0=ALU.mult, op1=ALU.add), [t_AB[hq], t_AB[2 + hq]], [t_AB[2 + hq]])
                    pt = [(sbt(st, "pt%d" % i, [128, 512 + 32], F32), T()) for i in range(3)]
                    deferred = []
                    SEG = ([] if last else [(0, LC, 1)]) + [(512, 512, 0), (1024, 512, 0), (0, 512, 0), (1536, 512, 0)]
                    for si_, (c0, n, isctx) in enumerate(SEG):
                        if (not isctx) and c0 == 0:
                            P.op("dve", lambda h: h.tensor_scalar(out=upad[:, :, UPAD - 8:UPAD], in0=upad[:, :, UPAD - 8:UPAD], scalar1=halov[:, 0:1], scalar2=None, op0=ALU.mult),
                                 [t_uh, t_halov], [t_uh])
                            P.op("dve", lambda h: h.tensor_scalar(out=upad[:, :, UPAD + NTOK:UPAD + NTOK + 8], in0=upad[:, :, UPAD + NTOK:UPAD + NTOK + 8], scalar1=halov[:, 1:2], scalar2=None, op0=ALU.mult),
                                 [t_uh, t_halov], [t_uh])
                        U = upadc if isctx else upad
                        tus = [t_uc] if isctx else ([TU[c0 // 512]] + ([t_uh] if (c0 == 0 or c0 + 512 == NTOK) else []) + ([TU[c0 // 512 - 1]] if c0 > 0 else []) + ([TU[c0 // 512 + 1]] if c0 + 512 < NTOK else []))
                        fx = pfixc if isctx else pfix
                        seq_n = LC if isctx else NTOK
                        for ch in range(2):
                            (a, t_a), (b, t_b), (cbuf, t_c) = pt
                            base = UPAD + c0 - 16
                            W = n + 32
                            P.op("dve", lambda h, a=a, U=U, ch=ch, base=base, W=W: h.tensor_tensor(out=a[:, 1:W], in0=U[:, ch, base + 1:base + W], in1=U[:, ch, base:base + W - 1], op=ALU.add), tus, [t_a])
                            if ch == 0:
                                P.op("dve", lambda h, a=a, cbuf=cbuf: h.tensor_copy(out=cbuf[0:64, 0:n], in_=a[0:64, 16:16 + n]), [t_a], [t_c])
                                P.op("dve", lambda h, a=a, cbuf=cbuf: h.tensor_tensor(out=cbuf[64:128, 0:n], in0=a[64:128, 17:17 + n], in1=a[64:128, 15:15 + n], op=ALU.add), [t_a], [t_c])
                            else:
                                P.op("dve", lambda h, a=a, b=b, W=W: h.tensor_tensor(out=b[:, 3:W], in0=a[:, 3:W], in1=a[:, 1:W - 2], op=ALU.add), [t_a], [t_b])
                                P.op("dve", lambda h, a=a, b=b, W=W: h.tensor_tensor(out=a[:, 7:W], in0=b[:, 7:W], in1=b[:, 3:W - 4], op=ALU.add), [t_b, t_a], [t_a])
                                P.op("dve", lambda h, a=a, cbuf=cbuf: h.tensor_copy(out=cbuf[0:64, 0:n], in_=a[0:64, 19:19 + n]), [t_a], [t_c])
                                P.op("dve", lambda h, a=a, cbuf=cbuf: h.tensor_tensor(out=cbuf[64:128, 0:n], in0=a[64:128, 23:23 + n], in1=a[64:128, 15:15 + n], op=ALU.add), [t_a], [t_c])
                            if c0 == 0:
                                P.op("dve", lambda h, cbuf=cbuf, fx=fx, ch=ch: h.tensor_tensor(out=cbuf[:, 0:8], in0=cbuf[:, 0:8], in1=fx[:, ch, 0, :], op=ALU.mult), [t_c, t_pfix], [t_c])
                            if c0 + n == seq_n:
                                P.op("dve", lambda h, cbuf=cbuf, fx=fx, ch=ch: h.tensor_tensor(out=cbuf[:, n - 8:n], in0=cbuf[:, n - 8:n], in1=fx[:, ch, 1, :], op=ALU.mult), [t_c, t_pfix], [t_c])
                            wl, wh = (2, 4) if ch == 0 else (8, 16)
                            oc0 = NTOK if isctx else c0
                            tyc = cat_T([ch], oc0, n)
                            P.op("dve", lambda h, cbuf=cbuf, wl=wl: h.tensor_scalar(out=cbuf[0:64, 0:n], in0=cbuf[0:64, 0:n], scalar1=1.0 / wl, scalar2=None, op0=ALU.mult), [t_c], [t_c])
                            P.op("dve", lambda h, cbuf=cbuf, wh=wh: h.tensor_scalar(out=cbuf[64:128, 0:n], in0=cbuf[64:128, 0:n], scalar1=1.0 / wh, scalar2=None, op0=ALU.mult), [t_c], [t_c])
                            P.op("dve", lambda h, cbuf=cbuf, U=U, ch=ch, oc0=oc0: h.tensor_tensor(out=catT[:, ch, oc0:oc0 + n], in0=cbuf[:, 0:n], in1=U[:, ch, UPAD + c0:UPAD + c0 + n], op=ALU.subtract),
                                 [t_c] + tus, tyc)
                            deferred.append((ch, oc0, n))

                    ti = 0
                    for pi in range(2):
                        for jt in range(2):
                            accs = [next_bank(), next_bank()]
                            for ng_ in range(4):
                                tab, t_tab, dkt = tabs[ti % 2]
                                ti += 1
                                P.dma("sp", tab[:], I["dfttab"][pi, jt, ng_], [], [t_tab], dkt)
                                for nn in range(4):
                                    nch = ng_ * 4 + nn + 16 * pi
                                    for cs in range(2):
                                        for m in range(2):
                                            first = (ng_ == 0 and nn == 0 and cs == 0)
                                            lastmm = (ng_ == 3 and nn == 3 and cs == 1)
                                            P.op("pe", lambda h, m=m, nch=nch, cs=cs, tab=tab, nn=nn, first=first, lastmm=lastmm, acc=accs[m][0]:
                                                 h.matmul(acc, lhsT=AB[:, nch, cs * 256 + m * 128:cs * 256 + (m + 1) * 128], rhs=tab[:, nn, cs, :], start=first, stop=lastmm),
                                                 [t_AB[nch // 8], t_tab], [accs[m][1]], inc=(lastmm or (nn == 3 and cs == 1 and m == 1)))
                            for m in range(2):
                                P.op("act", lambda h, m=m, acc=accs[m][0], jt=jt, pi=pi: h.copy(out=catT[:, 6 + m, jt * 1024:(jt + 1) * 1024].rearrange("p (j two) -> p j two", two=2)[:, :, pi], in_=acc),
                                     [accs[m][1]], cat_T([6 + m], jt * 1024, 1024))
                    if not last:
                        accs = [next_bank(), next_bank()]
                        for nn in range(2):
                            for cs in range(2):
                                for m in range(2):
                                    first = (nn == 0 and cs == 0)
                                    lastmm = (nn == 1 and cs == 1)
                                    P.op("pe", lambda h, m=m, nn=nn, cs=cs, first=first, lastmm=lastmm, acc=accs[m][0]:
                                         h.matmul(acc[:, 0:256], lhsT=ABc[:, nn, cs * 256 + m * 128:cs * 256 + (m + 1) * 128], rhs=d256[:, nn, cs, :], start=first, stop=lastmm),
                                         [t_ABc, t_d256], [accs[m][1]], inc=lastmm)
                        for m in range(2):
                            P.op("act", lambda h, m=m, acc=accs[m][0]: h.copy(out=catT[:, 6 + m, NTOK:TOK], in_=acc[:, 0:256]), [accs[m][1]], cat_T([6 + m], NTOK, LC))
                    for (ch, oc0, n) in deferred:
                        pb, tpb = next_bank()
                        P.op("pe", lambda h, pb=pb, ch=ch, oc0=oc0, n=n: h.matmul(pb[:, 0:n], lhsT=pwbd[:, ch, :], rhs=catT[:, ch, oc0:oc0 + n], start=True, stop=True), cat_T([ch], oc0, n) + [t_pwbd], [tpb])
                        P.op("act", lambda h, pb=pb, ch=ch, oc0=oc0, n=n: h.activation(out=catT[:, ch, oc0:oc0 + n], in_=pb[:, 0:n], func=AF.Copy, scale=psc[:, ch:ch + 1]),
                             [tpb, t_psc], cat_T([ch], oc0, n))
                    P.barrier()
                if "catT" in DUMP and l == layers[-1] and stop in ("A3", "A2"):
                    pass
                if stop == "A3":
                    break

                rstds2 = Ring([(sbt(mxa, "rstd2_%d" % i, [128, 512], F32), T()) for i in range(2)])
                tmps2 = Ring([(sbt(mxa, "n2tmp%d" % i, [128, 512], F32), T()) for i in range(3)])
                G4 = [g for g in GROUPS if not (g[2] and last)]

                def emit_A4(c0, n, isctx):
                    for m in range(8):
                        pb, tpb = next_bank()
                        for k in range(8):
                            P.op("pe", lambda h, k=k, m=m, pb=pb: h.matmul(pb[:, 0:n], lhsT=wout[:, k, m * 128:(m + 1) * 128], rhs=catT[:, k, c0:c0 + n],
                                                                           start=(k == 0), stop=(k == 7)), [t_wout] + cat_T([k], c0, n), [tpb], inc=(k == 7))
                        P.op("dve", lambda h, m=m, pb=pb: h.scalar_tensor_tensor(out=x_ap(m, c0, n), in0=pb[:, 0:n], scalar=modv(l, 2, m, isctx), in1=x_ap(m, c0, n),
                                                                                 op0=ALU.mult, op1=ALU.add), [tpb, t_modl[l], x_T(m, c0)], [x_T(m, c0)])

                def emit_N2(c0, n, isctx):
                    norm_mod((rstds2, tmps2), l, 1, c0, n, isctx, lambda k, c0=c0, n=n: catT[:, k, c0:c0 + n], lambda k, c0=c0, n=n: cat_T([k], c0, n))
                for gi_, g_ in enumerate(G4):
                    emit_A4(*g_)
                    if gi_ >= 1:
                        emit_N2(*G4[gi_ - 1])
                emit_N2(*G4[-1])
                P.barrier()
            if stop in ("A1", "A2", "A3", "A4"):
                break

            MG = [g for g in GROUPS if not (g[2] and last)]
            ntile = sum(g[1] for g in MG) // 128
            h2T = catT
            with ExitStack() as st:
                gatesT = sbt(st, "gatesT", [16, TOK], BF16); t_gT = T()
                sel = sbt(st, "sel", [16, 16, 128], BF16); t_sel = T()
                P.dma("sp", sel[:], I["sel"], [], [t_sel], dk_misc)
                wg = [sbt(st, "wg%d" % i, [128, 8, 512], BF16) for i in range(2)]
                wu = [sbt(st, "wu%d" % i, [128, 8, 512], BF16) for i in range(2)]
                wd = [sbt(st, "wd%d" % i, [128, 4, D], BF16) for i in range(2)]
                t_wg, t_wu, t_wd = [T(), T()], [T(), T()], [T(), T()]
                def load_expert(e):
                    s = e % 2
                    P.dma("pool", wg[s][:], I["w_gate"][l, e].rearrange("(k p) c -> p k c", p=128), [], [t_wg[s]], dk_ex[s][0])
                    P.dma("pool", wu[s][:], I["w_up"][l, e].rearrange("(k p) c -> p k c", p=128), [], [t_wu[s]], dk_ex[s][1])
                    P.dma("pool", wd[s][:], I["w_down"][l, e].rearrange("(k p) c -> p k c", p=128), [], [t_wd[s]], dk_ex[s][2])
                load_expert(0)
                with ExitStack() as s2:
                    wr = sbt(s2, "wr", [128, 8, 20], BF16); t_wr = T()
                    brt = sbt(s2, "brt", [128, 20], F32); t_br = T()
                    P.dma("pool", wr[:], I["wr"][l].rearrange("(k p) c -> p k c", p=128), [], [t_wr], dk_misc)
                    P.dma("sp", brt[:], I["br"][l], [], [t_br], dk_misc)
                    pbr, tpbr = next_bank()
                    for tt in range(ntile):
                        for k in range(8):
                            P.op("pe", lambda h, tt=tt, k=k: h.matmul(pbr[:, tt * 20:(tt + 1) * 20], lhsT=h2T[:, k, tt * 128:(tt + 1) * 128], rhs=wr[:, k, :],
                                                                      start=(k == 0), stop=(k == 7)), [t_wr] + cat_T([k], tt * 128, 128), [tpbr], inc=(k == 7))
                    NT = ntile
                    rt = lambda nm, w: (sbt(s2, nm, [128, NT, w], F32), T())
                    Lg, t_L = rt("Lg", 20)
                    P.op("dve", lambda h: h.tensor_tensor(out=Lg[:], in0=pbr[:, 0:NT * 20].rearrange("p (t c) -> p t c", c=20), in1=brt[:].unsqueeze(1).to_broadcast([128, NT, 20]), op=ALU.add),
                         [tpbr, t_br], [t_L])
                    mg, t_mg = rt("mg", 1)
                    P.op("dve", lambda h: h.tensor_reduce(out=mg[:, :, 0], in_=Lg[:, :, 0:4], axis=AX.X, op=ALU.max), [t_L], [t_mg])
                    eg, t_eg = rt("eg", 4)
                    P.op("dve", lambda h: h.tensor_tensor(out=eg[:], in0=Lg[:, :, 0:4], in1=mg[:].to_broadcast([128, NT, 4]), op=ALU.subtract), [t_L, t_mg], [t_eg])
                    oh, t_oh = rt("oh", 4)
                    P.op("dve", lambda h: h.tensor_single_scalar(out=oh[:], in_=eg[:], scalar=0.0, op=ALU.is_ge), [t_eg], [t_oh])
                    P.op("act", lambda h: h.activation(out=eg[:], in_=eg[:], func=AF.Exp), [t_eg], [t_eg])
                    pg, t_pg = rt("pg", 1)
                    P.op("dve", lambda h: h.tensor_reduce(out=pg[:, :, 0], in_=eg[:], axis=AX.X, op=ALU.add), [t_eg], [t_pg])
                    P.op("dve", lambda h: h.reciprocal(out=pg[:], in_=pg[:]), [t_pg], [t_pg])
                    P.op("dve", lambda h: h.tensor_scalar(out=oh[:], in0=oh[:], scalar1=BIG, scalar2=-BIG, op0=ALU.mult, op1=ALU.add), [t_oh], [t_oh])
                    lm, t_lm = rt("lm", 16)
                    P.op("dve", lambda h: h.tensor_tensor(out=lm[:].rearrange("p t (g e) -> p t g e", g=4), in0=Lg[:, :, 4:20].rearrange("p t (g e) -> p t g e", g=4),
                                                          in1=oh[:].unsqueeze(3).to_broadcast([128, NT, 4, 4]), op=ALU.add), [t_L, t_oh], [t_lm])
                    m1_, t_m1_ = rt("m1_", 1)
                    P.op("dve", lambda h: h.tensor_reduce(out=m1_[:, :, 0], in_=lm[:], axis=AX.X, op=ALU.max), [t_lm], [t_m1_])
                    is1, t_is1 = rt("is1", 16)
                    P.op("dve", lambda h: h.tensor_tensor(out=is1[:], in0=lm[:], in1=m1_[:].to_broadcast([128, NT, 16]), op=ALU.is_ge), [t_lm, t_m1_], [t_is1])
                    lm2, t_lm2 = rt("lm2", 16)
                    P.op("dve", lambda h: h.scalar_tensor_tensor(out=lm2[:], in0=is1[:], scalar=-BIG, in1=lm[:], op0=ALU.mult, op1=ALU.add), [t_is1, t_lm], [t_lm2])
                    m2_, t_m2_ = rt("m2_", 1)
                    P.op("dve", lambda h: h.tensor_reduce(out=m2_[:, :, 0], in_=lm2[:], axis=AX.X, op=ALU.max), [t_lm2], [t_m2_])
                    selm, t_selm = rt("selm", 16)
                    P.op("dve", lambda h: h.tensor_tensor(out=selm[:], in0=lm[:], in1=m2_[:].to_broadcast([128, NT, 16]), op=ALU.is_ge), [t_lm, t_m2_], [t_selm])
                    P.op("dve", lambda h: h.tensor_tensor(out=lm2[:], in0=lm[:], in1=m1_[:].to_broadcast([128, NT, 16]), op=ALU.subtract), [t_lm, t_m1_, t_lm2], [t_lm2])
                    P.op("dve", lambda h: h.tensor_scalar(out=lm2[:], in0=lm2[:], scalar1=-80.0, scalar2=None, op0=ALU.max), [t_lm2], [t_lm2])
                    P.op("act", lambda h: h.activation(out=lm2[:], in_=lm2[:], func=AF.Exp), [t_lm2], [t_lm2])
                    P.op("dve", lambda h: h.tensor_tensor(out=lm2[:], in0=lm2[:], in1=selm[:], op=ALU.mult), [t_lm2, t_selm], [t_lm2])
                    den, t_den = rt("den", 1)
                    P.op("dve", lambda h: h.tensor_reduce(out=den[:, :, 0], in_=lm2[:], axis=AX.X, op=ALU.add), [t_lm2], [t_den])
                    P.op("dve", lambda h: h.reciprocal(out=den[:], in_=den[:]), [t_den], [t_den])
                    P.op("dve", lambda h: h.tensor_tensor(out=den[:], in0=den[:], in1=pg[:], op=ALU.mult), [t_den, t_pg], [t_den])
                    P.op("dve", lambda h: h.tensor_tensor(out=lm2[:], in0=lm2[:], in1=den[:].to_broadcast([128, NT, 16]), op=ALU.mult), [t_lm2, t_den], [t_lm2])
                    for tt in range(ntile):
                        pb, tpb = next_bank()
                        P.op("pe", lambda h, tt=tt, pb=pb: h.transpose(pb[0:16, 0:128], lm2[:, tt, :], ident32[:]), [t_lm2, t_const], [tpb])
                        P.op("act", lambda h, tt=tt, pb=pb: h.copy(out=gatesT[:, tt * 128:(tt + 1) * 128], in_=pb[0:16, 0:128]), [tpb], [t_gT])
                    P.barrier()
                if "gates" in DUMP:
                    P.dma("pool", DUMP["gates"], gatesT[:], [t_gT], [], dk_out)

                load_expert(1)
                aT = Ring([(sbt(st, "aT%d" % i, [128, 4, 512], BF16), T()) for i in range(2)])
                sgr = Ring([(sbt(st, "sg%d" % i, [128, 512], BF16), T()) for i in range(3)])
                sg2r = Ring([(sbt(st, "sg2_%d" % i, [128, 512], BF16), T()) for i in range(3)])
                gbr = Ring([(sbt(st, "gb%d" % i, [128, 512], BF16), T()) for i in range(2)])

                hide = HIDE_MOD1 and l == layers[0]
                if hide:
                    wm1 = sbt(st, "wm1", [128, 8, 1024], BF16); t_wm1 = T()
                    reserved.add(7)
                    mod_piece_dma(layers[1], 0, wm1, t_wm1)
                for e in range(16):
                    s = e % 2
                    if hide and e < 6:
                        mod_piece_mm(layers[1], e, wm1, t_wm1, bank(7), TP[7])
                        if e + 1 < 6:
                            mod_piece_dma(layers[1], e + 1, wm1, t_wm1)
                        else:
                            mod_finalize(layers[1], bank(7), TP[7])
                            reserved.discard(7)
                    astate = {}

                    def emit_GU(gi):
                        c0, n, isctx = MG[gi]
                        pbg, tpbg = next_bank()
                        P.op("pe", lambda h, e=e, pbg=pbg: h.matmul(pbg[:, 0:n], lhsT=sel[:, e, :], rhs=gatesT[:, c0:c0 + n], start=True, stop=True), [t_gT, t_sel], [tpbg])
                        gb, t_gb = gbr.next()
                        P.op("act", lambda h, gb=gb, pbg=pbg: h.copy(out=gb[:, 0:n], in_=pbg[:, 0:n]), [tpbg], [t_gb])
                        a, t_a = aT.next()
                        for dc in range(4):
                            pg_, tpg_ = next_bank()
                            for k in range(8):
                                P.op("pe", lambda h, k=k, dc=dc, pg_=pg_, s=s: h.matmul(pg_[:, 0:n], lhsT=wg[s][:, k, dc * 128:(dc + 1) * 128], rhs=h2T[:, k, c0:c0 + n],
                                                                                        start=(k == 0), stop=(k == 7)), [t_wg[s]] + cat_T([k], c0, n), [tpg_], inc=(k == 7))
                            pu_, tpu_ = next_bank()
                            for k in range(8):
                                P.op("pe", lambda h, k=k, dc=dc, pu_=pu_, s=s: h.matmul(pu_[:, 0:n], lhsT=wu[s][:, k, dc * 128:(dc + 1) * 128], rhs=h2T[:, k, c0:c0 + n],
                                                                                        start=(k == 0), stop=(k == 7)), [t_wu[s]] + cat_T([k], c0, n), [tpu_], inc=(k == 7))
                            sg, t_sg = sgr.next()
                            P.op("act", lambda h, sg=sg, pg_=pg_: h.activation(out=sg[:, 0:n], in_=pg_[:, 0:n], func=AF.Silu), [tpg_], [t_sg])
                            sg2, t_sg2 = sg2r.next()
                            P.op("pool", lambda h, sg=sg, sg2=sg2, gb=gb: h.tensor_tensor(out=sg2[:, 0:n], in0=sg[:, 0:n], in1=gb[:, 0:n], op=ALU.mult), [t_sg, t_gb], [t_sg2])
                            P.op("dve", lambda h, a=a, dc=dc, pu_=pu_, sg2=sg2: h.tensor_tensor(out=a[:, dc, 0:n], in0=pu_[:, 0:n], in1=sg2[:, 0:n], op=ALU.mult), [tpu_, t_sg2], [t_a])
                        astate[gi] = (a, t_a)

                    def emit_DOWN(gi):
                        c0, n, isctx = MG[gi]
                        a, t_a = astate.pop(gi)
                        for m in range(8):
                            py, tpy = next_bank()
                            for dc in range(4):
                                P.op("pe", lambda h, dc=dc, m=m, py=py, a=a, s=s: h.matmul(py[:, 0:n], lhsT=wd[s][:, dc, m * 128:(m + 1) * 128], rhs=a[:, dc, 0:n],
                                                                                           start=(dc == 0), stop=(dc == 3)), [t_wd[s], t_a], [tpy], inc=(dc == 3))
                            P.op("dve", lambda h, m=m, py=py: h.scalar_tensor_tensor(out=x_ap(m, c0, n), in0=py[:, 0:n], scalar=modv(l, 5, m, isctx), in1=x_ap(m, c0, n),
                                                                                     op0=ALU.mult, op1=ALU.add), [tpy, t_modl[l], x_T(m, c0)], [x_T(m, c0)])
                    emit_GU(0)
                    for gi in range(len(MG)):
                        if gi + 1 < len(MG):
                            emit_GU(gi + 1)
                        emit_DOWN(gi)
                    if e + 2 < 16:
                        load_expert(e + 2)
                P.barrier()
            if stop == "B%d" % l:
                break

        if "xT" in DUMP:
            for m in range(8):
                P.dma("sp", DUMP["xT"][m * 128:(m + 1) * 128, :], xT[:, m, :], TX[m], [], dk_out)
        if "xcT" in DUMP:
            for m in range(8):
                P.dma("sp", DUMP["xcT"][m * 128:(m + 1) * 128, :], xcT[:, m, :], [TXC[m]], [], dk_out)
        if "catT" in DUMP:
            for k in range(8):
                P.dma("pool", DUMP["catT"][k * 128:(k + 1) * 128, :], catT[:, k, :], TC[k], [], dk_out)
        for m in range(8):
            P.dma("sp", outT[m * 128:(m + 1) * 128, :], xT[:, m, :], TX[m], [], dk_out)
        P.barrier()
        P.emit()
    return nc


_CACHE = {}


def kernel(**inputs):
    in_maps = host_prep(**inputs)
    if "nc" not in _CACHE:
        _CACHE["nc"] = build()
    nc = _CACHE["nc"]
    res = run_bass_kernel_spmd(nc, in_maps, core_ids=list(range(8)))
    out = np.empty((4, 4096, D), np.float32)
    for core in range(8):
        b, par = core // 2, core % 2
        out[b, par * NTOK:(par + 1) * NTOK, :] = res.results[core]["outT"].T
    return out
```
